# Optimizing a Trainium2 kernel written in Bass

```python
import jax
import jax.numpy as jnp
from jax import lax
import numpy as np

D_MODEL = 2048
BATCH = 2
SEQ = 8192
DEPTH = 2

GRID_W = 64
CTX_LEN = 256
HEAD_DIM = 128
N_Q_HEADS = 8
N_KV_HEADS = 2
Q_PER_KV = N_Q_HEADS // N_KV_HEADS
Q_DIM = N_Q_HEADS * HEAD_DIM
KV_DIM = N_KV_HEADS * HEAD_DIM
WINDOW = 128
ATTN_BLOCK = 128
ATTN_SCALE = HEAD_DIM ** -0.5
ROPE_BASE = 10000.0
ROPE_PAIRS = HEAD_DIM // 4
CONV_DIM = D_MODEL // 2
SHORT_CONV_W = 3
EVEN_IN_DIM = Q_DIM + 2 * KV_DIM + 3 * CONV_DIM
EVEN_SPLITS = (Q_DIM, Q_DIM + KV_DIM, Q_DIM + 2 * KV_DIM, Q_DIM + 2 * KV_DIM + CONV_DIM,
               Q_DIM + 2 * KV_DIM + 2 * CONV_DIM)
D_FF = 5632
D_RNN = D_MODEL
N_RNN_HEADS = 16
RNN_HEAD_DIM = D_RNN // N_RNN_HEADS
N_DIRS = 2
RG_CONV_W = 4
RG_CONV_PAD_LEFT = 2
RG_C = 8.0
N_EXPERTS = 8
TOP_K = 2
D_FF_EXPERT = 7168
MOE_BLOCK = 128
NORM_EPS = 1e-6
NEG_INF = -1e30

kernel_name = 'hybrid_swa_shortconv_rglru_moe_prefix_dit'


def rmsnorm(h, g):
    h32 = h.astype(jnp.float32)
    y = h32 * lax.rsqrt(jnp.mean(h32 * h32, axis=-1, keepdims=True) + NORM_EPS)
    return y.astype(h.dtype) * g


def modulate(u, shift, scale):
    return u * (1 + scale) + shift


def adaln(cvec, w_mod, b_mod, n_chunks):
    width = n_chunks * D_MODEL
    m = jax.nn.silu(cvec) @ w_mod[:, :width] + b_mod[:width]
    return jnp.split(m[..., None, :], n_chunks, axis=-1)


def depthwise_conv(u, w, b, pad_left):
    width = w.shape[0]
    t = u.shape[1]
    up = jnp.pad(u, ((0, 0), (pad_left, width - 1 - pad_left), (0, 0)))
    y = b
    for tap in range(width):
        y = y + w[tap] * up[:, tap:tap + t]
    return y


def swiglu(u, w_gate, w_up, w_down):
    return (jax.nn.silu(u @ w_gate) * (u @ w_up)) @ w_down


def axial_rope_tables(n_tokens):
    rows = n_tokens // GRID_W
    row_id = jnp.broadcast_to(jnp.arange(rows)[:, None], (rows, GRID_W)).reshape(-1).astype(jnp.float32)
    col_id = jnp.broadcast_to(jnp.arange(GRID_W)[None, :], (rows, GRID_W)).reshape(-1).astype(jnp.float32)
    inv_freq = ROPE_BASE ** (-jnp.arange(ROPE_PAIRS, dtype=jnp.float32) / ROPE_PAIRS)
    ang_r = row_id[:, None] * inv_freq
    ang_c = col_id[:, None] * inv_freq
    ang = jnp.concatenate([ang_r, ang_r, ang_c, ang_c], axis=-1)
    return jnp.cos(ang), jnp.sin(ang)


def apply_rope(t, cos, sin):
    q = ROPE_PAIRS
    shape = (1, t.shape[1]) + (1,) * (t.ndim - 3) + (HEAD_DIM,)
    cos = cos.reshape(shape).astype(t.dtype)
    sin = sin.reshape(shape).astype(t.dtype)
    rot = jnp.concatenate([-t[..., q:2 * q], t[..., :q], -t[..., 3 * q:], t[..., 2 * q:3 * q]], axis=-1)
    return t * cos + rot * sin


def context_attention(q, k, v, sinks):
    bn, t = q.shape[:2]
    s = jnp.einsum('bqhgd,bkhd->bhgqk', q, k).astype(jnp.float32) * ATTN_SCALE
    sink = jnp.broadcast_to(sinks.astype(jnp.float32).reshape(1, N_KV_HEADS, Q_PER_KV, 1, 1), s.shape[:-1] + (1,))
    p = jax.nn.softmax(jnp.concatenate([sink, s], axis=-1), axis=-1)[..., 1:].astype(v.dtype)
    o = jnp.einsum('bhgqk,bkhd->bqhgd', p, v)
    return o.reshape(bn, t, Q_DIM)


def banded_blocks(t, n_blocks):
    bn = t.shape[0]
    tp = jnp.pad(t, ((0, 0), (ATTN_BLOCK, ATTN_BLOCK), (0, 0), (0, 0)))
    tp = tp.reshape(bn, n_blocks + 2, ATTN_BLOCK, N_KV_HEADS, HEAD_DIM)
    return jnp.concatenate([tp[:, :-2], tp[:, 1:-1], tp[:, 2:]], axis=2)


def windowed_attention(q, k, v, k_ctx, v_ctx, sinks):
    bn, s_len = q.shape[:2]
    n_ctx = k_ctx.shape[1]
    nb = s_len // ATTN_BLOCK
    qb = q.reshape(bn, nb, ATTN_BLOCK, N_KV_HEADS, Q_PER_KV, HEAD_DIM)
    kw = banded_blocks(k, nb)
    vw = banded_blocks(v, nb)
    s_win = jnp.einsum('bnqhgd,bnkhd->bnhgqk', qb, kw).astype(jnp.float32) * ATTN_SCALE
    s_ctx = jnp.einsum('bnqhgd,bchd->bnhgqc', qb, k_ctx).astype(jnp.float32) * ATTN_SCALE
    qi = jnp.arange(ATTN_BLOCK)
    kj = jnp.arange(3 * ATTN_BLOCK)
    rel = kj[None, :] - ATTN_BLOCK - qi[:, None]
    key_pos = jnp.arange(nb)[:, None] * ATTN_BLOCK - ATTN_BLOCK + kj[None, :]
    mask = (jnp.abs(rel) <= WINDOW)[None] & ((key_pos >= 0) & (key_pos < s_len))[:, None, :]
    s_win = jnp.where(mask[None, :, None, None], s_win, NEG_INF)
    sink = jnp.broadcast_to(sinks.astype(jnp.float32).reshape(1, 1, N_KV_HEADS, Q_PER_KV, 1, 1),
                            s_win.shape[:-1] + (1,))
    p = jax.nn.softmax(jnp.concatenate([sink, s_ctx, s_win], axis=-1), axis=-1).astype(v.dtype)
    p_ctx = p[..., 1:1 + n_ctx]
    p_win = p[..., 1 + n_ctx:]
    o = (jnp.einsum('bnhgqc,bchd->bnqhgd', p_ctx, v_ctx)
         + jnp.einsum('bnhgqk,bnkhd->bnqhgd', p_win, vw))
    return o.reshape(bn, s_len, Q_DIM)


def rglru_coeffs(xc, ga_w, ga_b, gx_w, gx_b, lam):
    bn, t, _ = xc.shape
    xh = xc.reshape(bn, t, N_RNN_HEADS, RNN_HEAD_DIM)

    def block_diag(w, b):
        y = jnp.einsum('bthi,zhij->zbthj', xh, w).reshape(N_DIRS, bn, t, D_RNN)
        return y.astype(jnp.float32) + b.astype(jnp.float32)[:, None, None, :]

    r = jax.nn.sigmoid(block_diag(ga_w, ga_b))
    i = jax.nn.sigmoid(block_diag(gx_w, gx_b))
    log_a = -RG_C * r * jax.nn.softplus(-lam.astype(jnp.float32))[:, None, None, :]
    a = jnp.exp(log_a)
    b = jnp.sqrt(-jnp.expm1(2.0 * log_a)) * i * xc.astype(jnp.float32)[None]
    return a, b


def _affine_combine(left, right):
    a_l, b_l = left
    a_r, b_r = right
    return a_l * a_r, a_r * b_l + b_r


def linear_scan(a, b, reverse):
    return lax.associative_scan(_affine_combine, (a, b), axis=1, reverse=reverse)


def moe_swiglu(xf, router_w, router_b, w_gate, w_up, w_down):
    n_tok, d = xf.shape
    logits = (xf @ router_w).astype(jnp.float32) + router_b.astype(jnp.float32)
    top_logits, top_idx = lax.top_k(logits, TOP_K)
    top_w = jax.nn.softmax(top_logits, axis=-1)
    n_assign = n_tok * TOP_K
    flat_e = top_idx.reshape(-1)
    flat_tok = jnp.arange(n_assign, dtype=jnp.int32) // TOP_K
    flat_w = top_w.reshape(-1)
    order = jnp.argsort(flat_e)
    sorted_e = flat_e[order]
    counts = jnp.zeros((N_EXPERTS,), jnp.int32).at[flat_e].add(1)
    padded = (counts + MOE_BLOCK - 1) // MOE_BLOCK * MOE_BLOCK
    pad_end = jnp.cumsum(padded)
    pad_start = pad_end - padded
    grp_start = jnp.cumsum(counts) - counts
    rank = jnp.arange(n_assign, dtype=jnp.int32) - grp_start[sorted_e]
    dest = pad_start[sorted_e] + rank
    n_blocks = -(-n_assign // MOE_BLOCK) + N_EXPERTS
    cap = n_blocks * MOE_BLOCK
    slot_tok = jnp.full((cap,), n_tok, jnp.int32).at[dest].set(flat_tok[order])
    slot_w = jnp.zeros((cap,), jnp.float32).at[dest].set(flat_w[order])
    block_e = jnp.minimum(jnp.searchsorted(pad_end, jnp.arange(n_blocks) * MOE_BLOCK, side='right'),
                          N_EXPERTS - 1)
    x_pad = jnp.concatenate([xf, jnp.zeros((1, d), xf.dtype)], axis=0)
    xb = x_pad[slot_tok].reshape(n_blocks, MOE_BLOCK, d)

    def expert_block(args):
        xblk, e = args
        return (jax.nn.silu(xblk @ w_gate[e]) * (xblk @ w_up[e])) @ w_down[e]

    yb = lax.map(expert_block, (xb, block_e)).reshape(cap, d)
    y = jax.ops.segment_sum(yb * slot_w[:, None].astype(yb.dtype), slot_tok, num_segments=n_tok + 1)
    return y[:n_tok]


def even_layer(h_c, h_l, c, c_ctx, w_mod, b_mod, norm1_g, w_in, sinks, conv_w, conv_b, w_out,
               norm2_g, ffn_w_gate, ffn_w_up, ffn_w_down, rope_cos, rope_sin):
    sh1_l, sc1_l, g1_l, sh2_l, sc2_l, g2_l = adaln(c, w_mod, b_mod, 6)
    sh1_c, sc1_c, g1_c, sh2_c, sc2_c, g2_c = adaln(c_ctx, w_mod, b_mod, 6)

    def mixer_inputs(h, shift, scale):
        z = modulate(rmsnorm(h, norm1_g), shift, scale) @ w_in
        bn, t, _ = z.shape
        q, k, v, xin, b_gate, c_gate = jnp.split(z, EVEN_SPLITS, axis=-1)
        q = q.reshape(bn, t, N_KV_HEADS, Q_PER_KV, HEAD_DIM)
        k = k.reshape(bn, t, N_KV_HEADS, HEAD_DIM)
        v = v.reshape(bn, t, N_KV_HEADS, HEAD_DIM)
        conv_out = b_gate * depthwise_conv(c_gate * xin, conv_w, conv_b, (SHORT_CONV_W - 1) // 2)
        return q, k, v, conv_out

    q_c, k_c, v_c, conv_c = mixer_inputs(h_c, sh1_c, sc1_c)
    q_l, k_l, v_l, conv_l = mixer_inputs(h_l, sh1_l, sc1_l)
    q_l = apply_rope(q_l, rope_cos, rope_sin)
    k_l = apply_rope(k_l, rope_cos, rope_sin)
    attn_c = context_attention(q_c, k_c, v_c, sinks)
    attn_l = windowed_attention(q_l, k_l, v_l, k_c, v_c, sinks)
    h_c = h_c + g1_c * (jnp.concatenate([attn_c, conv_c], axis=-1) @ w_out)
    h_l = h_l + g1_l * (jnp.concatenate([attn_l, conv_l], axis=-1) @ w_out)
    h_c = h_c + g2_c * swiglu(modulate(rmsnorm(h_c, norm2_g), sh2_c, sc2_c), ffn_w_gate, ffn_w_up, ffn_w_down)
    h_l = h_l + g2_l * swiglu(modulate(rmsnorm(h_l, norm2_g), sh2_l, sc2_l), ffn_w_gate, ffn_w_up, ffn_w_down)
    return h_c, h_l


def odd_layer(h_c, h_l, c, c_ctx, w_mod, b_mod, norm1_g, w_in, conv_w, conv_b, ga_w, ga_b, gx_w, gx_b,
              lam, w_out, norm2_g, router_w, router_b, moe_w_gate, moe_w_up, moe_w_down):
    sh1_l, sc1_l, g1_l, sh2_l, sc2_l, g2_l = adaln(c, w_mod, b_mod, 6)
    sh1_c, sc1_c = adaln(c_ctx, w_mod, b_mod, 2)
    xr_c = modulate(rmsnorm(h_c, norm1_g), sh1_c, sc1_c) @ w_in[:, D_RNN:]
    a_c, b_c = rglru_coeffs(depthwise_conv(xr_c, conv_w, conv_b, RG_CONV_PAD_LEFT), ga_w, ga_b, gx_w, gx_b, lam)
    h0_f = linear_scan(a_c[0], b_c[0], False)[1][:, -1]
    h0_b = linear_scan(a_c[1], b_c[1], True)[1][:, 0]
    z_l = modulate(rmsnorm(h_l, norm1_g), sh1_l, sc1_l) @ w_in
    gate_l, xr_l = jnp.split(z_l, [D_RNN], axis=-1)
    a_l, b_l = rglru_coeffs(depthwise_conv(xr_l, conv_w, conv_b, RG_CONV_PAD_LEFT), ga_w, ga_b, gx_w, gx_b, lam)
    cum_f, hz_f = linear_scan(a_l[0], b_l[0], False)
    cum_b, hz_b = linear_scan(a_l[1], b_l[1], True)
    rec = cum_f * h0_f[:, None] + hz_f + cum_b * h0_b[:, None] + hz_b
    y = (rec.astype(h_l.dtype) * jax.nn.gelu(gate_l)) @ w_out
    h_l = h_l + g1_l * y
    u = modulate(rmsnorm(h_l, norm2_g), sh2_l, sc2_l)
    bn, t, d = u.shape
    moe_out = moe_swiglu(u.reshape(bn * t, d), router_w, router_b, moe_w_gate, moe_w_up, moe_w_down)
    return h_l + g2_l * moe_out.reshape(bn, t, d)


def setup_inputs(seed: int = 0) -> dict:
    key = jax.random.key(seed)
    ks = iter(jax.random.split(key, 40))
    d = D_MODEL

    def nrm(shape, scale):
        return jax.random.normal(next(ks), shape, jnp.float32) * scale

    def gain():
        return 1.0 + nrm((d,), 0.02)

    u = jax.random.uniform(next(ks), (N_DIRS, D_RNN), jnp.float32, minval=0.9, maxval=0.999)
    a0 = u ** (1.0 / RG_C)
    lam = jnp.log(a0) - jnp.log1p(-a0)
    return {
        'x': nrm((BATCH, SEQ, d), 1.0),
        'c': nrm((BATCH, d), 1.0),
        'ctx': nrm((BATCH, CTX_LEN, d), 1.0),
        'c_ctx': nrm((d,), 1.0),
        'l0_w_mod': nrm((d, 6 * d), 0.5 * d ** -0.5),
        'l0_b_mod': nrm((6 * d,), 0.01),
        'l0_norm1_g': gain(),
        'l0_w_in': nrm((d, EVEN_IN_DIM), d ** -0.5),
        'l0_sinks': nrm((N_Q_HEADS,), 0.5),
        'l0_conv_w': nrm((SHORT_CONV_W, CONV_DIM), SHORT_CONV_W ** -0.5),
        'l0_conv_b': nrm((CONV_DIM,), 0.01),
        'l0_w_out': nrm((Q_DIM + CONV_DIM, d), (Q_DIM + CONV_DIM) ** -0.5),
        'l0_norm2_g': gain(),
        'l0_ffn_w_gate': nrm((d, D_FF), d ** -0.5),
        'l0_ffn_w_up': nrm((d, D_FF), d ** -0.5),
        'l0_ffn_w_down': nrm((D_FF, d), D_FF ** -0.5),
        'l1_w_mod': nrm((d, 6 * d), 0.5 * d ** -0.5),
        'l1_b_mod': nrm((6 * d,), 0.01),
        'l1_norm1_g': gain(),
        'l1_w_in': nrm((d, 2 * D_RNN), d ** -0.5),
        'l1_conv_w': nrm((RG_CONV_W, D_RNN), RG_CONV_W ** -0.5),
        'l1_conv_b': nrm((D_RNN,), 0.01),
        'l1_gate_a_w': nrm((N_DIRS, N_RNN_HEADS, RNN_HEAD_DIM, RNN_HEAD_DIM), RNN_HEAD_DIM ** -0.5),
        'l1_gate_a_b': nrm((N_DIRS, D_RNN), 0.01),
        'l1_gate_x_w': nrm((N_DIRS, N_RNN_HEADS, RNN_HEAD_DIM, RNN_HEAD_DIM), RNN_HEAD_DIM ** -0.5),
        'l1_gate_x_b': nrm((N_DIRS, D_RNN), 0.01),
        'l1_lambda': lam,
        'l1_w_out': nrm((D_RNN, d), D_RNN ** -0.5),
        'l1_norm2_g': gain(),
        'l1_router_w': nrm((d, N_EXPERTS), d ** -0.5),
        'l1_router_b': nrm((N_EXPERTS,), 0.01),
        'l1_moe_w_gate': nrm((N_EXPERTS, d, D_FF_EXPERT), d ** -0.5),
        'l1_moe_w_up': nrm((N_EXPERTS, d, D_FF_EXPERT), d ** -0.5),
        'l1_moe_w_down': nrm((N_EXPERTS, D_FF_EXPERT, d), D_FF_EXPERT ** -0.5),
        'final_norm_g': gain(),
    }


def reference(x, c, ctx, c_ctx,
              l0_w_mod, l0_b_mod, l0_norm1_g, l0_w_in, l0_sinks, l0_conv_w, l0_conv_b, l0_w_out,
              l0_norm2_g, l0_ffn_w_gate, l0_ffn_w_up, l0_ffn_w_down,
              l1_w_mod, l1_b_mod, l1_norm1_g, l1_w_in, l1_conv_w, l1_conv_b, l1_gate_a_w, l1_gate_a_b,
              l1_gate_x_w, l1_gate_x_b, l1_lambda, l1_w_out, l1_norm2_g, l1_router_w, l1_router_b,
              l1_moe_w_gate, l1_moe_w_up, l1_moe_w_down, final_norm_g):
    rope_cos, rope_sin = axial_rope_tables(x.shape[1])
    layer_params = [
        (l0_w_mod, l0_b_mod, l0_norm1_g, l0_w_in, l0_sinks, l0_conv_w, l0_conv_b, l0_w_out,
         l0_norm2_g, l0_ffn_w_gate, l0_ffn_w_up, l0_ffn_w_down),
        (l1_w_mod, l1_b_mod, l1_norm1_g, l1_w_in, l1_conv_w, l1_conv_b, l1_gate_a_w, l1_gate_a_b,
         l1_gate_x_w, l1_gate_x_b, l1_lambda, l1_w_out, l1_norm2_g, l1_router_w, l1_router_b,
         l1_moe_w_gate, l1_moe_w_up, l1_moe_w_down),
    ]
    h_c, h_l = ctx, x
    for layer in range(DEPTH):
        if layer % 2 == 0:
            h_c, h_l = even_layer(h_c, h_l, c, c_ctx, *layer_params[layer], rope_cos, rope_sin)
        else:
            h_l = odd_layer(h_c, h_l, c, c_ctx, *layer_params[layer])
    return rmsnorm(h_l, final_norm_g)
```

```python
import contextlib
import numpy as np
import concourse.bass as bass
import concourse.mybir as mybir
from concourse.bass_utils import run_bass_kernel_spmd

F32 = mybir.dt.float32
BF16 = mybir.dt.bfloat16
I32 = mybir.dt.int32
AF = mybir.ActivationFunctionType
ALU = mybir.AluOpType
AX = mybir.AxisListType

SAME_ENGINE_SYNC = True
COMPUTE = ("pe", "act", "dve", "pool")


class Op:
    __slots__ = ("eng", "fn", "deps", "dma", "chan", "sig", "val", "idx")

    def __init__(self, eng, fn, dma, chan):
        self.eng = eng
        self.fn = fn
        self.dma = dma
        self.chan = chan
        self.deps = []
        self.sig = False
        self.val = 0
        self.idx = 0


class _Rec:
    def __init__(self):
        self.call = None

    def __getattr__(self, name):
        def f(*a, **k):
            self.call = (name, a, k)
            return self
        return f


class Prog:
    def __init__(self, nc, same_engine_sync=SAME_ENGINE_SYNC):
        self.nc = nc
        self.ops = []
        self.last_w = {}
        self.readers = {}
        self.same = same_engine_sync
        self.final_chans = set()

    def op(self, eng, fn, reads=(), writes=(), dma=False, chan=None, final=False):
        rec = _Rec()
        fn(rec)
        o = Op(eng, rec.call, dma, chan)
        o.idx = len(self.ops)
        deps = set()
        for r in reads:
            w = self.last_w.get(r)
            if w is not None:
                deps.add(w)
        for wk in writes:
            w = self.last_w.get(wk)
            if w is not None:
                deps.add(w)
            for rd in self.readers.get(wk, ()):
                deps.add(rd)
        deps.discard(o.idx)
        o.deps = sorted(deps)
        for r in reads:
            self.readers.setdefault(r, []).append(o.idx)
        for wk in writes:
            self.last_w[wk] = o.idx
            self.readers[wk] = []
        self.ops.append(o)
        if final:
            assert dma
            self.final_chans.add(chan)
        return o

    def pe(self, fn, reads=(), writes=()):
        return self.op("pe", fn, reads, writes)

    def act(self, fn, reads=(), writes=()):
        return self.op("act", fn, reads, writes)

    def dve(self, fn, reads=(), writes=()):
        return self.op("dve", fn, reads, writes)

    def pool(self, fn, reads=(), writes=()):
        return self.op("pool", fn, reads, writes)

    def dma(self, q, fn, chan, reads=(), writes=(), final=False):
        return self.op(q, fn, reads, writes, dma=True, chan=chan, final=final)

    def emit(self):
        nc = self.nc
        ops = self.ops
        for o in ops:
            for d in o.deps:
                a = ops[d]
                if a.dma:
                    continue
                if a.eng == o.eng and not o.dma:
                    if a.eng == "pe" or not self.same:
                        continue
                a.sig = True
        cnt = {e: 0 for e in COMPUTE + ("sp",)}
        chan_cnt = {}
        for o in ops:
            if o.dma:
                chan_cnt[o.chan] = chan_cnt.get(o.chan, 0) + 16
                o.val = chan_cnt[o.chan]
            elif o.sig:
                cnt[o.eng] += 1
                o.val = cnt[o.eng]
        chans = sorted(chan_cnt)
        engs = ("pe", "act", "dve", "pool", "sp")
        with contextlib.ExitStack() as st:
            sems = {}
            for e in COMPUTE:
                sems[e] = st.enter_context(nc.semaphore("s_" + e))
            for c in chans:
                sems["c:" + c] = st.enter_context(nc.semaphore("c_" + c))
            block = st.enter_context(nc.Block())
            handles = {"pe": nc.tensor, "act": nc.scalar, "dve": nc.vector,
                       "pool": nc.gpsimd, "sp": nc.sync}
            final = [(c, chan_cnt[c]) for c in sorted(self.final_chans)]

            def make(ename):
                def body(eng):
                    waited = {}
                    for o in ops:
                        if o.eng != ename:
                            continue
                        for d in o.deps:
                            a = ops[d]
                            if a.dma:
                                key, v = "c:" + a.chan, a.val
                            else:
                                if a.eng == ename and not o.dma:
                                    if ename == "pe" or not self.same:
                                        continue
                                key, v = a.eng, a.val
                            if waited.get(key, 0) >= v:
                                continue
                            waited[key] = v
                            eng.wait_ge(sems[key], v)
                        nm, a, k = o.fn
                        ins = getattr(eng, nm)(*a, **k)
                        if o.dma:
                            ins.then_inc(sems["c:" + o.chan], 16)
                        elif o.sig:
                            ins.then_inc(sems[o.eng], 1)
                    if ename == "sp":
                        for c, v in final:
                            eng.wait_ge(sems["c:" + c], v)
                return body

            used = set(o.eng for o in ops) | {"sp"}
            for e in engs:
                if e in used:
                    getattr(block, {"pe": "tensor", "act": "scalar", "dve": "vector",
                                    "pool": "gpsimd", "sp": "sync"}[e])(make(e))


D = 2048
NCH = 16
DFF = 5632
NFC = 44
EPS = 1e-6
ATT_SCALE = 128 ** -0.5


def build_A(n_tiles):
    T_OWN = 512 * n_tiles
    T_EXT = T_OWN + 256
    nc = bass.Bass("TRN2", target_bir_lowering=False)
    dt = lambda n, s, k="ExternalInput", d=F32: nc.dram_tensor(n, s, d, kind=k).ap()
    xe = dt("xe", [T_EXT, D])
    ctx = dt("ctx", [256, D])
    modv = dt("modv", [128, NCH, 8])
    grow = dt("grow", [4, D])
    ng = dt("ng", [128, NCH, 2])
    w_in = dt("w_in", [D, 4608])
    w_out = dt("w_out", [D, D])
    w_g = dt("w_g", [D, DFF])
    w_u = dt("w_u", [D, DFF])
    w_d = dt("w_d", [DFF, D])
    convw = dt("convw", [128, 8, 4])
    sinks = dt("sinks", [1, 8])
    cosT = dt("cosT", [128, T_EXT])
    sinT = dt("sinT", [128, T_EXT])
    hval = dt("hval", [128, 4])
    cst = dt("cst", [128, 3, 128])
    bmask = dt("bmask", [128, 3, 128])
    h1 = dt("h1", [T_OWN, D], "ExternalOutput")
    hc1 = dt("hc1", [256, D], "ExternalOutput")

    w_in_v = w_in.rearrange("(kc p) n -> p kc n", p=128)
    w_out_v = w_out.rearrange("(kc p) n -> p kc n", p=128)
    w_g_v = w_g.rearrange("(kc p) n -> p kc n", p=128)
    w_u_v = w_u.rearrange("(kc p) n -> p kc n", p=128)
    w_d_v = w_d.rearrange("(fc p) n -> p fc n", p=128)

    with contextlib.ExitStack() as st:
        sb = lambda n, s, d=F32: st.enter_context(nc.sbuf_tensor(n, s, d))
        hbuf = sb("hbuf", [128, 4, D])
        stg = sb("stg", [128, 1, D])
        xhat2 = [sb("xhat%d" % i, [128, D]) for i in range(2)]
        nslot = [0]
        bufA = sb("bufA", [128, NCH * 768], BF16)
        bufB = sb("bufB", [128, NCH, 512], BF16)
        qT = sb("qT", [128, 4, 8, 128], BF16)
        kT = sb("kT", [128, 2, 768], BF16)
        Vt = sb("Vt", [128, 6, 256], BF16)
        kTc = sb("kTc", [128, 2, 256], BF16)
        Vc = sb("Vc", [128, 2, 256], BF16)
        cos_t = sb("cos_t", [128, 768])
        sin_t = sb("sin_t", [128, 768])
        pT2 = [sb("pT%d" % i, [128, 5, 512], BF16) for i in range(2)]
        den_single = sb("den0", [128, 512])
        den2 = [den_single, den_single]
        m01 = sb("m01", [128, 2, 512], BF16)
        ones_row = sb("ones_row", [1, 128])
        esrow = sb("esrow", [1, 2, 512])
        pslot = [0]
        qsb = sb("qsb", [128, 512])
        t1 = sb("t1", [128, 512])
        t2 = sb("t2", [128, 512])
        xin_sb = sb("xin_sb", [128, 514])
        p_sb = sb("p_sb", [128, 514])
        acc = sb("acc", [128, 512])
        gsb = sb("gsb", [128, 512])
        wb = [sb("wb%d" % i, [128, NCH, 512], BF16) for i in range(3)]
        modv_t = sb("modv_t", [128, NCH, 8])
        ng_t = sb("ng_t", [128, NCH, 2])
        gm = sb("gm", [128, NCH, 4])
        grow_s = sb("grow_s", [128, 2, 512])
        convw_t = sb("convw_t", [128, 8, 4])
        esink = sb("esink", [128, 8])
        hval_t = sb("hval_t", [128, 4])
        cst_t = sb("cst_t", [128, 3, 128])
        ones_bf = sb("ones_bf", [128, 128], BF16)
        bmask_t = sb("bmask_t", [128, 3, 128])
        ss2 = [sb("ss%d" % i, [128, 8]) for i in range(2)]
        halo2 = sb("halo2", [128, NCH, 2], BF16)
        ps = [st.enter_context(nc.psum_tensor("ps%d" % i, [128, 512], F32)) for i in range(8)]

        P = Prog(nc)
        bank_ctr = [0]

        def nb():
            b = bank_ctr[0] % 8
            bank_ctr[0] += 1
            return b

        ld = lambda dst, src, key, q="sp": P.dma(q, lambda e: e.dma_start(out=dst, in_=src), "c_" + key, writes=[key])
        ld(modv_t[:], modv, "modv")
        ld(ng_t[:], ng, "ng")
        ld(convw_t[:], convw, "convw")
        ld(hval_t[:], hval, "hval")
        ld(cst_t[:], cst, "cst")
        ld(bmask_t[:], bmask, "bmask")
        ld(esink[:], sinks.partition_broadcast(128), "esink")
        P.act(lambda e: e.activation(esink[:], esink[:], AF.Exp), reads=["esink"], writes=["esink"])
        P.dve(lambda e: e.memset(ones_bf[:], 1.0), writes=["ones"])
        P.dve(lambda e: e.memset(ones_row[:], 1.0), writes=["ones"])
        for mi, wi in enumerate((0, 2)):
            for hh in range(4):
                P.dve(lambda e: e.tensor_copy(m01[:, mi, hh * 128:(hh + 1) * 128], bmask_t[:, wi, :]), reads=["bmask"], writes=["m01"])
        for h8 in range(8):
            P.dve(lambda e: e.tensor_scalar(esrow[0:1, h8 // 4, (h8 % 4) * 128:(h8 % 4 + 1) * 128], ones_row[0:1, :],
                                            esink[0:1, h8:h8 + 1], None, ALU.mult), reads=["ones", "esink"], writes=["esrow"])
        for j, (gi, sci) in enumerate([(0, 1), (0, 3), (1, 5), (1, 7)]):
            P.dve(lambda e, j=j, gi=gi, sci=sci: e.scalar_tensor_tensor(
                gm[:, :, j], modv_t[:, :, sci], 1.0, ng_t[:, :, gi], ALU.add, ALU.mult),
                reads=["modv", "ng"], writes=["gm"])
        ident = cst_t[:, 0, :]
        rotp = cst_t[:, 1, :]

        wslot = [0]

        wcache = {}
        hwq = [0]

        def wload(srcs, gid):
            s = wslot[0] % 3
            wslot[0] += 1
            kcn = srcs[0][2]
            ncols = max(c0 + n for _, c0, _, n in srcs)
            if gid not in wcache:
                for src, c0, kcn_, n in srcs:
                    P.dma("pool", lambda e: e.dma_start(out=wb[s][:, 0:kcn_, c0:c0 + n], in_=src), "w%d" % s, writes=["wb%d" % s])
                scr = nc.dram_tensor("scr_" + gid, [128, NCH, 512], BF16, kind="Internal").ap()
                wcache[gid] = scr
                P.dma("sp", lambda e: e.dma_start(out=scr[:, 0:kcn, 0:ncols], in_=wb[s][:, 0:kcn, 0:ncols]), "wst%d" % s,
                      reads=["wb%d" % s], writes=["scr_" + gid])
            else:
                scr = wcache[gid]
                q = "sp" if hwq[0] % 2 == 0 else "pool"
                hwq[0] += 1
                P.dma(q, lambda e: e.dma_start(out=wb[s][:, 0:kcn, 0:ncols], in_=scr[:, 0:kcn, 0:ncols]), "w%d%s" % (s, q),
                      reads=["scr_" + gid], writes=["wb%d" % s])
            return s

        gslot = [0]

        def gload(gi, nt):
            sl = gslot[0] % 2
            gslot[0] += 1
            P.dma("sp", lambda e: e.dma_start(out=grow_s[:, sl, :], in_=grow[gi:gi + 1, nt * 512:(nt + 1) * 512].partition_broadcast(128)),
                  "c_grow%d" % sl, writes=["grow%d" % sl])
            return sl

        def norm_block(src_ap, key_src, dstT, col0, gcol, shcol, key_dst):
            ns = nslot[0] % 2
            nslot[0] += 1
            xhat, ss, kx, ks = xhat2[ns], ss2[ns], "xhat%d" % ns, "ss%d" % ns
            P.dve(lambda e: e.memset(ss[:, 0:1], 0.0), writes=[ks])
            P.act(lambda e: e.activation(xhat[:], src_ap, AF.Square, accum_out=ss[:, 0:1]),
                  reads=[key_src, ks], writes=[kx, ks])
            P.act(lambda e: e.activation(ss[:, 1:2], ss[:, 0:1], AF.Sqrt, bias=EPS, scale=1.0 / D),
                  reads=[ks], writes=[ks])
            P.dve(lambda e: e.reciprocal(ss[:, 2:3], ss[:, 1:2]), reads=[ks], writes=[ks])
            P.dve(lambda e: e.tensor_scalar(xhat[:], src_ap, ss[:, 2:3], None, ALU.mult),
                  reads=[key_src, ks, kx], writes=[kx])
            for q4 in range(4):
                b = nb()
                for j in range(4):
                    c = q4 * 4 + j
                    P.pe(lambda e: e.transpose(ps[b][:, j * 128:(j + 1) * 128], xhat[:, c * 128:(c + 1) * 128], ident),
                         reads=[kx, "cst"], writes=["ps%d" % b])
                for j in range(4):
                    c = q4 * 4 + j
                    P.act(lambda e: e.activation(
                        dstT(c, col0), ps[b][:, j * 128:(j + 1) * 128], AF.Identity,
                        bias=modv_t[:, c, shcol:shcol + 1], scale=gm[:, c, gcol:gcol + 1]),
                        reads=["ps%d" % b, "gm", "modv"], writes=[key_dst])

        xnT = lambda c, col0, w=128: bufA[:, c * 768 + col0: c * 768 + col0 + w]
        unT = lambda c, col0, w=128: bufB[:, c, col0:col0 + w]

        def segment(kind, ti, kv_only=False):
            lat = kind == "lat"
            nown = 4 if lat else 2
            next_ = 6 if lat else 2
            own0 = 128 if lat else 0
            NO = nown * 128
            NE = next_ * 128
            src = xe if lat else ctx
            row0 = ti * 512 if lat else 0
            g1i, g2i = (0, 1) if lat else (2, 3)
            gc1, sh1, gc2, sh2 = (0, 0, 2, 4) if lat else (1, 2, 3, 6)
            dst = h1 if lat else hc1
            tag = "%s%d" % (kind, ti)
            if lat:
                P.dma("sp", lambda e: e.dma_start(out=cos_t[:], in_=cosT[:, row0:row0 + 768]), "c_cos", writes=["cos"])
                P.dma("sp", lambda e: e.dma_start(out=sin_t[:], in_=sinT[:, row0:row0 + 768]), "c_sin", writes=["sin"])
            for eb in range(next_):
                if lat and eb in (0, 5):
                    ap = stg[:, 0, :]
                    key = "stg0"
                else:
                    ob = eb - 1 if lat else eb
                    ap = hbuf[:, ob, :]
                    key = "hb%d" % ob
                P.dma("sp", lambda e, ap=ap, eb=eb: e.dma_start(out=ap, in_=src[row0 + eb * 128: row0 + (eb + 1) * 128, :]),
                      "c_" + key, writes=[key])
                norm_block(ap, key, xnT, eb * 128, gc1, sh1, "xnT")
            if lat:
                P.dve(lambda e: e.tensor_copy(halo2[:, :, 0:1], bufA[:].rearrange("p (c t) -> p c t", t=768)[:, :, 127:128]),
                      reads=["xnT"], writes=["halo2"])
                P.dve(lambda e: e.tensor_copy(halo2[:, :, 1:2], bufA[:].rearrange("p (c t) -> p c t", t=768)[:, :, 640:641]),
                      reads=["xnT"], writes=["halo2"])

            def proj(s, wc0, ncols, col0, n, b):
                for kc in range(NCH):
                    P.pe(lambda e, kc=kc: e.matmul(ps[b][:, 0:n], wb[s][:, kc, wc0:wc0 + 128], xnT(kc, col0, n),
                                                   start=(kc == 0), stop=(kc == NCH - 1)),
                         reads=["wb%d" % s, "xnT"], writes=["ps%d" % b])

            def rope(b, n, col0, dst, view):
                P.act(lambda e: e.copy(qsb[:, 0:n], ps[b][:, 0:n]), reads=["ps%d" % b], writes=["qsb"])
                b2 = nb()
                P.pe(lambda e: e.matmul(ps[b2][:, 0:n], rotp, qsb[:, 0:n], start=True, stop=True),
                     reads=["qsb", "cst"], writes=["ps%d" % b2])
                P.dve(lambda e: e.tensor_tensor(t1[:, 0:n], qsb[:, 0:n], cos_t[:, col0:col0 + n], ALU.mult),
                      reads=["qsb", "cos"], writes=["t1"])
                P.dve(lambda e: e.tensor_tensor(t2[:, 0:n], ps[b2][:, 0:n], sin_t[:, col0:col0 + n], ALU.mult),
                      reads=["ps%d" % b2, "sin"], writes=["t2"])
                P.dve(lambda e: e.tensor_tensor(dst, view(t1[:, 0:n]), view(t2[:, 0:n]), ALU.add),
                      reads=["t1", "t2"], writes=["qk"])

            for g2 in range(0 if kv_only else 2):
                s = wload([(w_in_v[:, :, g2 * 512:(g2 + 1) * 512], 0, NCH, 512)], "q%d" % g2)
                for j in range(4):
                    hd = g2 * 4 + j
                    b = nb()
                    proj(s, j * 128, 128, own0, NO, b)

                    v3 = lambda a: a.rearrange("p (a b) -> p a b", b=128)
                    if lat:
                        rope(b, NO, own0, qT[:, 0:nown, hd, :], v3)
                    else:
                        P.act(lambda e, b=b, hd=hd: e.copy(qT[:, 0:nown, hd, :], v3(ps[b][:, 0:NO])),
                              reads=["ps%d" % b], writes=["qk"])
            s = wload([(w_in_v[:, :, 1024:1536], 0, NCH, 512)], "kv")
            kdst = kT if lat else kTc
            for g in range(2):
                for (c0, n) in ([(0, 512), (512, 256)] if lat else [(0, 256)]):
                    b = nb()
                    proj(s, g * 128, 128, c0, n, b)

                    if lat:
                        rope(b, n, c0, kdst[:, g, c0:c0 + n], lambda a: a)
                    else:
                        P.act(lambda e, b=b, g=g, c0=c0, n=n: e.copy(kdst[:, g, c0:c0 + n], ps[b][:, 0:n]),
                              reads=["ps%d" % b], writes=["qk"])
            vdst = Vt if lat else Vc
            for eb in range(next_):
                b = nb()
                for kc in range(NCH):
                    P.pe(lambda e, kc=kc, eb=eb, b=b: e.matmul(ps[b][:, 0:256], xnT(kc, eb * 128), wb[s][:, kc, 256:512],
                                                              start=(kc == 0), stop=(kc == NCH - 1)),
                         reads=["wb%d" % s, "xnT"], writes=["ps%d" % b])
                P.act(lambda e, eb=eb, b=b: e.copy(vdst[:, eb, :], ps[b][:, 0:256]), reads=["ps%d" % b], writes=["qk"])
            if kv_only:
                return
            kv_blocks = (lambda qb: [("c", 0), ("c", 1), ("w", qb), ("w", qb + 1), ("w", qb + 2)]) if lat else \
                        (lambda qb: [("c", 0), ("c", 1)])
            for qb in range(nown):
                for g in range(2):
                    blks = kv_blocks(qb)
                    pp = pslot[0] % 2
                    pslot[0] += 1
                    pTt = pT2[pp]
                    for j, (kind_b, eb) in enumerate(blks):
                        b = nb()
                        ksrc = (kTc if (kind_b == "c") else kT)
                        P.pe(lambda e: e.matmul(ps[b][:, :], ksrc[:, g, eb * 128:(eb + 1) * 128],
                                                qT[:, qb, 4 * g:4 * g + 4, :].rearrange("p a b -> p (a b)"), start=True, stop=True),
                             reads=["qk"], writes=["ps%d" % b])
                        pk = "pT%d_%d" % (pp, j)
                        if kind_b == "w" and ((eb == 0 and ti == 0) or (eb == 5 and ti == n_tiles - 1)):
                            hb = hval_t[:, 2:3] if eb == 0 else hval_t[:, 3:4]
                            P.act(lambda e: e.activation(pTt[:, j, :], ps[b][:, :], AF.Exp, bias=hb, scale=ATT_SCALE),
                                  reads=["ps%d" % b, "hval"], writes=[pk])
                        else:
                            P.act(lambda e: e.activation(pTt[:, j, :], ps[b][:, :], AF.Exp, scale=ATT_SCALE),
                                  reads=["ps%d" % b], writes=[pk])
                        if kind_b == "w" and eb - qb != 1:
                            mi = 0 if eb - qb == 0 else 1
                            P.dve(lambda e: e.tensor_tensor(pTt[:, j, :], pTt[:, j, :], m01[:, mi, :], ALU.mult),
                                  reads=[pk, "m01"], writes=[pk])
                    bd = nb()
                    bo = nb()
                    nblk = len(blks)
                    for j, (kind_b, eb) in enumerate(blks):
                        P.pe(lambda e: e.matmul(ps[bd][:, :], ones_bf[:], pTt[:, j, :], start=(j == 0), stop=False),
                             reads=["ones", "pT%d_%d" % (pp, j)], writes=["ps%d" % bd])
                    P.pe(lambda e: e.matmul(ps[bd][:, :], ones_row[0:1, :], esrow[0:1, g, :], start=False, stop=True),
                         reads=["ones", "esrow"], writes=["ps%d" % bd])
                    for j, (kind_b, eb) in enumerate(blks):
                        vsrc = Vc if kind_b == "c" else Vt
                        P.pe(lambda e: e.matmul(ps[bo][:, :], vsrc[:, eb, g * 128:(g + 1) * 128], pTt[:, j, :],
                                                start=(j == 0), stop=(j == nblk - 1)),
                             reads=["qk", "pT%d_%d" % (pp, j)], writes=["ps%d" % bo])
                    P.dve(lambda e: e.reciprocal(den2[pp][:], ps[bd][:, :]), reads=["ps%d" % bd], writes=["den0"])
                    P.dve(lambda e: e.tensor_tensor(
                        bufB[:, 4 * g:4 * g + 4, qb * 128:(qb + 1) * 128],
                        ps[bo][:, :].rearrange("p (a b) -> p a b", b=128),
                        den2[pp][:].rearrange("p (a b) -> p a b", b=128), ALU.mult),
                        reads=["ps%d" % bo, "den0"], writes=["mixT"])
            for c in range(8):
                s = wload([(w_in_v[:, :, 1536 + c * 128:1536 + (c + 1) * 128], 0, NCH, 128),
                           (w_in_v[:, :, 2560 + c * 128:2560 + (c + 1) * 128], 128, NCH, 128),
                           (w_in_v[:, :, 3584 + c * 128:3584 + (c + 1) * 128], 256, NCH, 128)], "cv%d" % c)
                bx, bb, bc = nb(), nb(), nb()
                proj(s, 0, 128, own0, NO, bx)
                proj(s, 128, 128, own0, NO, bb)
                proj(s, 256, 128, own0, NO, bc)
                P.act(lambda e, bx=bx: e.copy(xin_sb[:, 1:1 + NO], ps[bx][:, 0:NO]), reads=["ps%d" % bx], writes=["xin_sb"])
                P.dve(lambda e, bc=bc: e.tensor_tensor(p_sb[:, 1:1 + NO], ps[bc][:, 0:NO], xin_sb[:, 1:1 + NO], ALU.mult),
                      reads=["ps%d" % bc, "xin_sb"], writes=["p_sb"])
                if lat:
                    bh = nb()
                    for kc in range(NCH):
                        P.pe(lambda e, kc=kc, bh=bh, s=s: e.matmul(ps[bh][:, 0:2], wb[s][:, kc, 0:128], halo2[:, kc, :],
                                                                 start=(kc == 0), stop=(kc == NCH - 1)),
                             reads=["wb%d" % s, "halo2"], writes=["ps%d" % bh])
                    for kc in range(NCH):
                        P.pe(lambda e, kc=kc, bh=bh, s=s: e.matmul(ps[bh][:, 2:4], wb[s][:, kc, 256:384], halo2[:, kc, :],
                                                                 start=(kc == 0), stop=(kc == NCH - 1)),
                             reads=["wb%d" % s, "halo2"], writes=["ps%d" % bh])
                    P.act(lambda e, bh=bh: e.copy(t2[:, 0:2], ps[bh][:, 0:2]), reads=["ps%d" % bh], writes=["t2"])
                    P.dve(lambda e, bh=bh: e.tensor_tensor(t1[:, 0:2], ps[bh][:, 2:4], t2[:, 0:2], ALU.mult),
                          reads=["ps%d" % bh, "t2"], writes=["t1"])
                    if ti == 0:
                        P.dve(lambda e: e.tensor_tensor(p_sb[:, 0:1], t1[:, 0:1], hval_t[:, 0:1], ALU.mult),
                              reads=["t1", "hval"], writes=["p_sb"])
                    else:
                        P.dve(lambda e: e.tensor_copy(p_sb[:, 0:1], t1[:, 0:1]), reads=["t1"], writes=["p_sb"])
                    if ti == n_tiles - 1:
                        P.dve(lambda e: e.tensor_tensor(p_sb[:, 513:514], t1[:, 1:2], hval_t[:, 1:2], ALU.mult),
                              reads=["t1", "hval"], writes=["p_sb"])
                    else:
                        P.dve(lambda e: e.tensor_copy(p_sb[:, 513:514], t1[:, 1:2]), reads=["t1"], writes=["p_sb"])
                else:
                    P.dve(lambda e: e.memset(p_sb[:, 0:1], 0.0), writes=["p_sb"])
                    P.dve(lambda e: e.memset(p_sb[:, 1 + NO:2 + NO], 0.0), writes=["p_sb"])
                P.dve(lambda e, c=c: e.tensor_scalar(acc[:, 0:NO], p_sb[:, 0:NO], convw_t[:, c, 0:1], convw_t[:, c, 3:4],
                                                    ALU.mult, ALU.add), reads=["p_sb", "convw"], writes=["acc"])
                P.dve(lambda e, c=c: e.scalar_tensor_tensor(acc[:, 0:NO], p_sb[:, 1:1 + NO], convw_t[:, c, 1:2], acc[:, 0:NO],
                                                           ALU.mult, ALU.add), reads=["p_sb", "convw", "acc"], writes=["acc"])
                P.dve(lambda e, c=c: e.scalar_tensor_tensor(acc[:, 0:NO], p_sb[:, 2:2 + NO], convw_t[:, c, 2:3], acc[:, 0:NO],
                                                           ALU.mult, ALU.add), reads=["p_sb", "convw", "acc"], writes=["acc"])
                P.dve(lambda e, c=c, bb=bb: e.tensor_tensor(bufB[:, 8 + c, 0:NO], ps[bb][:, 0:NO], acc[:, 0:NO], ALU.mult),
                      reads=["ps%d" % bb, "acc"], writes=["mixT"])
            for nt in range(4):
                s = wload([(w_out_v[:, :, nt * 512:(nt + 1) * 512], 0, NCH, 512)], "wo%d" % nt)
                gs = gload(g1i, nt)
                for ob in range(nown):
                    b = nb()
                    for kc in range(NCH):
                        P.pe(lambda e, kc=kc, ob=ob, b=b, s=s: e.matmul(ps[b][:, :], bufB[:, kc, ob * 128:(ob + 1) * 128],
                                                                        wb[s][:, kc, :], start=(kc == 0), stop=(kc == NCH - 1)),
                             reads=["wb%d" % s, "mixT"], writes=["ps%d" % b])
                    P.dve(lambda e, b=b, gs=gs: e.tensor_tensor(gsb[:], ps[b][:, :], grow_s[:, gs, :], ALU.mult),
                          reads=["ps%d" % b, "grow%d" % gs], writes=["gsb"])
                    P.dve(lambda e, ob=ob, nt=nt: e.tensor_tensor(hbuf[:, ob, nt * 512:(nt + 1) * 512],
                                                                  hbuf[:, ob, nt * 512:(nt + 1) * 512], gsb[:], ALU.add),
                          reads=["gsb", "hb%d" % ob], writes=["hb%d" % ob])
            for ob in range(nown):
                norm_block(hbuf[:, ob, :], "hb%d" % ob, unT, ob * 128, gc2, sh2, "mixT")
            aT = lambda fc, col0, w: bufA[:, fc * 512 + col0: fc * 512 + col0 + w]
            for half in range(2):
                for pr in range(11):
                    f0 = (half * 22 + pr * 2) * 128
                    s = wload([(w_g_v[:, :, f0:f0 + 256], 0, NCH, 256), (w_u_v[:, :, f0:f0 + 256], 256, NCH, 256)], "gu%d_%d" % (half, pr))
                    for j in range(2):
                        bg_, bu_ = nb(), nb()
                        for kc in range(NCH):
                            P.pe(lambda e, kc=kc, j=j, s=s, b=bg_: e.matmul(ps[b][:, 0:NO], wb[s][:, kc, j * 128:(j + 1) * 128],
                                                                           bufB[:, kc, 0:NO], start=(kc == 0), stop=(kc == NCH - 1)),
                                 reads=["wb%d" % s, "mixT"], writes=["ps%d" % bg_])
                        for kc in range(NCH):
                            P.pe(lambda e, kc=kc, j=j, s=s, b=bu_: e.matmul(ps[b][:, 0:NO], wb[s][:, kc, 256 + j * 128:256 + (j + 1) * 128],
                                                                           bufB[:, kc, 0:NO], start=(kc == 0), stop=(kc == NCH - 1)),
                                 reads=["wb%d" % s, "mixT"], writes=["ps%d" % bu_])
                        P.act(lambda e, b=bg_: e.activation(gsb[:, 0:NO], ps[b][:, 0:NO], AF.Silu), reads=["ps%d" % bg_], writes=["gsb"])
                        fc = pr * 2 + j
                        P.dve(lambda e, b=bu_, fc=fc: e.tensor_tensor(aT(fc, 0, NO), ps[b][:, 0:NO], gsb[:, 0:NO], ALU.mult),
                              reads=["ps%d" % bu_, "gsb", "xnT"], writes=["xnT"])
                for nt in range(4):
                    banks = [nb() for _ in range(nown)]
                    gs = gload(g2i, nt)
                    for kh in range(2):
                        fc0 = half * 22 + kh * 11
                        s = wload([(w_d_v[:, fc0:fc0 + 11, nt * 512:(nt + 1) * 512], 0, 11, 512)], "wd%d_%d_%d" % (half, nt, kh))
                        for ob in range(nown):
                            b = banks[ob]
                            for k in range(11):
                                P.pe(lambda e, k=k, kh=kh, ob=ob, b=b, s=s: e.matmul(
                                    ps[b][:, :], aT(kh * 11 + k, ob * 128, 128), wb[s][:, k, :],
                                    start=(kh == 0 and k == 0), stop=(kh == 1 and k == 10)),
                                    reads=["wb%d" % s, "xnT"], writes=["ps%d" % b])
                    for ob in range(nown):
                        b = banks[ob]
                        P.dve(lambda e, b=b, gs=gs: e.tensor_tensor(gsb[:], ps[b][:, :], grow_s[:, gs, :], ALU.mult),
                              reads=["ps%d" % b, "grow%d" % gs], writes=["gsb"])
                        P.dve(lambda e, ob=ob, nt=nt: e.tensor_tensor(hbuf[:, ob, nt * 512:(nt + 1) * 512],
                                                                      hbuf[:, ob, nt * 512:(nt + 1) * 512], gsb[:], ALU.add),
                              reads=["gsb", "hb%d" % ob], writes=["hb%d" % ob])
            orow0 = ti * 512 if lat else 0
            for ob in range(nown):
                P.dma("sp", lambda e, ob=ob: e.dma_start(out=dst[orow0 + ob * 128: orow0 + (ob + 1) * 128, :], in_=hbuf[:, ob, :]),
                      "o_%s%d" % (kind, ob), reads=["hb%d" % ob], final=True)

        segment("ctx", 0, kv_only=True)
        for ti in range(n_tiles):
            segment("lat", ti)
        segment("ctx", 0)
        P.emit()
    return nc


D = 2048
NCH = 16
EPS = 1e-6


def build_BC(mode, T):
    nc = bass.Bass("TRN2", target_bir_lowering=False)
    dt = lambda n, s, k="ExternalInput", d=F32: nc.dram_tensor(n, s, d, kind=k).ap()
    NB = T // 128
    h1 = dt("h1", [T, D])
    halo = dt("halo", [3, D])
    modv = dt("modv", [128, NCH, 6])
    ng1 = dt("ng1", [128, NCH, 2])
    w_in = dt("w_in", [D, 2 * D])
    convw = dt("convw", [128, NCH, 5])
    ga_w = dt("ga_w", [2, 16, 128, 128])
    gx_w = dt("gx_w", [2, 16, 128, 128])
    gab = dt("gab", [128, NCH, 4])
    lam = dt("lam", [128, NCH, 2])
    hval = dt("hval", [128, 2])
    ident_d = dt("ident", [128, 128])
    if mode == "B":
        hc1 = dt("hc1", [256, D])
        stats = dt("stats", [128, NCH, 4], "ExternalOutput")
        cstate = dt("cstate", [128, NCH, 2], "ExternalOutput")
    else:
        chain = dt("chain", [128, NCH, 14])
        w_out = dt("w_out", [D, D])
        rows = dt("rows", [1, D])
        rw = dt("rw", [128, NCH, 8])
        rb = dt("rb", [1, 8])
        h2 = dt("h2", [T, D], "ExternalOutput")
        u_o = dt("uT", [D, T], "ExternalOutput")
        wts = dt("wts", [T, 8], "ExternalOutput")
        yT_d = nc.dram_tensor("yT_d", [NCH, 128, T], BF16, kind="Internal").ap()
        w_out_v = w_out.rearrange("(kc p) n -> p kc n", p=128)
    w_in_v = w_in.rearrange("(kc p) n -> p kc n", p=128)
    TE = T + 3
    tiles = [(c0, min(512, T - c0)) for c0 in range(0, T, 512)]

    with contextlib.ExitStack() as st:
        sb = lambda n, s, d=F32: st.enter_context(nc.sbuf_tensor(n, s, d))
        xnT = sb("xnT", [128, NCH, TE], BF16)
        blk = sb("blk", [128, D])
        xhat = sb("xhat", [128, D])
        xr2 = [sb("xr%d" % i, [128, max(TE, 2048)]) for i in range(2)]
        xc2 = [sb("xc%d" % i, [128, T]) for i in range(2)]
        xcb2 = [sb("xcb%d" % i, [128, T], BF16) for i in range(2)]
        xr = xr2[0]
        av = [sb("a%d" % z, [128, T]) for z in range(2)]
        bv = [sb("b%d" % z, [128, T]) for z in range(2)]
        r_t = sb("r_t", [128, 512])
        wb = [sb("wb%d" % i, [128, NCH, 256], BF16) for i in range(2)]
        gw = [sb("gw%d" % i, [128, 4, 128], BF16) for i in range(2)]
        modv_t = sb("modv_t", [128, NCH, 6])
        ng_t = sb("ng_t", [128, NCH, 2])
        gm = sb("gm", [128, NCH, 3])
        convw_t = sb("convw_t", [128, NCH, 5])
        gab_t = sb("gab_t", [128, NCH, 4])
        lam_t = sb("lam_t", [128, NCH, 2])
        negc = sb("negc", [128, NCH, 2])
        neg2c = sb("neg2c", [128, NCH, 2])
        hval_t = sb("hval_t", [128, 2])
        ident = sb("ident_t", [128, 128])
        ss = sb("ss", [128, 8])
        racc = sb("racc", [128, 2, 8])
        if mode == "B":
            stats_t = sb("stats_t", [128, NCH, 4])
            cst_t = sb("cst_t", [128, NCH, 2])
        else:
            gel = sb("gel", [128, T])
            ybf = sb("ybf", [128, T], BF16)
            chain_t = sb("chain_t", [128, NCH, 14])
            Hin = sb("Hin", [128, NCH, 2])
            rows_t = sb("rows_t", [128, D])
            rw_t = sb("rw_t", [128, NCH, 8])
            rb_t = sb("rb_t", [128, 8])
            lg = sb("lg", [128, 8, 8])
        ps = [st.enter_context(nc.psum_tensor("ps%d" % i, [128, 512], F32)) for i in range(8)]

        P = Prog(nc)
        bank_ctr = [0]

        def nb():
            b = bank_ctr[0] % 8
            bank_ctr[0] += 1
            return b

        ld = lambda dst, src, key, q="sp": P.dma(q, lambda e: e.dma_start(out=dst, in_=src), "c_" + key, writes=[key])
        ld(modv_t[:], modv, "modv")
        ld(ng_t[:], ng1, "ng")
        ld(convw_t[:], convw, "convw")
        ld(gab_t[:], gab, "gab")
        ld(lam_t[:], lam, "lam")
        ld(hval_t[:], hval, "hval")
        ld(ident[:], ident_d, "ident")
        P.act(lambda e: e.activation(negc[:], lam_t[:], AF.Exp, scale=-1.0), reads=["lam"], writes=["negc"])
        P.act(lambda e: e.activation(negc[:], negc[:], AF.Ln, bias=1.0), reads=["negc"], writes=["negc"])
        P.dve(lambda e: e.tensor_scalar(neg2c[:], negc[:], -16.0, None, ALU.mult), reads=["negc"], writes=["neg2c"])
        P.dve(lambda e: e.tensor_scalar(negc[:], negc[:], -8.0, None, ALU.mult), reads=["negc", "neg2c"], writes=["negc"])
        for j, (sci, gi) in enumerate([(1, 0), (3, 0), (5, 1)]):
            P.dve(lambda e, j=j, sci=sci: e.scalar_tensor_tensor(gm[:, :, j], modv_t[:, :, sci], 1.0, ng_t[:, :, gi], ALU.add, ALU.mult),
                  reads=["modv", "ng"], writes=["gm"])
        if mode == "C":
            ld(chain_t[:], chain, "chain")
            ld(rw_t[:], rw, "rw")
            ld(rb_t[:], rb.partition_broadcast(128), "rb")
            ld(rows_t[:], rows.partition_broadcast(128), "rows3")
            P.dve(lambda e: e.tensor_copy(Hin[:], chain_t[:, :, 0:2]), reads=["chain"], writes=["Hin"])
            for z in range(2):
                for j in range(3):
                    ca = 2 + z * 6 + 2 * j
                    P.dve(lambda e, z=z, ca=ca: e.tensor_tensor(Hin[:, :, z], Hin[:, :, z], chain_t[:, :, ca], ALU.mult),
                          reads=["Hin", "chain"], writes=["Hin"])
                    P.dve(lambda e, z=z, ca=ca: e.tensor_tensor(Hin[:, :, z], Hin[:, :, z], chain_t[:, :, ca + 1], ALU.add),
                          reads=["Hin", "chain"], writes=["Hin"])

        def norm_block(src_ap, key_src, ncols, col0, gcol, shcol):
            P.dve(lambda e: e.memset(ss[:, 0:1], 0.0), writes=["ss"])
            P.act(lambda e: e.activation(xhat[:], src_ap, AF.Square, accum_out=ss[:, 0:1]),
                  reads=[key_src, "ss"], writes=["xhat", "ss"])
            P.act(lambda e: e.activation(ss[:, 1:2], ss[:, 0:1], AF.Sqrt, bias=EPS, scale=1.0 / D), reads=["ss"], writes=["ss"])
            P.dve(lambda e: e.reciprocal(ss[:, 2:3], ss[:, 1:2]), reads=["ss"], writes=["ss"])
            P.dve(lambda e: e.tensor_scalar(xhat[:], src_ap, ss[:, 2:3], None, ALU.mult),
                  reads=[key_src, "ss", "xhat"], writes=["xhat"])
            for q4 in range(4):
                b = nb()
                for j in range(4):
                    c = q4 * 4 + j
                    P.pe(lambda e: e.transpose(ps[b][:, j * 128:(j + 1) * 128], xhat[:, c * 128:(c + 1) * 128], ident[:]),
                         reads=["xhat", "ident"], writes=["ps%d" % b])
                for j in range(4):
                    c = q4 * 4 + j
                    P.act(lambda e: e.activation(xnT[:, c, col0:col0 + ncols], ps[b][:, j * 128:j * 128 + ncols], AF.Identity,
                                                 bias=modv_t[:, c, shcol:shcol + 1], scale=gm[:, c, gcol:gcol + 1]),
                          reads=["ps%d" % b, "gm", "modv"], writes=["xnT"])

        wslot = [0]

        def mixer(src, Ts, has_halo, gcol, shcol, out_stats):
            nblk = Ts // 128
            P.dve(lambda e: e.memset(blk[:], 0.0), writes=["blk"])
            if has_halo:
                P.dma("sp", lambda e: e.dma_start(out=blk[0:3, :], in_=halo), "c_blk", writes=["blk"])
            norm_block(blk[:], "blk", 3, 0, gcol, shcol)
            P.dve(lambda e: e.tensor_copy(xnT[:, :, Ts + 2:Ts + 3], xnT[:, :, 2:3]), reads=["xnT"], writes=["xnT"])
            for ob in range(nblk):
                P.dma("sp", lambda e: e.dma_start(out=blk[:], in_=src[ob * 128:(ob + 1) * 128, :]), "c_blk", writes=["blk"])
                norm_block(blk[:], "blk", 128, 2 + ob * 128, gcol, shcol)
            tl = [(c0, min(512, Ts - c0)) for c0 in range(0, Ts, 512)]
            wbase = wslot[0]
            wslot[0] += NCH

            def front(h):
                s = (wbase + h) % 2
                xr, xc, xcb = xr2[s], xc2[s], xcb2[s]
                kxr, kxc, kxcb = "xr%d" % s, "xc%d" % s, "xcb%d" % s
                if s == 0:
                    kxr = "xr"
                r_all, i_all = blk, xhat
                P.dma("pool", lambda e: e.dma_start(out=wb[s][:, :, 0:128], in_=w_in_v[:, :, h * 128:(h + 1) * 128]),
                      "w%d" % s, writes=["wb%d" % s])
                P.dma("pool", lambda e: e.dma_start(out=wb[s][:, :, 128:256], in_=w_in_v[:, :, D + h * 128:D + (h + 1) * 128]),
                      "w%d" % s, writes=["wb%d" % s])
                P.dma("pool", lambda e: e.dma_start(out=gw[s][:, 0:2, :], in_=ga_w[:, h, :, :].rearrange("z i j -> i z j")),
                      "gw%d" % s, writes=["gw%d" % s])
                P.dma("pool", lambda e: e.dma_start(out=gw[s][:, 2:4, :], in_=gx_w[:, h, :, :].rearrange("z i j -> i z j")),
                      "gw%d" % s, writes=["gw%d" % s])
                for (c0, n) in [(0, min(512, Ts + 3))] + [(c, min(512, Ts + 3 - c)) for c in range(512, Ts + 3, 512)]:
                    b = nb()
                    for kc in range(NCH):
                        P.pe(lambda e: e.matmul(ps[b][:, 0:n], wb[s][:, kc, 128:256], xnT[:, kc, c0:c0 + n],
                                                start=(kc == 0), stop=(kc == NCH - 1)),
                             reads=["wb%d" % s, "xnT"], writes=["ps%d" % b])
                    P.act(lambda e: e.copy(xr[:, c0:c0 + n], ps[b][:, 0:n]), reads=["ps%d" % b], writes=[kxr])
                if has_halo:
                    P.dve(lambda e: e.tensor_scalar(xr[:, 0:2], xr[:, 0:2], hval_t[:, 0:1], None, ALU.mult),
                          reads=[kxr, "hval"], writes=[kxr])
                    P.dve(lambda e: e.tensor_scalar(xr[:, Ts + 2:Ts + 3], xr[:, Ts + 2:Ts + 3], hval_t[:, 1:2], None, ALU.mult),
                          reads=[kxr, "hval"], writes=[kxr])
                else:
                    P.dve(lambda e: e.memset(xr[:, 0:2], 0.0), reads=[kxr], writes=[kxr])
                    P.dve(lambda e: e.memset(xr[:, Ts + 2:Ts + 3], 0.0), reads=[kxr], writes=[kxr])
                P.dve(lambda e: e.tensor_scalar(xc[:, 0:Ts], xr[:, 0:Ts], convw_t[:, h, 0:1], convw_t[:, h, 4:5], ALU.mult, ALU.add),
                      reads=[kxr, "convw"], writes=[kxc])
                for k in range(1, 4):
                    P.dve(lambda e: e.scalar_tensor_tensor(xc[:, 0:Ts], xr[:, k:k + Ts], convw_t[:, h, k:k + 1], xc[:, 0:Ts],
                                                           ALU.mult, ALU.add), reads=[kxr, "convw", kxc], writes=[kxc])
                P.dve(lambda e: e.tensor_copy(xcb[:, 0:Ts], xc[:, 0:Ts]), reads=[kxc], writes=[kxcb])

            def back(h):
                s = (wbase + h) % 2
                xr, xc, xcb = xr2[s], xc2[s], xcb2[s]
                kxr, kxc, kxcb = "xr%d" % s, "xc%d" % s, "xcb%d" % s
                if s == 0:
                    kxr = "xr"
                r_all, i_all = blk, xhat
                if mode == "C":
                    for (c0, n) in tl:
                        b = nb()
                        for kc in range(NCH):
                            P.pe(lambda e: e.matmul(ps[b][:, 0:n], wb[s][:, kc, 0:128], xnT[:, kc, 2 + c0:2 + c0 + n],
                                                    start=(kc == 0), stop=(kc == NCH - 1)),
                                 reads=["wb%d" % s, "xnT"], writes=["ps%d" % b])
                        P.act(lambda e: e.activation(gel[:, c0:c0 + n], ps[b][:, 0:n], AF.Gelu), reads=["ps%d" % b], writes=["gel"])
                if out_stats is not None:
                    P.dve(lambda e: e.memset(racc[:], 0.0), writes=["racc"])
                for z in range(2):
                    for ti, (c0, n) in enumerate(tl):
                        br = nb()
                        P.pe(lambda e: e.matmul(ps[br][:, 0:n], gw[s][:, z, :], xcb[:, c0:c0 + n], start=True, stop=True),
                             reads=["gw%d" % s, kxcb], writes=["ps%d" % br])
                        if out_stats is not None:
                            P.act(lambda e: e.activation(r_all[:, c0:c0 + n], ps[br][:, 0:n], AF.Sigmoid, bias=gab_t[:, h, z:z + 1],
                                                         accum_out=racc[:, z, ti:ti + 1]),
                                  reads=["ps%d" % br, "gab", "racc"], writes=["blk", "racc"])
                        else:
                            P.act(lambda e: e.activation(r_all[:, c0:c0 + n], ps[br][:, 0:n], AF.Sigmoid, bias=gab_t[:, h, z:z + 1]),
                                  reads=["ps%d" % br, "gab"], writes=["blk"])
                    for ti, (c0, n) in enumerate(tl):
                        bi = nb()
                        P.pe(lambda e: e.matmul(ps[bi][:, 0:n], gw[s][:, 2 + z, :], xcb[:, c0:c0 + n], start=True, stop=True),
                             reads=["gw%d" % s, kxcb], writes=["ps%d" % bi])
                        P.act(lambda e: e.activation(i_all[:, c0:c0 + n], ps[bi][:, 0:n], AF.Sigmoid, bias=gab_t[:, h, 2 + z:3 + z]),
                              reads=["ps%d" % bi, "gab"], writes=["xhat"])
                    P.act(lambda e: e.activation(av[z][:, 0:Ts], r_all[:, 0:Ts], AF.Exp, scale=negc[:, h, z:z + 1]),
                          reads=["blk", "negc"], writes=["a%d" % z])
                    P.act(lambda e: e.activation(r_all[:, 0:Ts], r_all[:, 0:Ts], AF.Exp, scale=neg2c[:, h, z:z + 1]),
                          reads=["blk", "neg2c"], writes=["blk"])
                    P.act(lambda e: e.activation(r_all[:, 0:Ts], r_all[:, 0:Ts], AF.Sqrt, bias=1.0, scale=-1.0),
                          reads=["blk"], writes=["blk"])
                    P.dve(lambda e: e.tensor_tensor(i_all[:, 0:Ts], i_all[:, 0:Ts], r_all[:, 0:Ts], ALU.mult),
                          reads=["xhat", "blk"], writes=["xhat"])
                    P.dve(lambda e: e.tensor_tensor(bv[z][:, 0:Ts], i_all[:, 0:Ts], xc[:, 0:Ts], ALU.mult),
                          reads=["xhat", kxc], writes=["b%d" % z])
                if out_stats is None:
                    P.dve(lambda e: e.tensor_tensor_scan(bv[0][:, 0:Ts], av[0][:, 0:Ts], bv[0][:, 0:Ts], Hin[:, h, 0:1], ALU.mult, ALU.add),
                          reads=["a0", "b0", "Hin"], writes=["b0"])
                    P.dve(lambda e: e.tensor_tensor_scan(bv[1][:, Ts - 1::-1], av[1][:, Ts - 1::-1], bv[1][:, Ts - 1::-1],
                                                         Hin[:, h, 1:2], ALU.mult, ALU.add),
                          reads=["a1", "b1", "Hin"], writes=["b1"])
                    P.dve(lambda e: e.tensor_tensor(bv[0][:, 0:Ts], bv[0][:, 0:Ts], bv[1][:, 0:Ts], ALU.add),
                          reads=["b0", "b1"], writes=["b0"])
                    P.dve(lambda e: e.tensor_tensor(ybf[:, 0:Ts], bv[0][:, 0:Ts], gel[:, 0:Ts], ALU.mult),
                          reads=["b0", "gel"], writes=["ybf"])
                    P.dma("sp", lambda e: e.dma_start(out=yT_d[h], in_=ybf[:, 0:Ts]), "c_y", reads=["ybf"], writes=["yT_d"])
                else:
                    tile_, kind = out_stats
                    P.dve(lambda e: e.tensor_tensor_scan(bv[0][:, 0:Ts], av[0][:, 0:Ts], bv[0][:, 0:Ts], 0.0, ALU.mult, ALU.add),
                          reads=["a0", "b0"], writes=["b0"])
                    P.dve(lambda e: e.tensor_tensor_scan(bv[1][:, Ts - 1::-1], av[1][:, Ts - 1::-1], bv[1][:, Ts - 1::-1],
                                                         0.0, ALU.mult, ALU.add),
                          reads=["a1", "b1"], writes=["b1"])
                    if kind == "lat":
                        P.dve(lambda e: e.tensor_copy(tile_[:, h, 1:2], bv[0][:, Ts - 1:Ts]), reads=["b0"], writes=["stt"])
                        P.dve(lambda e: e.tensor_copy(tile_[:, h, 3:4], bv[1][:, 0:1]), reads=["b1"], writes=["stt"])
                        for z in range(2):
                            P.dve(lambda e: e.reduce_sum(ss[:, 4 + z:5 + z], racc[:, z, :], axis=AX.X), reads=["racc"], writes=["ss"])
                            P.act(lambda e: e.activation(tile_[:, h, 2 * z:2 * z + 1], ss[:, 4 + z:5 + z], AF.Exp, scale=negc[:, h, z:z + 1]),
                                  reads=["ss", "negc"], writes=["stt"])
                    else:
                        P.dve(lambda e: e.tensor_copy(tile_[:, h, 0:1], bv[0][:, Ts - 1:Ts]), reads=["b0"], writes=["stt"])
                        P.dve(lambda e: e.tensor_copy(tile_[:, h, 1:2], bv[1][:, 0:1]), reads=["b1"], writes=["stt"])


            front(0)
            for h in range(NCH):
                if h + 1 < NCH:
                    front(h + 1)
                back(h)

        if mode == "B":
            mixer(hc1, 256, False, 1, 2, (cst_t, "ctx"))
            mixer(h1, T, True, 0, 0, (stats_t, "lat"))
            P.dma("sp", lambda e: e.dma_start(out=stats, in_=stats_t[:]), "o_st", reads=["stt"], final=True)
            P.dma("sp", lambda e: e.dma_start(out=cstate, in_=cst_t[:]), "o_cs", reads=["stt"], final=True)
        else:
            mixer(h1, T, True, 0, 0, None)
            yT_v = yT_d.rearrange("c p t -> p c t")
            if T >= 2048:
                hosts = [(xc2[0], "xc0"), (xc2[1], "xc1"), (av[0], "a0"), (av[1], "a1"), (bv[0], "b0"), (bv[1], "b1"),
                         (gel, "gel"), (xr2[1], "xr1")]
                wres = [(t_[:, 0:2048].bitcast(BF16).rearrange("p (kc n) -> p kc n", n=256), k_) for t_, k_ in hosts]
            else:
                wres_t = [st.enter_context(nc.sbuf_tensor("wres%d" % i, [128, NCH, 256], BF16)) for i in range(8)]
                wres = [(wres_t[i][:], "wres%d" % i) for i in range(8)]
            for nt in range(8):
                P.dma("pool", lambda e: e.dma_start(out=wres[nt][0], in_=w_out_v[:, :, nt * 256:(nt + 1) * 256]),
                      "wres%d" % nt, writes=[wres[nt][1]])
            for ob in range(NB):
                P.dma("sp", lambda e: e.dma_start(out=xnT[:, :, 0:128], in_=yT_v[:, :, ob * 128:(ob + 1) * 128]), "c_yl",
                      reads=["yT_d"], writes=["xnT"])
                P.dma("sp", lambda e: e.dma_start(out=blk[:], in_=h1[ob * 128:(ob + 1) * 128, :]), "c_blk", writes=["blk"])
                for nt in range(8):
                    b = nb()
                    for kc in range(NCH):
                        P.pe(lambda e: e.matmul(ps[b][:, 0:256], xnT[:, kc, 0:128], wres[nt][0][:, kc, :], start=(kc == 0), stop=(kc == NCH - 1)),
                             reads=[wres[nt][1], "xnT"], writes=["ps%d" % b])
                    P.dve(lambda e: e.tensor_tensor(r_t[:, 0:256], ps[b][:, 0:256], rows_t[:, nt * 256:(nt + 1) * 256], ALU.mult),
                          reads=["ps%d" % b, "rows3"], writes=["r_t"])
                    P.dve(lambda e: e.tensor_tensor(blk[:, nt * 256:(nt + 1) * 256], blk[:, nt * 256:(nt + 1) * 256], r_t[:, 0:256], ALU.add),
                          reads=["r_t", "blk"], writes=["blk"])
                P.dma("sp", lambda e: e.dma_start(out=h2[ob * 128:(ob + 1) * 128, :], in_=blk[:]), "o_h2", reads=["blk"], final=True)
                P.dve(lambda e: e.memset(ss[:, 0:1], 0.0), writes=["ss"])
                P.act(lambda e: e.activation(xhat[:], blk[:], AF.Square, accum_out=ss[:, 0:1]), reads=["blk", "ss"], writes=["xhat", "ss"])
                P.act(lambda e: e.activation(ss[:, 1:2], ss[:, 0:1], AF.Sqrt, bias=EPS, scale=1.0 / D), reads=["ss"], writes=["ss"])
                P.dve(lambda e: e.reciprocal(ss[:, 2:3], ss[:, 1:2]), reads=["ss"], writes=["ss"])
                P.dve(lambda e: e.tensor_scalar(xhat[:], blk[:], ss[:, 2:3], None, ALU.mult),
                      reads=["blk", "ss", "xhat"], writes=["xhat"])
                uT = xr[:, 0:2048].rearrange("p (c t) -> p c t", t=128)
                for q4 in range(4):
                    b = nb()
                    for j in range(4):
                        c = q4 * 4 + j
                        P.pe(lambda e: e.transpose(ps[b][:, j * 128:(j + 1) * 128], xhat[:, c * 128:(c + 1) * 128], ident[:]),
                             reads=["xhat", "ident"], writes=["ps%d" % b])
                    for j in range(4):
                        c = q4 * 4 + j
                        P.act(lambda e: e.activation(uT[:, c, :], ps[b][:, j * 128:(j + 1) * 128], AF.Identity,
                                                     bias=modv_t[:, c, 4:5], scale=gm[:, c, 2:3]),
                              reads=["ps%d" % b, "gm", "modv"], writes=["xr"])
                P.dma("sp", lambda e: e.dma_start(out=u_o.rearrange("(c p) t -> p c t", p=128)[:, :, ob * 128:(ob + 1) * 128], in_=uT),
                      "o_u", reads=["xr"], final=True)
                if True:
                    pass
                b = nb()
                for kc in range(NCH):
                    P.pe(lambda e: e.matmul(ps[b][:, 0:8], uT[:, kc, :], rw_t[:, kc, :], start=(kc == 0), stop=(kc == NCH - 1)),
                         reads=["xr", "rw"], writes=["ps%d" % b])
                L, m1, k1, L2, m2, k2, e2, w1 = [lg[:, i, :] for i in range(8)]
                P.dve(lambda e: e.tensor_tensor(L, ps[b][:, 0:8], rb_t[:], ALU.add), reads=["ps%d" % b, "rb"], writes=["lg"])
                P.dve(lambda e: e.reduce_max(m1[:, 0:1], L, axis=AX.X), reads=["lg"], writes=["lg"])
                P.dve(lambda e: e.tensor_scalar(k1, L, m1[:, 0:1], None, ALU.is_equal), reads=["lg"], writes=["lg"])
                P.dve(lambda e: e.scalar_tensor_tensor(L2, k1, -1e30, L, ALU.mult, ALU.add), reads=["lg"], writes=["lg"])
                P.dve(lambda e: e.reduce_max(m2[:, 0:1], L2, axis=AX.X), reads=["lg"], writes=["lg"])
                P.dve(lambda e: e.tensor_scalar(k2, L2, m2[:, 0:1], None, ALU.is_equal), reads=["lg"], writes=["lg"])
                P.dve(lambda e: e.tensor_tensor(e2[:, 0:1], m2[:, 0:1], m1[:, 0:1], ALU.subtract), reads=["lg"], writes=["lg"])
                P.act(lambda e: e.activation(e2[:, 0:1], e2[:, 0:1], AF.Exp), reads=["lg"], writes=["lg"])
                P.dve(lambda e: e.tensor_scalar(w1[:, 0:1], e2[:, 0:1], 1.0, None, ALU.add), reads=["lg"], writes=["lg"])
                P.dve(lambda e: e.reciprocal(w1[:, 0:1], w1[:, 0:1]), reads=["lg"], writes=["lg"])
                P.dve(lambda e: e.tensor_tensor(w1[:, 1:2], e2[:, 0:1], w1[:, 0:1], ALU.mult), reads=["lg"], writes=["lg"])
                P.dve(lambda e: e.tensor_scalar(k1, k1, w1[:, 0:1], None, ALU.mult), reads=["lg"], writes=["lg"])
                P.dve(lambda e: e.scalar_tensor_tensor(k1, k2, w1[:, 1:2], k1, ALU.mult, ALU.add), reads=["lg"], writes=["lg"])
                P.dma("sp", lambda e: e.dma_start(out=wts[ob * 128:(ob + 1) * 128, :], in_=k1), "o_w", reads=["lg"], final=True)
        P.emit()
    return nc


D = 2048
NCH = 16
EPS = 1e-6


def build_M():
    nc = bass.Bass("TRN2", target_bir_lowering=False)
    dt = lambda n, s, k="ExternalInput", d=F32: nc.dram_tensor(n, s, d, kind=k).ap()
    wm = dt("wm", [2, D, 1536])
    bm = dt("bm", [128, 2, 12])
    cv = dt("cv", [128, NCH, 3])
    out = dt("mout", [128, 2, 12, 3], "ExternalOutput")
    with contextlib.ExitStack() as st:
        sb = lambda n, s, d=F32: st.enter_context(nc.sbuf_tensor(n, s, d))
        w_t = sb("w_t", [128, NCH, 1536])
        bm_t = sb("bm_t", [128, 2, 12])
        cv_t = sb("cv_t", [128, NCH, 3])
        o_t = sb("o_t", [128, 2, 12, 3])
        ps = [st.enter_context(nc.psum_tensor("ps%d" % i, [128, 512], F32)) for i in range(2)]
        P = Prog(nc)
        P.dma("sp", lambda e: e.dma_start(out=bm_t[:], in_=bm), "c_bm", writes=["bm"])
        P.dma("sp", lambda e: e.dma_start(out=cv_t[:], in_=cv), "c_cv", writes=["cv"])
        P.act(lambda e: e.activation(cv_t[:], cv_t[:], AF.Silu), reads=["cv"], writes=["cv"])
        for l in range(2):
            for half in range(2):
                P.dma("sp" if half == 0 else "act", lambda e: e.dma_start(out=w_t[:, :, half * 768:(half + 1) * 768],
                                                  in_=wm[l].rearrange("(kc p) n -> p kc n", p=128)[:, :, half * 768:(half + 1) * 768]),
                      "c_w%d" % half, writes=["w%d" % half])
            for j in range(12):
                b = j % 2
                for kc in range(NCH):
                    P.pe(lambda e: e.matmul(ps[b][:, 0:3], w_t[:, kc, j * 128:(j + 1) * 128], cv_t[:, kc, :],
                                            start=(kc == 0), stop=(kc == NCH - 1)), reads=["w%d" % (j // 6), "cv"], writes=["ps%d" % b])
                P.dve(lambda e: e.tensor_scalar(o_t[:, l, j, :], ps[b][:, 0:3], bm_t[:, l, j:j + 1], None, ALU.add),
                      reads=["ps%d" % b, "bm"], writes=["o"])
        P.dma("sp", lambda e: e.dma_start(out=out, in_=o_t[:]), "o_o", reads=["o"], final=True)
        P.emit()
    return nc


def build_D(R, DFF=7168):
    NFC = DFF // 128
    nc = bass.Bass("TRN2", target_bir_lowering=False)
    dt = lambda n, s, k="ExternalInput", d=F32: nc.dram_tensor(n, s, d, kind=k).ap()
    uT = dt("uT", [D, R])
    wsel = dt("wsel", [128, R // 128])
    w_g = dt("w_g", [D, DFF])
    w_u = dt("w_u", [D, DFF])
    w_d = dt("w_d", [DFF, D])
    y = dt("y", [R, D], "ExternalOutput")
    uT_v = uT.rearrange("(kc p) r -> p kc r", p=128)
    w_g_v = w_g.rearrange("(kc p) n -> p kc n", p=128)
    w_u_v = w_u.rearrange("(kc p) n -> p kc n", p=128)
    w_d_v = w_d.rearrange("(fc p) n -> p fc n", p=128)
    KQ = 14
    with contextlib.ExitStack() as st:
        sb = lambda n, s, d=F32: st.enter_context(nc.sbuf_tensor(n, s, d))
        un = sb("un", [128, NCH, 512], BF16)
        aT = sb("aT", [128, NFC, 512], BF16)
        wb = [sb("wb%d" % i, [128, NCH, 512], BF16) for i in range(3)]
        gsb = sb("gsb", [128, 512])
        ob_t = [sb("ob%d" % i, [128, D]) for i in range(4)]
        ws_t = sb("ws_t", [128, 4])
        ps = [st.enter_context(nc.psum_tensor("ps%d" % i, [128, 512], F32)) for i in range(8)]
        P = Prog(nc)
        bank_ctr = [0]

        def nb():
            b = bank_ctr[0] % 8
            bank_ctr[0] += 1
            return b
        wslot = [0]

        wcache = {}
        hwq = [0]

        def wload(srcs, gid):
            s = wslot[0] % 3
            wslot[0] += 1
            kcn = srcs[0][2]
            ncols = max(c0 + n for _, c0, _, n in srcs)
            if gid not in wcache:
                for src, c0, kcn_, n in srcs:
                    P.dma("pool", lambda e: e.dma_start(out=wb[s][:, 0:kcn_, c0:c0 + n], in_=src), "w%d" % s, writes=["wb%d" % s])
                if R > 512:
                    scr = nc.dram_tensor("scr_" + gid, [128, NCH, 512], BF16, kind="Internal").ap()
                    wcache[gid] = scr
                    P.dma("sp", lambda e: e.dma_start(out=scr[:, 0:kcn, 0:ncols], in_=wb[s][:, 0:kcn, 0:ncols]), "wst%d" % s,
                          reads=["wb%d" % s], writes=["scr_" + gid])
            else:
                scr = wcache[gid]
                q = "sp" if hwq[0] % 2 == 0 else "pool"
                hwq[0] += 1
                P.dma(q, lambda e: e.dma_start(out=wb[s][:, 0:kcn, 0:ncols], in_=scr[:, 0:kcn, 0:ncols]), "w%d%s" % (s, q),
                      reads=["scr_" + gid], writes=["wb%d" % s])
            return s
        for rt in range((R + 511) // 512):
            r0 = rt * 512
            NR = min(512, R - r0)
            NOB = NR // 128
            P.dma("pool", lambda e: e.dma_start(out=un[:, :, 0:NR], in_=uT_v[:, :, r0:r0 + NR]), "c_un", writes=["un"])
            P.dma("sp", lambda e: e.dma_start(out=ws_t[:, 0:NOB], in_=wsel[:, rt * 4:rt * 4 + NOB]), "c_ws", writes=["ws"])
            for pr in range(NFC // 2):
                f0 = pr * 256
                s = wload([(w_g_v[:, :, f0:f0 + 256], 0, NCH, 256), (w_u_v[:, :, f0:f0 + 256], 256, NCH, 256)], "gu%d" % pr)
                for j in range(2):
                    bg_, bu_ = nb(), nb()
                    for kc in range(NCH):
                        P.pe(lambda e: e.matmul(ps[bg_][:, 0:NR], wb[s][:, kc, j * 128:(j + 1) * 128], un[:, kc, 0:NR],
                                                start=(kc == 0), stop=(kc == NCH - 1)), reads=["wb%d" % s, "un"], writes=["ps%d" % bg_])
                    for kc in range(NCH):
                        P.pe(lambda e: e.matmul(ps[bu_][:, 0:NR], wb[s][:, kc, 256 + j * 128:256 + (j + 1) * 128], un[:, kc, 0:NR],
                                                start=(kc == 0), stop=(kc == NCH - 1)), reads=["wb%d" % s, "un"], writes=["ps%d" % bu_])
                    P.act(lambda e: e.activation(gsb[:, 0:NR], ps[bg_][:, 0:NR], AF.Silu), reads=["ps%d" % bg_], writes=["gsb"])
                    P.dve(lambda e: e.tensor_tensor(aT[:, pr * 2 + j, 0:NR], ps[bu_][:, 0:NR], gsb[:, 0:NR], ALU.mult),
                          reads=["ps%d" % bu_, "gsb"], writes=["aT"])
            for nt in range(4):
                banks = [nb() for _ in range(NOB)]
                for kq in range(NFC // KQ):
                    s = wload([(w_d_v[:, kq * KQ:(kq + 1) * KQ, nt * 512:(nt + 1) * 512], 0, KQ, 512)], "wd%d_%d" % (nt, kq))
                    for ob in range(NOB):
                        b = banks[ob]
                        for k in range(KQ):
                            P.pe(lambda e: e.matmul(ps[b][:, :], aT[:, kq * KQ + k, ob * 128:(ob + 1) * 128], wb[s][:, k, :],
                                                    start=(kq == 0 and k == 0), stop=(kq == NFC // KQ - 1 and k == KQ - 1)),
                                 reads=["wb%d" % s, "aT"], writes=["ps%d" % b])
                for ob in range(NOB):
                    b = banks[ob]
                    P.dve(lambda e: e.tensor_scalar(ob_t[ob][:, nt * 512:(nt + 1) * 512], ps[b][:, :], ws_t[:, ob:ob + 1], None, ALU.mult),
                          reads=["ps%d" % b, "ws"], writes=["obuf%d" % ob])
                    if nt == 3:
                        P.dma("sp", lambda e: e.dma_start(out=y[r0 + ob * 128:r0 + (ob + 1) * 128, :], in_=ob_t[ob][:]),
                              "o_y%d" % ob, reads=["obuf%d" % ob], final=True)
        P.emit()
    return nc


def build_E(T):
    nc = bass.Bass("TRN2", target_bir_lowering=False)
    dt = lambda n, s, k="ExternalInput", d=F32: nc.dram_tensor(n, s, d, kind=k).ap()
    h2 = dt("h2", [T, D])
    yp = dt("yp", [T, 2, D])
    rows = dt("rows", [2, D])
    out = dt("out", [T, D], "ExternalOutput")
    with contextlib.ExitStack() as st:
        sb = lambda n, s, d=F32: st.enter_context(nc.sbuf_tensor(n, s, d))
        hb = [sb("hb%d" % i, [128, D]) for i in range(2)]
        yb = [sb("yb%d" % i, [128, 2, D]) for i in range(2)]
        junk = sb("junk", [128, D])
        rows_t = sb("rows_t", [128, 2, D])
        ss = sb("ss", [128, 2, 4])
        P = Prog(nc)
        for i in range(2):
            P.dma("sp", lambda e: e.dma_start(out=rows_t[:, i, :], in_=rows[i:i + 1, :].partition_broadcast(128)), "c_r%d" % i, writes=["rows"])
        for ob in range(T // 128):
            s = ob % 2
            H, Y, S_ = hb[s], yb[s], ss[:, s, :]
            P.dma("sp", lambda e: e.dma_start(out=H[:], in_=h2[ob * 128:(ob + 1) * 128, :]), "c_h%d" % s, writes=["h%d" % s])
            P.dma("act", lambda e: e.dma_start(out=Y[:], in_=yp[ob * 128:(ob + 1) * 128, :, :]), "c_y%d" % s, writes=["y%d" % s])
            P.dve(lambda e: e.tensor_tensor(Y[:, 0, :], Y[:, 0, :], Y[:, 1, :], ALU.add), reads=["y%d" % s], writes=["y%d" % s])
            P.dve(lambda e: e.tensor_tensor(Y[:, 0, :], Y[:, 0, :], rows_t[:, 0, :], ALU.mult), reads=["y%d" % s, "rows"], writes=["y%d" % s])
            P.dve(lambda e: e.tensor_tensor(H[:], H[:], Y[:, 0, :], ALU.add), reads=["y%d" % s, "h%d" % s], writes=["h%d" % s])
            P.dve(lambda e: e.memset(S_[:, 0:1], 0.0), writes=["ss%d" % s])
            P.act(lambda e: e.activation(junk[:], H[:], AF.Square, accum_out=S_[:, 0:1]), reads=["h%d" % s, "ss%d" % s], writes=["junk", "ss%d" % s])
            P.act(lambda e: e.activation(S_[:, 1:2], S_[:, 0:1], AF.Sqrt, bias=EPS, scale=1.0 / D), reads=["ss%d" % s], writes=["ss%d" % s])
            P.dve(lambda e: e.reciprocal(S_[:, 2:3], S_[:, 1:2]), reads=["ss%d" % s], writes=["ss%d" % s])
            P.dve(lambda e: e.scalar_tensor_tensor(H[:], H[:], S_[:, 2:3], rows_t[:, 1, :], ALU.mult, ALU.mult),
                  reads=["h%d" % s, "ss%d" % s, "rows"], writes=["h%d" % s])
            P.dma("sp", lambda e: e.dma_start(out=out[ob * 128:(ob + 1) * 128, :], in_=H[:]), "o_%d" % s, reads=["h%d" % s], final=True)
        P.emit()
    return nc

import numpy as np

GRID_W = 64


def fm(v):
    return np.ascontiguousarray(np.asarray(v, np.float32).reshape(16, 128).T)


def rope_tables(pos):
    pos = np.asarray(pos, np.int64)
    row = (pos // GRID_W).astype(np.float32)
    col = (pos % GRID_W).astype(np.float32)
    inv = (10000.0 ** (-np.arange(32, dtype=np.float32) / 32)).astype(np.float32)
    ar = row[:, None] * inv
    ac = col[:, None] * inv
    ang = np.concatenate([ar, ar, ac, ac], axis=-1)
    sgn = np.ones(128, np.float32)
    sgn[0:32] = -1
    sgn[64:96] = -1
    return (np.ascontiguousarray(np.cos(ang).T.astype(np.float32)),
            np.ascontiguousarray((np.sin(ang) * sgn).T.astype(np.float32)))


def consts_A():
    cst = np.zeros((128, 3, 128), np.float32)
    cst[:, 0, :] = np.eye(128, dtype=np.float32)
    perm = np.arange(128)
    perm[0:32] += 32
    perm[32:64] -= 32
    perm[64:96] += 32
    perm[96:128] -= 32
    for m in range(128):
        cst[perm[m], 1, m] = 1.0
    j = np.arange(128)[:, None]
    i = np.arange(128)[None, :]
    bm = np.zeros((128, 3, 128), np.float32)
    for wi in range(3):
        jj = wi * 128 + j
        valid = (jj >= i) & (jj <= i + 256)
        bm[:, wi, :] = np.where(valid, 1.0, 0.0)
    return cst, bm


def prep_A(x_b, ctx_b, mod0, core_in_seq, n_cores_seq, n_tiles, p):
    T = 512 * n_tiles
    S = x_b.shape[0]
    lo = core_in_seq * T - 128
    xe = np.zeros((T + 256, 2048), np.float32)
    a, b = max(lo, 0), min(lo + T + 256, S)
    xe[a - lo:b - lo] = x_b[a:b]
    pos = np.arange(lo, lo + T + 256)
    cosT, sinT = rope_tables(np.clip(pos, 0, S - 1))
    vl = 1.0 if core_in_seq > 0 else 0.0
    vr = 1.0 if core_in_seq < n_cores_seq - 1 else 0.0
    hval = np.zeros((128, 4), np.float32)
    hval[:, 0] = vl
    hval[:, 1] = vr
    hval[:, 2] = 0.0 if vl else -1e30
    hval[:, 3] = 0.0 if vr else -1e30
    modv = np.stack([fm(mod0["sh1_l"]), fm(mod0["sc1_l"]), fm(mod0["sh1_c"]), fm(mod0["sc1_c"]),
                     fm(mod0["sh2_l"]), fm(mod0["sc2_l"]), fm(mod0["sh2_c"]), fm(mod0["sc2_c"])], axis=-1)
    grow = np.stack([mod0["g1_l"], mod0["g2_l"], mod0["g1_c"], mod0["g2_c"]]).astype(np.float32)
    ng = np.stack([fm(p["norm1_g"]), fm(p["norm2_g"])], axis=-1)
    cw = np.zeros((128, 8, 4), np.float32)
    for k in range(3):
        cw[:, :, k] = p["conv_w"][k].reshape(8, 128).T
    cw[:, :, 3] = p["conv_b"].reshape(8, 128).T
    cst, bm = consts_A()
    return {"xe": xe, "ctx": np.ascontiguousarray(ctx_b, dtype=np.float32), "modv": np.ascontiguousarray(modv),
            "grow": np.ascontiguousarray(grow), "ng": np.ascontiguousarray(ng),
            "w_in": p["w_in"], "w_out": p["w_out"], "w_g": p["ffn_w_gate"], "w_u": p["ffn_w_up"], "w_d": p["ffn_w_down"],
            "convw": cw, "sinks": p["sinks"].reshape(1, 8).astype(np.float32), "cosT": cosT, "sinT": sinT,
            "hval": hval, "cst": cst, "bmask": bm}

import numpy as np

def fm2(a):
    return np.ascontiguousarray(np.stack([fm(r) for r in a], axis=-1))

def prep_BC_common(h1_own, halo3, vl, vr, mod1, p):
    hval = np.zeros((128, 2), np.float32); hval[:, 0] = vl; hval[:, 1] = vr
    cw = np.concatenate([p["conv_w"], p["conv_b"][None]], axis=0)
    return {"h1": np.ascontiguousarray(h1_own, dtype=np.float32), "halo": np.ascontiguousarray(halo3, dtype=np.float32),
            "modv": fm2([mod1["sh1_l"], mod1["sc1_l"], mod1["sh1_c"], mod1["sc1_c"], mod1["sh2_l"], mod1["sc2_l"]]),
            "ng1": fm2([p["norm1_g"], p["norm2_g"]]), "w_in": p["w_in"], "convw": fm2(cw),
            "ga_w": p["gate_a_w"], "gx_w": p["gate_x_w"],
            "gab": fm2([p["gate_a_b"][0], p["gate_a_b"][1], p["gate_x_b"][0], p["gate_x_b"][1]]),
            "lam": fm2([p["lambda"][0], p["lambda"][1]]), "hval": hval, "ident": np.eye(128, dtype=np.float32)}

def prep_C_extra(chain14, mod1, p):
    rw = np.ascontiguousarray(p["router_w"].reshape(16, 128, 8).transpose(1, 0, 2)).astype(np.float32)
    return {"chain": np.ascontiguousarray(chain14, dtype=np.float32), "w_out": p["w_out"],
            "rows": mod1["g1_l"].reshape(1, 2048).astype(np.float32), "rw": rw,
            "rb": p["router_b"].reshape(1, 8).astype(np.float32)}


N_CORES = 8
T_CORE = 2048
SEQ = 8192


def _run(nc, in_maps):
    res = run_bass_kernel_spmd(nc, in_maps, core_ids=list(range(len(in_maps))))
    return res.results


def _unfm(t):
    return np.ascontiguousarray(t.transpose(1, 0)).reshape(-1)


def kernel(**inp):
    f32 = lambda a: np.ascontiguousarray(np.asarray(a), dtype=np.float32)
    x, c, ctx, c_ctx = f32(inp["x"]), f32(inp["c"]), f32(inp["ctx"]), f32(inp["c_ctx"])
    p0 = {k[3:]: f32(v) for k, v in inp.items() if k.startswith("l0_")}
    p1 = {k[3:]: f32(v) for k, v in inp.items() if k.startswith("l1_")}
    fng = f32(inp["final_norm_g"])

    cvs = [c[0], c[1], c_ctx]
    cv = np.ascontiguousarray(np.stack([fm(v) for v in cvs], axis=-1))
    maps = []
    for j in range(N_CORES):
        sl = slice(j * 1536, (j + 1) * 1536)
        wm = np.ascontiguousarray(np.stack([p0["w_mod"][:, sl], p1["w_mod"][:, sl]]))
        bm = np.ascontiguousarray(np.stack([p0["b_mod"][sl].reshape(12, 128).T, p1["b_mod"][sl].reshape(12, 128).T], axis=1))
        maps.append({"wm": wm, "bm": bm, "cv": cv})
    rM = _run(build_M(), maps)
    mod = np.zeros((2, 3, 12288), np.float32)
    for j in range(N_CORES):
        mo = rM[j]["mout"]
        for l in range(2):
            for v in range(3):
                mod[l, v, j * 1536:(j + 1) * 1536] = mo[:, l, :, v].T.reshape(-1)
    names = ["sh1", "sc1", "g1", "sh2", "sc2", "g2"]

    def modd(l, b):
        d = {}
        for i, n in enumerate(names):
            d[n + "_l"] = mod[l, b, i * 2048:(i + 1) * 2048]
            d[n + "_c"] = mod[l, 2, i * 2048:(i + 1) * 2048]
        return d

    maps = [prep_A(x[cid // 4], ctx[cid // 4], modd(0, cid // 4), cid % 4, 4, 4, p0) for cid in range(N_CORES)]
    rA = _run(build_A(4), maps)
    h1 = [rA[cid]["h1"] for cid in range(N_CORES)]
    hc1 = [rA[(cid // 4) * 4]["hc1"] for cid in range(N_CORES)]
    del maps

    commons = []
    for cid in range(N_CORES):
        k = cid % 4
        halo = np.zeros((3, 2048), np.float32)
        if k > 0:
            halo[0:2] = h1[cid - 1][-2:]
        if k < 3:
            halo[2] = h1[cid + 1][0]
        commons.append(prep_BC_common(h1[cid], halo, 1.0 if k > 0 else 0.0, 1.0 if k < 3 else 0.0, modd(1, cid // 4), p1))
    rB = _run(build_BC("B", T_CORE), [dict(commons[cid], hc1=hc1[cid]) for cid in range(N_CORES)])

    maps = []
    for cid in range(N_CORES):
        b, k = cid // 4, cid % 4
        chain = np.zeros((128, 16, 14), np.float32)
        chain[:, :, 0:2] = rB[cid]["cstate"]
        fwd = [None] * (3 - k) + [b * 4 + cc for cc in range(0, k)]
        bwd = [None] * k + [b * 4 + cc for cc in range(3, k, -1)]
        for j in range(3):
            for z, lst in enumerate((fwd, bwd)):
                ca = 2 + z * 6 + 2 * j
                if lst[j] is None:
                    chain[:, :, ca] = 1.0
                else:
                    st_ = rB[lst[j]]["stats"]
                    chain[:, :, ca] = st_[:, :, 2 * z]
                    chain[:, :, ca + 1] = st_[:, :, 2 * z + 1]
        maps.append(dict(commons[cid], **prep_C_extra(chain, modd(1, b), p1)))
    rC = _run(build_BC("C", T_CORE), maps)
    del maps, commons
    h2 = [rC[cid]["h2"] for cid in range(N_CORES)]
    uT_all = np.concatenate([rC[cid]["uT"] for cid in range(N_CORES)], axis=1)
    wts_all = np.concatenate([rC[cid]["wts"] for cid in range(N_CORES)], axis=0)
    del rC

    idx = [np.nonzero(wts_all[:, e] > 0)[0] for e in range(8)]
    R = max(512, int(-(-max(len(i) for i in idx) // 256) * 256))
    maps = []
    for e in range(8):
        n = len(idx[e])
        us = np.zeros((2048, R), np.float32)
        us[:, :n] = uT_all[:, idx[e]]
        w = np.zeros((R,), np.float32)
        w[:n] = wts_all[idx[e], e]
        maps.append({"uT": us, "wsel": np.ascontiguousarray(w.reshape(-1, 128).T),
                     "w_g": p1["moe_w_gate"][e], "w_u": p1["moe_w_up"][e], "w_d": p1["moe_w_down"][e]})
    rD = _run(build_D(R), maps)
    del maps, uT_all
    n_tok = wts_all.shape[0]
    yp = np.zeros((n_tok, 2, 2048), np.float32)
    slot = np.zeros((n_tok,), np.int64)
    for e in range(8):
        n = len(idx[e])
        ok = slot[idx[e]] < 2
        ii = idx[e][ok]
        yp[ii, slot[ii]] = rD[e]["y"][:n][ok]
        slot[ii] += 1
    del rD

    maps = []
    for cid in range(N_CORES):
        b = cid // 4
        rows = np.ascontiguousarray(np.stack([mod[1, b, 5 * 2048:6 * 2048], fng]))
        maps.append({"h2": h2[cid], "yp": np.ascontiguousarray(yp[cid * T_CORE:(cid + 1) * T_CORE]), "rows": rows})
    rE = _run(build_E(T_CORE), maps)
    out = np.concatenate([rE[cid]["out"] for cid in range(N_CORES)], axis=0).reshape(2, SEQ, 2048)
    return np.ascontiguousarray(out, dtype=np.float32)
```

```python
import contextlib
import numpy as np
import concourse.bass as bass
import concourse.mybir as mybir
from concourse.bass_utils import run_bass_kernel_spmd

F32 = mybir.dt.float32
BF16 = mybir.dt.bfloat16
I32 = mybir.dt.int32
AF = mybir.ActivationFunctionType
ALU = mybir.AluOpType
AX = mybir.AxisListType

SAME_ENGINE_SYNC = True
COMPUTE = ("pe", "act", "dve", "pool")


class Op:
    __slots__ = ("eng", "fn", "deps", "dma", "chan", "sig", "val", "idx")

    def __init__(self, eng, fn, dma, chan):
        self.eng = eng
        self.fn = fn
        self.dma = dma
        self.chan = chan
        self.deps = []
        self.sig = False
        self.val = 0
        self.idx = 0


class _Rec:
    def __init__(self):
        self.call = None

    def __getattr__(self, name):
        def f(*a, **k):
            self.call = (name, a, k)
            return self
        return f


class Prog:
    def __init__(self, nc, same_engine_sync=SAME_ENGINE_SYNC):
        self.nc = nc
        self.ops = []
        self.last_w = {}
        self.readers = {}
        self.same = same_engine_sync
        self.final_chans = set()

    def op(self, eng, fn, reads=(), writes=(), dma=False, chan=None, final=False):
        rec = _Rec()
        fn(rec)
        o = Op(eng, rec.call, dma, chan)
        o.idx = len(self.ops)
        deps = set()
        for r in reads:
            w = self.last_w.get(r)
            if w is not None:
                deps.add(w)
        for wk in writes:
            w = self.last_w.get(wk)
            if w is not None:
                deps.add(w)
            for rd in self.readers.get(wk, ()):
                deps.add(rd)
        deps.discard(o.idx)
        o.deps = sorted(deps)
        for r in reads:
            self.readers.setdefault(r, []).append(o.idx)
        for wk in writes:
            self.last_w[wk] = o.idx
            self.readers[wk] = []
        self.ops.append(o)
        if final:
            assert dma
            self.final_chans.add(chan)
        return o

    def pe(self, fn, reads=(), writes=()):
        return self.op("pe", fn, reads, writes)

    def act(self, fn, reads=(), writes=()):
        return self.op("act", fn, reads, writes)

    def dve(self, fn, reads=(), writes=()):
        return self.op("dve", fn, reads, writes)

    def pool(self, fn, reads=(), writes=()):
        return self.op("pool", fn, reads, writes)

    def dma(self, q, fn, chan, reads=(), writes=(), final=False):
        return self.op(q, fn, reads, writes, dma=True, chan=chan, final=final)

    def emit(self):
        nc = self.nc
        ops = self.ops
        for o in ops:
            for d in o.deps:
                a = ops[d]
                if a.dma:
                    continue
                if a.eng == o.eng and not o.dma:
                    if a.eng == "pe" or not self.same:
                        continue
                a.sig = True
        cnt = {e: 0 for e in COMPUTE + ("sp",)}
        chan_cnt = {}
        for o in ops:
            if o.dma:
                chan_cnt[o.chan] = chan_cnt.get(o.chan, 0) + 16
                o.val = chan_cnt[o.chan]
            elif o.sig:
                cnt[o.eng] += 1
                o.val = cnt[o.eng]
        chans = sorted(chan_cnt)
        engs = ("pe", "act", "dve", "pool", "sp")
        with contextlib.ExitStack() as st:
            sems = {}
            for e in COMPUTE:
                sems[e] = st.enter_context(nc.semaphore("s_" + e))
            for c in chans:
                sems["c:" + c] = st.enter_context(nc.semaphore("c_" + c))
            block = st.enter_context(nc.Block())
            handles = {"pe": nc.tensor, "act": nc.scalar, "dve": nc.vector,
                       "pool": nc.gpsimd, "sp": nc.sync}
            final = [(c, chan_cnt[c]) for c in sorted(self.final_chans)]

            def make(ename):
                def body(eng):
                    waited = {}
                    for o in ops:
                        if o.eng != ename:
                            continue
                        for d in o.deps:
                            a = ops[d]
                            if a.dma:
                                key, v = "c:" + a.chan, a.val
                            else:
                                if a.eng == ename and not o.dma:
                                    if ename == "pe" or not self.same:
                                        continue
                                key, v = a.eng, a.val
                            if waited.get(key, 0) >= v:
                                continue
                            waited[key] = v
                            eng.wait_ge(sems[key], v)
                        nm, a, k = o.fn
                        ins = getattr(eng, nm)(*a, **k)
                        if o.dma:
                            ins.then_inc(sems["c:" + o.chan], 16)
                        elif o.sig:
                            ins.then_inc(sems[o.eng], 1)
                    if ename == "sp":
                        for c, v in final:
                            eng.wait_ge(sems["c:" + c], v)
                return body

            used = set(o.eng for o in ops) | {"sp"}
            for e in engs:
                if e in used:
                    getattr(block, {"pe": "tensor", "act": "scalar", "dve": "vector",
                                    "pool": "gpsimd", "sp": "sync"}[e])(make(e))


D = 2048
NCH = 16
DFF = 5632
NFC = 44
EPS = 1e-6
ATT_SCALE = 128 ** -0.5


def build_A(n_tiles):
    T_OWN = 512 * n_tiles
    T_EXT = T_OWN + 256
    nc = bass.Bass("TRN2", target_bir_lowering=False)
    dt = lambda n, s, k="ExternalInput", d=F32: nc.dram_tensor(n, s, d, kind=k).ap()
    xe = dt("xe", [T_EXT, D])
    ctx = dt("ctx", [256, D])
    modv = dt("modv", [128, NCH, 8])
    grow = dt("grow", [4, D])
    ng = dt("ng", [128, NCH, 2])
    w_in = dt("w_in", [D, 4608])
    w_out = dt("w_out", [D, D])
    w_g = dt("w_g", [D, DFF])
    w_u = dt("w_u", [D, DFF])
    w_d = dt("w_d", [DFF, D])
    convw = dt("convw", [128, 8, 4])
    sinks = dt("sinks", [1, 8])
    cosT = dt("cosT", [128, T_EXT])
    sinT = dt("sinT", [128, T_EXT])
    hval = dt("hval", [128, 4])
    cst = dt("cst", [128, 3, 128])
    bmask = dt("bmask", [128, 3, 128])
    h1 = dt("h1", [T_OWN, D], "ExternalOutput")
    hc1 = dt("hc1", [256, D], "ExternalOutput")

    w_in_v = w_in.rearrange("(kc p) n -> p kc n", p=128)
    w_out_v = w_out.rearrange("(kc p) n -> p kc n", p=128)
    w_g_v = w_g.rearrange("(kc p) n -> p kc n", p=128)
    w_u_v = w_u.rearrange("(kc p) n -> p kc n", p=128)
    w_d_v = w_d.rearrange("(fc p) n -> p fc n", p=128)

    with contextlib.ExitStack() as st:
        sb = lambda n, s, d=F32: st.enter_context(nc.sbuf_tensor(n, s, d))
        hbuf = sb("hbuf", [128, 4, D])
        stg = sb("stg", [128, 1, D])
        xhat2 = [sb("xhat%d" % i, [128, D]) for i in range(2)]
        nslot = [0]
        bufA = sb("bufA", [128, NCH * 768], BF16)
        bufB = sb("bufB", [128, NCH, 512], BF16)
        qT = sb("qT", [128, 4, 8, 128], BF16)
        kT = sb("kT", [128, 2, 768], BF16)
        Vt = sb("Vt", [128, 6, 256], BF16)
        kTc = sb("kTc", [128, 2, 256], BF16)
        Vc = sb("Vc", [128, 2, 256], BF16)
        cos_t = sb("cos_t", [128, 768])
        sin_t = sb("sin_t", [128, 768])
        pT2 = [sb("pT%d" % i, [128, 5, 512], BF16) for i in range(2)]
        den_single = sb("den0", [128, 512])
        den2 = [den_single, den_single]
        m01 = sb("m01", [128, 2, 512], BF16)
        ones_row = sb("ones_row", [1, 128])
        esrow = sb("esrow", [1, 2, 512])
        pslot = [0]
        qsb = sb("qsb", [128, 512])
        t1 = sb("t1", [128, 512])
        t2 = sb("t2", [128, 512])
        xin_sb = sb("xin_sb", [128, 514])
        p_sb = sb("p_sb", [128, 514])
        acc = sb("acc", [128, 512])
        gsb = sb("gsb", [128, 512])
        wb = [sb("wb%d" % i, [128, NCH, 512], BF16) for i in range(3)]
        modv_t = sb("modv_t", [128, NCH, 8])
        ng_t = sb("ng_t", [128, NCH, 2])
        gm = sb("gm", [128, NCH, 4])
        grow_s = sb("grow_s", [128, 2, 512])
        convw_t = sb("convw_t", [128, 8, 4])
        esink = sb("esink", [128, 8])
        hval_t = sb("hval_t", [128, 4])
        cst_t = sb("cst_t", [128, 3, 128])
        ones_bf = sb("ones_bf", [128, 128], BF16)
        bmask_t = sb("bmask_t", [128, 3, 128])
        ss2 = [sb("ss%d" % i, [128, 8]) for i in range(2)]
        halo2 = sb("halo2", [128, NCH, 2], BF16)
        ps = [st.enter_context(nc.psum_tensor("ps%d" % i, [128, 512], F32)) for i in range(8)]

        P = Prog(nc)
        bank_ctr = [0]

        def nb():
            b = bank_ctr[0] % 8
            bank_ctr[0] += 1
            return b

        ld = lambda dst, src, key, q="sp": P.dma(q, lambda e: e.dma_start(out=dst, in_=src), "c_" + key, writes=[key])
        ld(modv_t[:], modv, "modv")
        ld(ng_t[:], ng, "ng")
        ld(convw_t[:], convw, "convw")
        ld(hval_t[:], hval, "hval")
        ld(cst_t[:], cst, "cst")
        ld(bmask_t[:], bmask, "bmask")
        ld(esink[:], sinks.partition_broadcast(128), "esink")
        P.act(lambda e: e.activation(esink[:], esink[:], AF.Exp), reads=["esink"], writes=["esink"])
        P.dve(lambda e: e.memset(ones_bf[:], 1.0), writes=["ones"])
        P.dve(lambda e: e.memset(ones_row[:], 1.0), writes=["ones"])
        for mi, wi in enumerate((0, 2)):
            for hh in range(4):
                P.dve(lambda e: e.tensor_copy(m01[:, mi, hh * 128:(hh + 1) * 128], bmask_t[:, wi, :]), reads=["bmask"], writes=["m01"])
        for h8 in range(8):
            P.dve(lambda e: e.tensor_scalar(esrow[0:1, h8 // 4, (h8 % 4) * 128:(h8 % 4 + 1) * 128], ones_row[0:1, :],
                                            esink[0:1, h8:h8 + 1], None, ALU.mult), reads=["ones", "esink"], writes=["esrow"])
        for j, (gi, sci) in enumerate([(0, 1), (0, 3), (1, 5), (1, 7)]):
            P.dve(lambda e, j=j, gi=gi, sci=sci: e.scalar_tensor_tensor(
                gm[:, :, j], modv_t[:, :, sci], 1.0, ng_t[:, :, gi], ALU.add, ALU.mult),
                reads=["modv", "ng"], writes=["gm"])
        ident = cst_t[:, 0, :]
        rotp = cst_t[:, 1, :]

        wslot = [0]

        wcache = {}
        hwq = [0]

        def wload(srcs, gid):
            s = wslot[0] % 3
            wslot[0] += 1
            kcn = srcs[0][2]
            ncols = max(c0 + n for _, c0, _, n in srcs)
            if gid not in wcache:
                for src, c0, kcn_, n in srcs:
                    P.dma("pool", lambda e: e.dma_start(out=wb[s][:, 0:kcn_, c0:c0 + n], in_=src), "w%d" % s, writes=["wb%d" % s])
                scr = nc.dram_tensor("scr_" + gid, [128, NCH, 512], BF16, kind="Internal").ap()
                wcache[gid] = scr
                P.dma("sp", lambda e: e.dma_start(out=scr[:, 0:kcn, 0:ncols], in_=wb[s][:, 0:kcn, 0:ncols]), "wst%d" % s,
                      reads=["wb%d" % s], writes=["scr_" + gid])
            else:
                scr = wcache[gid]
                q = "sp" if hwq[0] % 2 == 0 else "pool"
                hwq[0] += 1
                P.dma(q, lambda e: e.dma_start(out=wb[s][:, 0:kcn, 0:ncols], in_=scr[:, 0:kcn, 0:ncols]), "w%d%s" % (s, q),
                      reads=["scr_" + gid], writes=["wb%d" % s])
            return s

        gslot = [0]

        def gload(gi, nt):
            sl = gslot[0] % 2
            gslot[0] += 1
            P.dma("sp", lambda e: e.dma_start(out=grow_s[:, sl, :], in_=grow[gi:gi + 1, nt * 512:(nt + 1) * 512].partition_broadcast(128)),
                  "c_grow%d" % sl, writes=["grow%d" % sl])
            return sl

        def norm_block(src_ap, key_src, dstT, col0, gcol, shcol, key_dst):
            ns = nslot[0] % 2
            nslot[0] += 1
            xhat, ss, kx, ks = xhat2[ns], ss2[ns], "xhat%d" % ns, "ss%d" % ns
            P.dve(lambda e: e.memset(ss[:, 0:1], 0.0), writes=[ks])
            P.act(lambda e: e.activation(xhat[:], src_ap, AF.Square, accum_out=ss[:, 0:1]),
                  reads=[key_src, ks], writes=[kx, ks])
            P.act(lambda e: e.activation(ss[:, 1:2], ss[:, 0:1], AF.Sqrt, bias=EPS, scale=1.0 / D),
                  reads=[ks], writes=[ks])
            P.dve(lambda e: e.reciprocal(ss[:, 2:3], ss[:, 1:2]), reads=[ks], writes=[ks])
            P.dve(lambda e: e.tensor_scalar(xhat[:], src_ap, ss[:, 2:3], None, ALU.mult),
                  reads=[key_src, ks, kx], writes=[kx])
            for q4 in range(4):
                b = nb()
                for j in range(4):
                    c = q4 * 4 + j
                    P.pe(lambda e: e.transpose(ps[b][:, j * 128:(j + 1) * 128], xhat[:, c * 128:(c + 1) * 128], ident),
                         reads=[kx, "cst"], writes=["ps%d" % b])
                for j in range(4):
                    c = q4 * 4 + j
                    P.act(lambda e: e.activation(
                        dstT(c, col0), ps[b][:, j * 128:(j + 1) * 128], AF.Identity,
                        bias=modv_t[:, c, shcol:shcol + 1], scale=gm[:, c, gcol:gcol + 1]),
                        reads=["ps%d" % b, "gm", "modv"], writes=[key_dst])

        xnT = lambda c, col0, w=128: bufA[:, c * 768 + col0: c * 768 + col0 + w]
        unT = lambda c, col0, w=128: bufB[:, c, col0:col0 + w]

        def segment(kind, ti, kv_only=False):
            lat = kind == "lat"
            nown = 4 if lat else 2
            next_ = 6 if lat else 2
            own0 = 128 if lat else 0
            NO = nown * 128
            NE = next_ * 128
            src = xe if lat else ctx
            row0 = ti * 512 if lat else 0
            g1i, g2i = (0, 1) if lat else (2, 3)
            gc1, sh1, gc2, sh2 = (0, 0, 2, 4) if lat else (1, 2, 3, 6)
            dst = h1 if lat else hc1
            tag = "%s%d" % (kind, ti)
            if lat:
                P.dma("sp", lambda e: e.dma_start(out=cos_t[:], in_=cosT[:, row0:row0 + 768]), "c_cos", writes=["cos"])
                P.dma("sp", lambda e: e.dma_start(out=sin_t[:], in_=sinT[:, row0:row0 + 768]), "c_sin", writes=["sin"])
            for eb in range(next_):
                if lat and eb in (0, 5):
                    ap = stg[:, 0, :]
                    key = "stg0"
                else:
                    ob = eb - 1 if lat else eb
                    ap = hbuf[:, ob, :]
                    key = "hb%d" % ob
                P.dma("sp", lambda e, ap=ap, eb=eb: e.dma_start(out=ap, in_=src[row0 + eb * 128: row0 + (eb + 1) * 128, :]),
                      "c_" + key, writes=[key])
                norm_block(ap, key, xnT, eb * 128, gc1, sh1, "xnT")
            if lat:
                P.dve(lambda e: e.tensor_copy(halo2[:, :, 0:1], bufA[:].rearrange("p (c t) -> p c t", t=768)[:, :, 127:128]),
                      reads=["xnT"], writes=["halo2"])
                P.dve(lambda e: e.tensor_copy(halo2[:, :, 1:2], bufA[:].rearrange("p (c t) -> p c t", t=768)[:, :, 640:641]),
                      reads=["xnT"], writes=["halo2"])

            def proj(s, wc0, ncols, col0, n, b):
                for kc in range(NCH):
                    P.pe(lambda e, kc=kc: e.matmul(ps[b][:, 0:n], wb[s][:, kc, wc0:wc0 + 128], xnT(kc, col0, n),
                                                   start=(kc == 0), stop=(kc == NCH - 1)),
                         reads=["wb%d" % s, "xnT"], writes=["ps%d" % b])

            def rope(b, n, col0, dst, view):
                P.act(lambda e: e.copy(qsb[:, 0:n], ps[b][:, 0:n]), reads=["ps%d" % b], writes=["qsb"])
                b2 = nb()
                P.pe(lambda e: e.matmul(ps[b2][:, 0:n], rotp, qsb[:, 0:n], start=True, stop=True),
                     reads=["qsb", "cst"], writes=["ps%d" % b2])
                P.dve(lambda e: e.tensor_tensor(t1[:, 0:n], qsb[:, 0:n], cos_t[:, col0:col0 + n], ALU.mult),
                      reads=["qsb", "cos"], writes=["t1"])
                P.dve(lambda e: e.tensor_tensor(t2[:, 0:n], ps[b2][:, 0:n], sin_t[:, col0:col0 + n], ALU.mult),
                      reads=["ps%d" % b2, "sin"], writes=["t2"])
                P.dve(lambda e: e.tensor_tensor(dst, view(t1[:, 0:n]), view(t2[:, 0:n]), ALU.add),
                      reads=["t1", "t2"], writes=["qk"])

            for g2 in range(0 if kv_only else 2):
                s = wload([(w_in_v[:, :, g2 * 512:(g2 + 1) * 512], 0, NCH, 512)], "q%d" % g2)
                for j in range(4):
                    hd = g2 * 4 + j
                    b = nb()
                    proj(s, j * 128, 128, own0, NO, b)

                    v3 = lambda a: a.rearrange("p (a b) -> p a b", b=128)
                    if lat:
                        rope(b, NO, own0, qT[:, 0:nown, hd, :], v3)
                    else:
                        P.act(lambda e, b=b, hd=hd: e.copy(qT[:, 0:nown, hd, :], v3(ps[b][:, 0:NO])),
                              reads=["ps%d" % b], writes=["qk"])
            s = wload([(w_in_v[:, :, 1024:1536], 0, NCH, 512)], "kv")
            kdst = kT if lat else kTc
            for g in range(2):
                for (c0, n) in ([(0, 512), (512, 256)] if lat else [(0, 256)]):
                    b = nb()
                    proj(s, g * 128, 128, c0, n, b)

                    if lat:
                        rope(b, n, c0, kdst[:, g, c0:c0 + n], lambda a: a)
                    else:
                        P.act(lambda e, b=b, g=g, c0=c0, n=n: e.copy(kdst[:, g, c0:c0 + n], ps[b][:, 0:n]),
                              reads=["ps%d" % b], writes=["qk"])
            vdst = Vt if lat else Vc
            for eb in range(next_):
                b = nb()
                for kc in range(NCH):
                    P.pe(lambda e, kc=kc, eb=eb, b=b: e.matmul(ps[b][:, 0:256], xnT(kc, eb * 128), wb[s][:, kc, 256:512],
                                                              start=(kc == 0), stop=(kc == NCH - 1)),
                         reads=["wb%d" % s, "xnT"], writes=["ps%d" % b])
                P.act(lambda e, eb=eb, b=b: e.copy(vdst[:, eb, :], ps[b][:, 0:256]), reads=["ps%d" % b], writes=["qk"])
            if kv_only:
                return
            kv_blocks = (lambda qb: [("c", 0), ("c", 1), ("w", qb), ("w", qb + 1), ("w", qb + 2)]) if lat else \
                        (lambda qb: [("c", 0), ("c", 1)])
            for qb in range(nown):
                for g in range(2):
                    blks = kv_blocks(qb)
                    pp = pslot[0] % 2
                    pslot[0] += 1
                    pTt = pT2[pp]
                    for j, (kind_b, eb) in enumerate(blks):
                        b = nb()
                        ksrc = (kTc if (kind_b == "c") else kT)
                        P.pe(lambda e: e.matmul(ps[b][:, :], ksrc[:, g, eb * 128:(eb + 1) * 128],
                                                qT[:, qb, 4 * g:4 * g + 4, :].rearrange("p a b -> p (a b)"), start=True, stop=True),
                             reads=["qk"], writes=["ps%d" % b])
                        pk = "pT%d_%d" % (pp, j)
                        if kind_b == "w" and ((eb == 0 and ti == 0) or (eb == 5 and ti == n_tiles - 1)):
                            hb = hval_t[:, 2:3] if eb == 0 else hval_t[:, 3:4]
                            P.act(lambda e: e.activation(pTt[:, j, :], ps[b][:, :], AF.Exp, bias=hb, scale=ATT_SCALE),
                                  reads=["ps%d" % b, "hval"], writes=[pk])
                        else:
                            P.act(lambda e: e.activation(pTt[:, j, :], ps[b][:, :], AF.Exp, scale=ATT_SCALE),
                                  reads=["ps%d" % b], writes=[pk])
                        if kind_b == "w" and eb - qb != 1:
                            mi = 0 if eb - qb == 0 else 1
                            P.dve(lambda e: e.tensor_tensor(pTt[:, j, :], pTt[:, j, :], m01[:, mi, :], ALU.mult),
                                  reads=[pk, "m01"], writes=[pk])
                    bd = nb()
                    bo = nb()
                    nblk = len(blks)
                    for j, (kind_b, eb) in enumerate(blks):
                        P.pe(lambda e: e.matmul(ps[bd][:, :], ones_bf[:], pTt[:, j, :], start=(j == 0), stop=False),
                             reads=["ones", "pT%d_%d" % (pp, j)], writes=["ps%d" % bd])
                    P.pe(lambda e: e.matmul(ps[bd][:, :], ones_row[0:1, :], esrow[0:1, g, :], start=False, stop=True),
                         reads=["ones", "esrow"], writes=["ps%d" % bd])
                    for j, (kind_b, eb) in enumerate(blks):
                        vsrc = Vc if kind_b == "c" else Vt
                        P.pe(lambda e: e.matmul(ps[bo][:, :], vsrc[:, eb, g * 128:(g + 1) * 128], pTt[:, j, :],
                                                start=(j == 0), stop=(j == nblk - 1)),
                             reads=["qk", "pT%d_%d" % (pp, j)], writes=["ps%d" % bo])
                    P.dve(lambda e: e.reciprocal(den2[pp][:], ps[bd][:, :]), reads=["ps%d" % bd], writes=["den0"])
                    P.dve(lambda e: e.tensor_tensor(
                        bufB[:, 4 * g:4 * g + 4, qb * 128:(qb + 1) * 128],
                        ps[bo][:, :].rearrange("p (a b) -> p a b", b=128),
                        den2[pp][:].rearrange("p (a b) -> p a b", b=128), ALU.mult),
                        reads=["ps%d" % bo, "den0"], writes=["mixT"])
            for c in range(8):
                s = wload([(w_in_v[:, :, 1536 + c * 128:1536 + (c + 1) * 128], 0, NCH, 128),
                           (w_in_v[:, :, 2560 + c * 128:2560 + (c + 1) * 128], 128, NCH, 128),
                           (w_in_v[:, :, 3584 + c * 128:3584 + (c + 1) * 128], 256, NCH, 128)], "cv%d" % c)
                bx, bb, bc = nb(), nb(), nb()
                proj(s, 0, 128, own0, NO, bx)
                proj(s, 128, 128, own0, NO, bb)
                proj(s, 256, 128, own0, NO, bc)
                P.act(lambda e, bx=bx: e.copy(xin_sb[:, 1:1 + NO], ps[bx][:, 0:NO]), reads=["ps%d" % bx], writes=["xin_sb"])
                P.dve(lambda e, bc=bc: e.tensor_tensor(p_sb[:, 1:1 + NO], ps[bc][:, 0:NO], xin_sb[:, 1:1 + NO], ALU.mult),
                      reads=["ps%d" % bc, "xin_sb"], writes=["p_sb"])
                if lat:
                    bh = nb()
                    for kc in range(NCH):
                        P.pe(lambda e, kc=kc, bh=bh, s=s: e.matmul(ps[bh][:, 0:2], wb[s][:, kc, 0:128], halo2[:, kc, :],
                                                                 start=(kc == 0), stop=(kc == NCH - 1)),
                             reads=["wb%d" % s, "halo2"], writes=["ps%d" % bh])
                    for kc in range(NCH):
                        P.pe(lambda e, kc=kc, bh=bh, s=s: e.matmul(ps[bh][:, 2:4], wb[s][:, kc, 256:384], halo2[:, kc, :],
                                                                 start=(kc == 0), stop=(kc == NCH - 1)),
                             reads=["wb%d" % s, "halo2"], writes=["ps%d" % bh])
                    P.act(lambda e, bh=bh: e.copy(t2[:, 0:2], ps[bh][:, 0:2]), reads=["ps%d" % bh], writes=["t2"])
                    P.dve(lambda e, bh=bh: e.tensor_tensor(t1[:, 0:2], ps[bh][:, 2:4], t2[:, 0:2], ALU.mult),
                          reads=["ps%d" % bh, "t2"], writes=["t1"])
                    if ti == 0:
                        P.dve(lambda e: e.tensor_tensor(p_sb[:, 0:1], t1[:, 0:1], hval_t[:, 0:1], ALU.mult),
                              reads=["t1", "hval"], writes=["p_sb"])
                    else:
                        P.dve(lambda e: e.tensor_copy(p_sb[:, 0:1], t1[:, 0:1]), reads=["t1"], writes=["p_sb"])
                    if ti == n_tiles - 1:
                        P.dve(lambda e: e.tensor_tensor(p_sb[:, 513:514], t1[:, 1:2], hval_t[:, 1:2], ALU.mult),
                              reads=["t1", "hval"], writes=["p_sb"])
                    else:
                        P.dve(lambda e: e.tensor_copy(p_sb[:, 513:514], t1[:, 1:2]), reads=["t1"], writes=["p_sb"])
                else:
                    P.dve(lambda e: e.memset(p_sb[:, 0:1], 0.0), writes=["p_sb"])
                    P.dve(lambda e: e.memset(p_sb[:, 1 + NO:2 + NO], 0.0), writes=["p_sb"])
                P.dve(lambda e, c=c: e.tensor_scalar(acc[:, 0:NO], p_sb[:, 0:NO], convw_t[:, c, 0:1], convw_t[:, c, 3:4],
                                                    ALU.mult, ALU.add), reads=["p_sb", "convw"], writes=["acc"])
                P.dve(lambda e, c=c: e.scalar_tensor_tensor(acc[:, 0:NO], p_sb[:, 1:1 + NO], convw_t[:, c, 1:2], acc[:, 0:NO],
                                                           ALU.mult, ALU.add), reads=["p_sb", "convw", "acc"], writes=["acc"])
                P.dve(lambda e, c=c: e.scalar_tensor_tensor(acc[:, 0:NO], p_sb[:, 2:2 + NO], convw_t[:, c, 2:3], acc[:, 0:NO],
                                                           ALU.mult, ALU.add), reads=["p_sb", "convw", "acc"], writes=["acc"])
                P.dve(lambda e, c=c, bb=bb: e.tensor_tensor(bufB[:, 8 + c, 0:NO], ps[bb][:, 0:NO], acc[:, 0:NO], ALU.mult),
                      reads=["ps%d" % bb, "acc"], writes=["mixT"])
            for nt in range(4):
                s = wload([(w_out_v[:, :, nt * 512:(nt + 1) * 512], 0, NCH, 512)], "wo%d" % nt)
                gs = gload(g1i, nt)
                for ob in range(nown):
                    b = nb()
                    for kc in range(NCH):
                        P.pe(lambda e, kc=kc, ob=ob, b=b, s=s: e.matmul(ps[b][:, :], bufB[:, kc, ob * 128:(ob + 1) * 128],
                                                                        wb[s][:, kc, :], start=(kc == 0), stop=(kc == NCH - 1)),
                             reads=["wb%d" % s, "mixT"], writes=["ps%d" % b])
                    P.dve(lambda e, b=b, gs=gs: e.tensor_tensor(gsb[:], ps[b][:, :], grow_s[:, gs, :], ALU.mult),
                          reads=["ps%d" % b, "grow%d" % gs], writes=["gsb"])
                    P.dve(lambda e, ob=ob, nt=nt: e.tensor_tensor(hbuf[:, ob, nt * 512:(nt + 1) * 512],
                                                                  hbuf[:, ob, nt * 512:(nt + 1) * 512], gsb[:], ALU.add),
                          reads=["gsb", "hb%d" % ob], writes=["hb%d" % ob])
            for ob in range(nown):
                norm_block(hbuf[:, ob, :], "hb%d" % ob, unT, ob * 128, gc2, sh2, "mixT")
            aT = lambda fc, col0, w: bufA[:, fc * 512 + col0: fc * 512 + col0 + w]
            for half in range(2):
                for pr in range(11):
                    f0 = (half * 22 + pr * 2) * 128
                    s = wload([(w_g_v[:, :, f0:f0 + 256], 0, NCH, 256), (w_u_v[:, :, f0:f0 + 256], 256, NCH, 256)], "gu%d_%d" % (half, pr))
                    for j in range(2):
                        bg_, bu_ = nb(), nb()
                        for kc in range(NCH):
                            P.pe(lambda e, kc=kc, j=j, s=s, b=bg_: e.matmul(ps[b][:, 0:NO], wb[s][:, kc, j * 128:(j + 1) * 128],
                                                                           bufB[:, kc, 0:NO], start=(kc == 0), stop=(kc == NCH - 1)),
                                 reads=["wb%d" % s, "mixT"], writes=["ps%d" % bg_])
                        for kc in range(NCH):
                            P.pe(lambda e, kc=kc, j=j, s=s, b=bu_: e.matmul(ps[b][:, 0:NO], wb[s][:, kc, 256 + j * 128:256 + (j + 1) * 128],
                                                                           bufB[:, kc, 0:NO], start=(kc == 0), stop=(kc == NCH - 1)),
                                 reads=["wb%d" % s, "mixT"], writes=["ps%d" % bu_])
                        P.act(lambda e, b=bg_: e.activation(gsb[:, 0:NO], ps[b][:, 0:NO], AF.Silu), reads=["ps%d" % bg_], writes=["gsb"])
                        fc = pr * 2 + j
                        P.dve(lambda e, b=bu_, fc=fc: e.tensor_tensor(aT(fc, 0, NO), ps[b][:, 0:NO], gsb[:, 0:NO], ALU.mult),
                              reads=["ps%d" % bu_, "gsb", "xnT"], writes=["xnT"])
                for nt in range(4):
                    banks = [nb() for _ in range(nown)]
                    gs = gload(g2i, nt)
                    for kh in range(2):
                        fc0 = half * 22 + kh * 11
                        s = wload([(w_d_v[:, fc0:fc0 + 11, nt * 512:(nt + 1) * 512], 0, 11, 512)], "wd%d_%d_%d" % (half, nt, kh))
                        for ob in range(nown):
                            b = banks[ob]
                            for k in range(11):
                                P.pe(lambda e, k=k, kh=kh, ob=ob, b=b, s=s: e.matmul(
                                    ps[b][:, :], aT(kh * 11 + k, ob * 128, 128), wb[s][:, k, :],
                                    start=(kh == 0 and k == 0), stop=(kh == 1 and k == 10)),
                                    reads=["wb%d" % s, "xnT"], writes=["ps%d" % b])
                    for ob in range(nown):
                        b = banks[ob]
                        P.dve(lambda e, b=b, gs=gs: e.tensor_tensor(gsb[:], ps[b][:, :], grow_s[:, gs, :], ALU.mult),
                              reads=["ps%d" % b, "grow%d" % gs], writes=["gsb"])
                        P.dve(lambda e, ob=ob, nt=nt: e.tensor_tensor(hbuf[:, ob, nt * 512:(nt + 1) * 512],
                                                                      hbuf[:, ob, nt * 512:(nt + 1) * 512], gsb[:], ALU.add),
                              reads=["gsb", "hb%d" % ob], writes=["hb%d" % ob])
            orow0 = ti * 512 if lat else 0
            for ob in range(nown):
                P.dma("sp", lambda e, ob=ob: e.dma_start(out=dst[orow0 + ob * 128: orow0 + (ob + 1) * 128, :], in_=hbuf[:, ob, :]),
                      "o_%s%d" % (kind, ob), reads=["hb%d" % ob], final=True)

        segment("ctx", 0, kv_only=True)
        for ti in range(n_tiles):
            segment("lat", ti)
        segment("ctx", 0)
        P.emit()
    return nc


D = 2048
NCH = 16
EPS = 1e-6


def build_BC(mode, T):
    nc = bass.Bass("TRN2", target_bir_lowering=False)
    dt = lambda n, s, k="ExternalInput", d=F32: nc.dram_tensor(n, s, d, kind=k).ap()
    NB = T // 128
    h1 = dt("h1", [T, D])
    halo = dt("halo", [3, D])
    modv = dt("modv", [128, NCH, 6])
    ng1 = dt("ng1", [128, NCH, 2])
    w_in = dt("w_in", [D, 2 * D])
    convw = dt("convw", [128, NCH, 5])
    ga_w = dt("ga_w", [2, 16, 128, 128])
    gx_w = dt("gx_w", [2, 16, 128, 128])
    gab = dt("gab", [128, NCH, 4])
    lam = dt("lam", [128, NCH, 2])
    hval = dt("hval", [128, 2])
    ident_d = dt("ident", [128, 128])
    if mode == "B":
        hc1 = dt("hc1", [256, D])
        stats = dt("stats", [128, NCH, 4], "ExternalOutput")
        cstate = dt("cstate", [128, NCH, 2], "ExternalOutput")
    else:
        chain = dt("chain", [128, NCH, 14])
        w_out = dt("w_out", [D, D])
        rows = dt("rows", [1, D])
        rw = dt("rw", [128, NCH, 8])
        rb = dt("rb", [1, 8])
        h2 = dt("h2", [T, D], "ExternalOutput")
        u_o = dt("uT", [D, T], "ExternalOutput")
        wts = dt("wts", [T, 8], "ExternalOutput")
        yT_d = nc.dram_tensor("yT_d", [NCH, 128, T], BF16, kind="Internal").ap()
        w_out_v = w_out.rearrange("(kc p) n -> p kc n", p=128)
    w_in_v = w_in.rearrange("(kc p) n -> p kc n", p=128)
    TE = T + 3
    tiles = [(c0, min(512, T - c0)) for c0 in range(0, T, 512)]

    with contextlib.ExitStack() as st:
        sb = lambda n, s, d=F32: st.enter_context(nc.sbuf_tensor(n, s, d))
        xnT = sb("xnT", [128, NCH, TE], BF16)
        blk = sb("blk", [128, D])
        xhat = sb("xhat", [128, D])
        xr2 = [sb("xr%d" % i, [128, max(TE, 2048)]) for i in range(2)]
        xc2 = [sb("xc%d" % i, [128, T]) for i in range(2)]
        xcb2 = [sb("xcb%d" % i, [128, T], BF16) for i in range(2)]
        xr = xr2[0]
        av = [sb("a%d" % z, [128, T]) for z in range(2)]
        bv = [sb("b%d" % z, [128, T]) for z in range(2)]
        r_t = sb("r_t", [128, 512])
        wb = [sb("wb%d" % i, [128, NCH, 256], BF16) for i in range(2)]
        gw = [sb("gw%d" % i, [128, 4, 128], BF16) for i in range(2)]
        modv_t = sb("modv_t", [128, NCH, 6])
        ng_t = sb("ng_t", [128, NCH, 2])
        gm = sb("gm", [128, NCH, 3])
        convw_t = sb("convw_t", [128, NCH, 5])
        gab_t = sb("gab_t", [128, NCH, 4])
        lam_t = sb("lam_t", [128, NCH, 2])
        negc = sb("negc", [128, NCH, 2])
        neg2c = sb("neg2c", [128, NCH, 2])
        hval_t = sb("hval_t", [128, 2])
        ident = sb("ident_t", [128, 128])
        ss = sb("ss", [128, 8])
        racc = sb("racc", [128, 2, 8])
        if mode == "B":
            stats_t = sb("stats_t", [128, NCH, 4])
            cst_t = sb("cst_t", [128, NCH, 2])
        else:
            gel = sb("gel", [128, T])
            ybf = sb("ybf", [128, T], BF16)
            chain_t = sb("chain_t", [128, NCH, 14])
            Hin = sb("Hin", [128, NCH, 2])
            rows_t = sb("rows_t", [128, D])
            rw_t = sb("rw_t", [128, NCH, 8])
            rb_t = sb("rb_t", [128, 8])
            lg = sb("lg", [128, 8, 8])
        ps = [st.enter_context(nc.psum_tensor("ps%d" % i, [128, 512], F32)) for i in range(8)]

        P = Prog(nc)
        bank_ctr = [0]

        def nb():
            b = bank_ctr[0] % 8
            bank_ctr[0] += 1
            return b

        ld = lambda dst, src, key, q="sp": P.dma(q, lambda e: e.dma_start(out=dst, in_=src), "c_" + key, writes=[key])
        ld(modv_t[:], modv, "modv")
        ld(ng_t[:], ng1, "ng")
        ld(convw_t[:], convw, "convw")
        ld(gab_t[:], gab, "gab")
        ld(lam_t[:], lam, "lam")
        ld(hval_t[:], hval, "hval")
        ld(ident[:], ident_d, "ident")
        P.act(lambda e: e.activation(negc[:], lam_t[:], AF.Exp, scale=-1.0), reads=["lam"], writes=["negc"])
        P.act(lambda e: e.activation(negc[:], negc[:], AF.Ln, bias=1.0), reads=["negc"], writes=["negc"])
        P.dve(lambda e: e.tensor_scalar(neg2c[:], negc[:], -16.0, None, ALU.mult), reads=["negc"], writes=["neg2c"])
        P.dve(lambda e: e.tensor_scalar(negc[:], negc[:], -8.0, None, ALU.mult), reads=["negc", "neg2c"], writes=["negc"])
        for j, (sci, gi) in enumerate([(1, 0), (3, 0), (5, 1)]):
            P.dve(lambda e, j=j, sci=sci: e.scalar_tensor_tensor(gm[:, :, j], modv_t[:, :, sci], 1.0, ng_t[:, :, gi], ALU.add, ALU.mult),
                  reads=["modv", "ng"], writes=["gm"])
        if mode == "C":
            ld(chain_t[:], chain, "chain")
            ld(rw_t[:], rw, "rw")
            ld(rb_t[:], rb.partition_broadcast(128), "rb")
            ld(rows_t[:], rows.partition_broadcast(128), "rows3")
            P.dve(lambda e: e.tensor_copy(Hin[:], chain_t[:, :, 0:2]), reads=["chain"], writes=["Hin"])
            for z in range(2):
                for j in range(3):
                    ca = 2 + z * 6 + 2 * j
                    P.dve(lambda e, z=z, ca=ca: e.tensor_tensor(Hin[:, :, z], Hin[:, :, z], chain_t[:, :, ca], ALU.mult),
                          reads=["Hin", "chain"], writes=["Hin"])
                    P.dve(lambda e, z=z, ca=ca: e.tensor_tensor(Hin[:, :, z], Hin[:, :, z], chain_t[:, :, ca + 1], ALU.add),
                          reads=["Hin", "chain"], writes=["Hin"])

        def norm_block(src_ap, key_src, ncols, col0, gcol, shcol):
            P.dve(lambda e: e.memset(ss[:, 0:1], 0.0), writes=["ss"])
            P.act(lambda e: e.activation(xhat[:], src_ap, AF.Square, accum_out=ss[:, 0:1]),
                  reads=[key_src, "ss"], writes=["xhat", "ss"])
            P.act(lambda e: e.activation(ss[:, 1:2], ss[:, 0:1], AF.Sqrt, bias=EPS, scale=1.0 / D), reads=["ss"], writes=["ss"])
            P.dve(lambda e: e.reciprocal(ss[:, 2:3], ss[:, 1:2]), reads=["ss"], writes=["ss"])
            P.dve(lambda e: e.tensor_scalar(xhat[:], src_ap, ss[:, 2:3], None, ALU.mult),
                  reads=[key_src, "ss", "xhat"], writes=["xhat"])
            for q4 in range(4):
                b = nb()
                for j in range(4):
                    c = q4 * 4 + j
                    P.pe(lambda e: e.transpose(ps[b][:, j * 128:(j + 1) * 128], xhat[:, c * 128:(c + 1) * 128], ident[:]),
                         reads=["xhat", "ident"], writes=["ps%d" % b])
                for j in range(4):
                    c = q4 * 4 + j
                    P.act(lambda e: e.activation(xnT[:, c, col0:col0 + ncols], ps[b][:, j * 128:j * 128 + ncols], AF.Identity,
                                                 bias=modv_t[:, c, shcol:shcol + 1], scale=gm[:, c, gcol:gcol + 1]),
                          reads=["ps%d" % b, "gm", "modv"], writes=["xnT"])

        wslot = [0]

        def mixer(src, Ts, has_halo, gcol, shcol, out_stats):
            nblk = Ts // 128
            P.dve(lambda e: e.memset(blk[:], 0.0), writes=["blk"])
            if has_halo:
                P.dma("sp", lambda e: e.dma_start(out=blk[0:3, :], in_=halo), "c_blk", writes=["blk"])
            norm_block(blk[:], "blk", 3, 0, gcol, shcol)
            P.dve(lambda e: e.tensor_copy(xnT[:, :, Ts + 2:Ts + 3], xnT[:, :, 2:3]), reads=["xnT"], writes=["xnT"])
            for ob in range(nblk):
                P.dma("sp", lambda e: e.dma_start(out=blk[:], in_=src[ob * 128:(ob + 1) * 128, :]), "c_blk", writes=["blk"])
                norm_block(blk[:], "blk", 128, 2 + ob * 128, gcol, shcol)
            tl = [(c0, min(512, Ts - c0)) for c0 in range(0, Ts, 512)]
            wbase = wslot[0]
            wslot[0] += NCH

            def front(h):
                s = (wbase + h) % 2
                xr, xc, xcb = xr2[s], xc2[s], xcb2[s]
                kxr, kxc, kxcb = "xr%d" % s, "xc%d" % s, "xcb%d" % s
                if s == 0:
                    kxr = "xr"
                r_all, i_all = blk, xhat
                P.dma("pool", lambda e: e.dma_start(out=wb[s][:, :, 0:128], in_=w_in_v[:, :, h * 128:(h + 1) * 128]),
                      "w%d" % s, writes=["wb%d" % s])
                P.dma("pool", lambda e: e.dma_start(out=wb[s][:, :, 128:256], in_=w_in_v[:, :, D + h * 128:D + (h + 1) * 128]),
                      "w%d" % s, writes=["wb%d" % s])
                P.dma("pool", lambda e: e.dma_start(out=gw[s][:, 0:2, :], in_=ga_w[:, h, :, :].rearrange("z i j -> i z j")),
                      "gw%d" % s, writes=["gw%d" % s])
                P.dma("pool", lambda e: e.dma_start(out=gw[s][:, 2:4, :], in_=gx_w[:, h, :, :].rearrange("z i j -> i z j")),
                      "gw%d" % s, writes=["gw%d" % s])
                for (c0, n) in [(0, min(512, Ts + 3))] + [(c, min(512, Ts + 3 - c)) for c in range(512, Ts + 3, 512)]:
                    b = nb()
                    for kc in range(NCH):
                        P.pe(lambda e: e.matmul(ps[b][:, 0:n], wb[s][:, kc, 128:256], xnT[:, kc, c0:c0 + n],
                                                start=(kc == 0), stop=(kc == NCH - 1)),
                             reads=["wb%d" % s, "xnT"], writes=["ps%d" % b])
                    P.act(lambda e: e.copy(xr[:, c0:c0 + n], ps[b][:, 0:n]), reads=["ps%d" % b], writes=[kxr])
                if has_halo:
                    P.dve(lambda e: e.tensor_scalar(xr[:, 0:2], xr[:, 0:2], hval_t[:, 0:1], None, ALU.mult),
                          reads=[kxr, "hval"], writes=[kxr])
                    P.dve(lambda e: e.tensor_scalar(xr[:, Ts + 2:Ts + 3], xr[:, Ts + 2:Ts + 3], hval_t[:, 1:2], None, ALU.mult),
                          reads=[kxr, "hval"], writes=[kxr])
                else:
                    P.dve(lambda e: e.memset(xr[:, 0:2], 0.0), reads=[kxr], writes=[kxr])
                    P.dve(lambda e: e.memset(xr[:, Ts + 2:Ts + 3], 0.0), reads=[kxr], writes=[kxr])
                P.dve(lambda e: e.tensor_scalar(xc[:, 0:Ts], xr[:, 0:Ts], convw_t[:, h, 0:1], convw_t[:, h, 4:5], ALU.mult, ALU.add),
                      reads=[kxr, "convw"], writes=[kxc])
                for k in range(1, 4):
                    P.dve(lambda e: e.scalar_tensor_tensor(xc[:, 0:Ts], xr[:, k:k + Ts], convw_t[:, h, k:k + 1], xc[:, 0:Ts],
                                                           ALU.mult, ALU.add), reads=[kxr, "convw", kxc], writes=[kxc])
                P.dve(lambda e: e.tensor_copy(xcb[:, 0:Ts], xc[:, 0:Ts]), reads=[kxc], writes=[kxcb])

            def back(h):
                s = (wbase + h) % 2
                xr, xc, xcb = xr2[s], xc2[s], xcb2[s]
                kxr, kxc, kxcb = "xr%d" % s, "xc%d" % s, "xcb%d" % s
                if s == 0:
                    kxr = "xr"
                r_all, i_all = blk, xhat
                if mode == "C":
                    for (c0, n) in tl:
                        b = nb()
                        for kc in range(NCH):
                            P.pe(lambda e: e.matmul(ps[b][:, 0:n], wb[s][:, kc, 0:128], xnT[:, kc, 2 + c0:2 + c0 + n],
                                                    start=(kc == 0), stop=(kc == NCH - 1)),
                                 reads=["wb%d" % s, "xnT"], writes=["ps%d" % b])
                        P.act(lambda e: e.activation(gel[:, c0:c0 + n], ps[b][:, 0:n], AF.Gelu), reads=["ps%d" % b], writes=["gel"])
                if out_stats is not None:
                    P.dve(lambda e: e.memset(racc[:], 0.0), writes=["racc"])
                for z in range(2):
                    for ti, (c0, n) in enumerate(tl):
                        br = nb()
                        P.pe(lambda e: e.matmul(ps[br][:, 0:n], gw[s][:, z, :], xcb[:, c0:c0 + n], start=True, stop=True),
                             reads=["gw%d" % s, kxcb], writes=["ps%d" % br])
                        if out_stats is not None:
                            P.act(lambda e: e.activation(r_all[:, c0:c0 + n], ps[br][:, 0:n], AF.Sigmoid, bias=gab_t[:, h, z:z + 1],
                                                         accum_out=racc[:, z, ti:ti + 1]),
                                  reads=["ps%d" % br, "gab", "racc"], writes=["blk", "racc"])
                        else:
                            P.act(lambda e: e.activation(r_all[:, c0:c0 + n], ps[br][:, 0:n], AF.Sigmoid, bias=gab_t[:, h, z:z + 1]),
                                  reads=["ps%d" % br, "gab"], writes=["blk"])
                    for ti, (c0, n) in enumerate(tl):
                        bi = nb()
                        P.pe(lambda e: e.matmul(ps[bi][:, 0:n], gw[s][:, 2 + z, :], xcb[:, c0:c0 + n], start=True, stop=True),
                             reads=["gw%d" % s, kxcb], writes=["ps%d" % bi])
                        P.act(lambda e: e.activation(i_all[:, c0:c0 + n], ps[bi][:, 0:n], AF.Sigmoid, bias=gab_t[:, h, 2 + z:3 + z]),
                              reads=["ps%d" % bi, "gab"], writes=["xhat"])
                    P.act(lambda e: e.activation(av[z][:, 0:Ts], r_all[:, 0:Ts], AF.Exp, scale=negc[:, h, z:z + 1]),
                          reads=["blk", "negc"], writes=["a%d" % z])
                    P.act(lambda e: e.activation(r_all[:, 0:Ts], r_all[:, 0:Ts], AF.Exp, scale=neg2c[:, h, z:z + 1]),
                          reads=["blk", "neg2c"], writes=["blk"])
                    P.act(lambda e: e.activation(r_all[:, 0:Ts], r_all[:, 0:Ts], AF.Sqrt, bias=1.0, scale=-1.0),
                          reads=["blk"], writes=["blk"])
                    P.dve(lambda e: e.tensor_tensor(i_all[:, 0:Ts], i_all[:, 0:Ts], r_all[:, 0:Ts], ALU.mult),
                          reads=["xhat", "blk"], writes=["xhat"])
                    P.dve(lambda e: e.tensor_tensor(bv[z][:, 0:Ts], i_all[:, 0:Ts], xc[:, 0:Ts], ALU.mult),
                          reads=["xhat", kxc], writes=["b%d" % z])
                if out_stats is None:
                    P.dve(lambda e: e.tensor_tensor_scan(bv[0][:, 0:Ts], av[0][:, 0:Ts], bv[0][:, 0:Ts], Hin[:, h, 0:1], ALU.mult, ALU.add),
                          reads=["a0", "b0", "Hin"], writes=["b0"])
                    P.dve(lambda e: e.tensor_tensor_scan(bv[1][:, Ts - 1::-1], av[1][:, Ts - 1::-1], bv[1][:, Ts - 1::-1],
                                                         Hin[:, h, 1:2], ALU.mult, ALU.add),
                          reads=["a1", "b1", "Hin"], writes=["b1"])
                    P.dve(lambda e: e.tensor_tensor(bv[0][:, 0:Ts], bv[0][:, 0:Ts], bv[1][:, 0:Ts], ALU.add),
                          reads=["b0", "b1"], writes=["b0"])
                    P.dve(lambda e: e.tensor_tensor(ybf[:, 0:Ts], bv[0][:, 0:Ts], gel[:, 0:Ts], ALU.mult),
                          reads=["b0", "gel"], writes=["ybf"])
                    P.dma("sp", lambda e: e.dma_start(out=yT_d[h], in_=ybf[:, 0:Ts]), "c_y", reads=["ybf"], writes=["yT_d"])
                else:
                    tile_, kind = out_stats
                    P.dve(lambda e: e.tensor_tensor_scan(bv[0][:, 0:Ts], av[0][:, 0:Ts], bv[0][:, 0:Ts], 0.0, ALU.mult, ALU.add),
                          reads=["a0", "b0"], writes=["b0"])
                    P.dve(lambda e: e.tensor_tensor_scan(bv[1][:, Ts - 1::-1], av[1][:, Ts - 1::-1], bv[1][:, Ts - 1::-1],
                                                         0.0, ALU.mult, ALU.add),
                          reads=["a1", "b1"], writes=["b1"])
                    if kind == "lat":
                        P.dve(lambda e: e.tensor_copy(tile_[:, h, 1:2], bv[0][:, Ts - 1:Ts]), reads=["b0"], writes=["stt"])
                        P.dve(lambda e: e.tensor_copy(tile_[:, h, 3:4], bv[1][:, 0:1]), reads=["b1"], writes=["stt"])
                        for z in range(2):
                            P.dve(lambda e: e.reduce_sum(ss[:, 4 + z:5 + z], racc[:, z, :], axis=AX.X), reads=["racc"], writes=["ss"])
                            P.act(lambda e: e.activation(tile_[:, h, 2 * z:2 * z + 1], ss[:, 4 + z:5 + z], AF.Exp, scale=negc[:, h, z:z + 1]),
                                  reads=["ss", "negc"], writes=["stt"])
                    else:
                        P.dve(lambda e: e.tensor_copy(tile_[:, h, 0:1], bv[0][:, Ts - 1:Ts]), reads=["b0"], writes=["stt"])
                        P.dve(lambda e: e.tensor_copy(tile_[:, h, 1:2], bv[1][:, 0:1]), reads=["b1"], writes=["stt"])


            front(0)
            for h in range(NCH):
                if h + 1 < NCH:
                    front(h + 1)
                back(h)

        if mode == "B":
            mixer(hc1, 256, False, 1, 2, (cst_t, "ctx"))
            mixer(h1, T, True, 0, 0, (stats_t, "lat"))
            P.dma("sp", lambda e: e.dma_start(out=stats, in_=stats_t[:]), "o_st", reads=["stt"], final=True)
            P.dma("sp", lambda e: e.dma_start(out=cstate, in_=cst_t[:]), "o_cs", reads=["stt"], final=True)
        else:
            mixer(h1, T, True, 0, 0, None)
            yT_v = yT_d.rearrange("c p t -> p c t")
            if T >= 2048:
                hosts = [(xc2[0], "xc0"), (xc2[1], "xc1"), (av[0], "a0"), (av[1], "a1"), (bv[0], "b0"), (bv[1], "b1"),
                         (gel, "gel"), (xr2[1], "xr1")]
                wres = [(t_[:, 0:2048].bitcast(BF16).rearrange("p (kc n) -> p kc n", n=256), k_) for t_, k_ in hosts]
            else:
                wres_t = [st.enter_context(nc.sbuf_tensor("wres%d" % i, [128, NCH, 256], BF16)) for i in range(8)]
                wres = [(wres_t[i][:], "wres%d" % i) for i in range(8)]
            for nt in range(8):
                P.dma("pool", lambda e: e.dma_start(out=wres[nt][0], in_=w_out_v[:, :, nt * 256:(nt + 1) * 256]),
                      "wres%d" % nt, writes=[wres[nt][1]])
            for ob in range(NB):
                P.dma("sp", lambda e: e.dma_start(out=xnT[:, :, 0:128], in_=yT_v[:, :, ob * 128:(ob + 1) * 128]), "c_yl",
                      reads=["yT_d"], writes=["xnT"])
                P.dma("sp", lambda e: e.dma_start(out=blk[:], in_=h1[ob * 128:(ob + 1) * 128, :]), "c_blk", writes=["blk"])
                for nt in range(8):
                    b = nb()
                    for kc in range(NCH):
                        P.pe(lambda e: e.matmul(ps[b][:, 0:256], xnT[:, kc, 0:128], wres[nt][0][:, kc, :], start=(kc == 0), stop=(kc == NCH - 1)),
                             reads=[wres[nt][1], "xnT"], writes=["ps%d" % b])
                    P.dve(lambda e: e.tensor_tensor(r_t[:, 0:256], ps[b][:, 0:256], rows_t[:, nt * 256:(nt + 1) * 256], ALU.mult),
                          reads=["ps%d" % b, "rows3"], writes=["r_t"])
                    P.dve(lambda e: e.tensor_tensor(blk[:, nt * 256:(nt + 1) * 256], blk[:, nt * 256:(nt + 1) * 256], r_t[:, 0:256], ALU.add),
                          reads=["r_t", "blk"], writes=["blk"])
                P.dma("sp", lambda e: e.dma_start(out=h2[ob * 128:(ob + 1) * 128, :], in_=blk[:]), "o_h2", reads=["blk"], final=True)
                P.dve(lambda e: e.memset(ss[:, 0:1], 0.0), writes=["ss"])
                P.act(lambda e: e.activation(xhat[:], blk[:], AF.Square, accum_out=ss[:, 0:1]), reads=["blk", "ss"], writes=["xhat", "ss"])
                P.act(lambda e: e.activation(ss[:, 1:2], ss[:, 0:1], AF.Sqrt, bias=EPS, scale=1.0 / D), reads=["ss"], writes=["ss"])
                P.dve(lambda e: e.reciprocal(ss[:, 2:3], ss[:, 1:2]), reads=["ss"], writes=["ss"])
                P.dve(lambda e: e.tensor_scalar(xhat[:], blk[:], ss[:, 2:3], None, ALU.mult),
                      reads=["blk", "ss", "xhat"], writes=["xhat"])
                uT = xr[:, 0:2048].rearrange("p (c t) -> p c t", t=128)
                for q4 in range(4):
                    b = nb()
                    for j in range(4):
                        c = q4 * 4 + j
                        P.pe(lambda e: e.transpose(ps[b][:, j * 128:(j + 1) * 128], xhat[:, c * 128:(c + 1) * 128], ident[:]),
                             reads=["xhat", "ident"], writes=["ps%d" % b])
                    for j in range(4):
                        c = q4 * 4 + j
                        P.act(lambda e: e.activation(uT[:, c, :], ps[b][:, j * 128:(j + 1) * 128], AF.Identity,
                                                     bias=modv_t[:, c, 4:5], scale=gm[:, c, 2:3]),
                              reads=["ps%d" % b, "gm", "modv"], writes=["xr"])
                P.dma("sp", lambda e: e.dma_start(out=u_o.rearrange("(c p) t -> p c t", p=128)[:, :, ob * 128:(ob + 1) * 128], in_=uT),
                      "o_u", reads=["xr"], final=True)
                if True:
                    pass
                b = nb()
                for kc in range(NCH):
                    P.pe(lambda e: e.matmul(ps[b][:, 0:8], uT[:, kc, :], rw_t[:, kc, :], start=(kc == 0), stop=(kc == NCH - 1)),
                         reads=["xr", "rw"], writes=["ps%d" % b])
                L, m1, k1, L2, m2, k2, e2, w1 = [lg[:, i, :] for i in range(8)]
                P.dve(lambda e: e.tensor_tensor(L, ps[b][:, 0:8], rb_t[:], ALU.add), reads=["ps%d" % b, "rb"], writes=["lg"])
                P.dve(lambda e: e.reduce_max(m1[:, 0:1], L, axis=AX.X), reads=["lg"], writes=["lg"])
                P.dve(lambda e: e.tensor_scalar(k1, L, m1[:, 0:1], None, ALU.is_equal), reads=["lg"], writes=["lg"])
                P.dve(lambda e: e.scalar_tensor_tensor(L2, k1, -1e30, L, ALU.mult, ALU.add), reads=["lg"], writes=["lg"])
                P.dve(lambda e: e.reduce_max(m2[:, 0:1], L2, axis=AX.X), reads=["lg"], writes=["lg"])
                P.dve(lambda e: e.tensor_scalar(k2, L2, m2[:, 0:1], None, ALU.is_equal), reads=["lg"], writes=["lg"])
                P.dve(lambda e: e.tensor_tensor(e2[:, 0:1], m2[:, 0:1], m1[:, 0:1], ALU.subtract), reads=["lg"], writes=["lg"])
                P.act(lambda e: e.activation(e2[:, 0:1], e2[:, 0:1], AF.Exp), reads=["lg"], writes=["lg"])
                P.dve(lambda e: e.tensor_scalar(w1[:, 0:1], e2[:, 0:1], 1.0, None, ALU.add), reads=["lg"], writes=["lg"])
                P.dve(lambda e: e.reciprocal(w1[:, 0:1], w1[:, 0:1]), reads=["lg"], writes=["lg"])
                P.dve(lambda e: e.tensor_tensor(w1[:, 1:2], e2[:, 0:1], w1[:, 0:1], ALU.mult), reads=["lg"], writes=["lg"])
                P.dve(lambda e: e.tensor_scalar(k1, k1, w1[:, 0:1], None, ALU.mult), reads=["lg"], writes=["lg"])
                P.dve(lambda e: e.scalar_tensor_tensor(k1, k2, w1[:, 1:2], k1, ALU.mult, ALU.add), reads=["lg"], writes=["lg"])
                P.dma("sp", lambda e: e.dma_start(out=wts[ob * 128:(ob + 1) * 128, :], in_=k1), "o_w", reads=["lg"], final=True)
        P.emit()
    return nc


D = 2048
NCH = 16
EPS = 1e-6


def build_M():
    nc = bass.Bass("TRN2", target_bir_lowering=False)
    dt = lambda n, s, k="ExternalInput", d=F32: nc.dram_tensor(n, s, d, kind=k).ap()
    wm = dt("wm", [2, D, 1536])
    bm = dt("bm", [128, 2, 12])
    cv = dt("cv", [128, NCH, 3])
    out = dt("mout", [128, 2, 12, 3], "ExternalOutput")
    with contextlib.ExitStack() as st:
        sb = lambda n, s, d=F32: st.enter_context(nc.sbuf_tensor(n, s, d))
        w_t = sb("w_t", [128, NCH, 1536])
        bm_t = sb("bm_t", [128, 2, 12])
        cv_t = sb("cv_t", [128, NCH, 3])
        o_t = sb("o_t", [128, 2, 12, 3])
        ps = [st.enter_context(nc.psum_tensor("ps%d" % i, [128, 512], F32)) for i in range(2)]
        P = Prog(nc)
        P.dma("sp", lambda e: e.dma_start(out=bm_t[:], in_=bm), "c_bm", writes=["bm"])
        P.dma("sp", lambda e: e.dma_start(out=cv_t[:], in_=cv), "c_cv", writes=["cv"])
        P.act(lambda e: e.activation(cv_t[:], cv_t[:], AF.Silu), reads=["cv"], writes=["cv"])
        for l in range(2):
            for half in range(2):
                P.dma("sp" if half == 0 else "act", lambda e: e.dma_start(out=w_t[:, :, half * 768:(half + 1) * 768],
                                                  in_=wm[l].rearrange("(kc p) n -> p kc n", p=128)[:, :, half * 768:(half + 1) * 768]),
                      "c_w%d" % half, writes=["w%d" % half])
            for j in range(12):
                b = j % 2
                for kc in range(NCH):
                    P.pe(lambda e: e.matmul(ps[b][:, 0:3], w_t[:, kc, j * 128:(j + 1) * 128], cv_t[:, kc, :],
                                            start=(kc == 0), stop=(kc == NCH - 1)), reads=["w%d" % (j // 6), "cv"], writes=["ps%d" % b])
                P.dve(lambda e: e.tensor_scalar(o_t[:, l, j, :], ps[b][:, 0:3], bm_t[:, l, j:j + 1], None, ALU.add),
                      reads=["ps%d" % b, "bm"], writes=["o"])
        P.dma("sp", lambda e: e.dma_start(out=out, in_=o_t[:]), "o_o", reads=["o"], final=True)
        P.emit()
    return nc


def build_D(R, DFF=7168):
    NFC = DFF // 128
    nc = bass.Bass("TRN2", target_bir_lowering=False)
    dt = lambda n, s, k="ExternalInput", d=F32: nc.dram_tensor(n, s, d, kind=k).ap()
    uT = dt("uT", [D, R])
    wsel = dt("wsel", [128, R // 128])
    w_g = dt("w_g", [D, DFF])
    w_u = dt("w_u", [D, DFF])
    w_d = dt("w_d", [DFF, D])
    y = dt("y", [R, D], "ExternalOutput")
    uT_v = uT.rearrange("(kc p) r -> p kc r", p=128)
    w_g_v = w_g.rearrange("(kc p) n -> p kc n", p=128)
    w_u_v = w_u.rearrange("(kc p) n -> p kc n", p=128)
    w_d_v = w_d.rearrange("(fc p) n -> p fc n", p=128)
    KQ = 14
    ST = 1024
    n_super = (R + ST - 1) // ST
    with contextlib.ExitStack() as st:
        sb = lambda n, s, d=F32: st.enter_context(nc.sbuf_tensor(n, s, d))
        un = sb("un", [128, NCH, ST], BF16)
        aT = sb("aT", [128, NFC, ST], BF16)
        wb = [sb("wb%d" % i, [128, NCH, 512], BF16) for i in range(3)]
        gsb = sb("gsb", [128, 512])
        ot = [sb("ot%d" % i, [128, 512]) for i in range(4)]
        ws_t = sb("ws_t", [128, 8])
        ps = [st.enter_context(nc.psum_tensor("ps%d" % i, [128, 512], F32)) for i in range(8)]
        P = Prog(nc)
        bank_ctr = [0]

        def nb():
            b = bank_ctr[0] % 8
            bank_ctr[0] += 1
            return b
        wslot = [0]
        oslot = [0]
        wcache = {}
        hwq = [0]

        def wload(srcs, gid):
            s = wslot[0] % 3
            wslot[0] += 1
            kcn = srcs[0][2]
            ncols = max(c0 + n for _, c0, _, n in srcs)
            if gid not in wcache:
                for src, c0, kcn_, n in srcs:
                    P.dma("pool", lambda e: e.dma_start(out=wb[s][:, 0:kcn_, c0:c0 + n], in_=src), "w%d" % s, writes=["wb%d" % s])
                if n_super > 1:
                    scr = nc.dram_tensor("scr_" + gid, [128, NCH, 512], BF16, kind="Internal").ap()
                    wcache[gid] = scr
                    P.dma("sp", lambda e: e.dma_start(out=scr[:, 0:kcn, 0:ncols], in_=wb[s][:, 0:kcn, 0:ncols]), "wst%d" % s,
                          reads=["wb%d" % s], writes=["scr_" + gid])
            else:
                scr = wcache[gid]
                q = "sp" if hwq[0] % 2 == 0 else "pool"
                hwq[0] += 1
                P.dma(q, lambda e: e.dma_start(out=wb[s][:, 0:kcn, 0:ncols], in_=scr[:, 0:kcn, 0:ncols]), "w%d%s" % (s, q),
                      reads=["scr_" + gid], writes=["wb%d" % s])
            return s
        for rt in range(n_super):
            r0 = rt * ST
            NR = min(ST, R - r0)
            NOB = NR // 128
            halves = [(c0, min(512, NR - c0)) for c0 in range(0, NR, 512)]
            P.dma("pool", lambda e: e.dma_start(out=un[:, :, 0:NR], in_=uT_v[:, :, r0:r0 + NR]), "c_un", writes=["un"])
            P.dma("sp", lambda e: e.dma_start(out=ws_t[:, 0:NOB], in_=wsel[:, rt * 8:rt * 8 + NOB]), "c_ws", writes=["ws"])
            for pr in range(NFC // 2):
                f0 = pr * 256
                s = wload([(w_g_v[:, :, f0:f0 + 256], 0, NCH, 256), (w_u_v[:, :, f0:f0 + 256], 256, NCH, 256)], "gu%d" % pr)
                for j in range(2):
                    for (c0, n) in halves:
                        bg_, bu_ = nb(), nb()
                        for kc in range(NCH):
                            P.pe(lambda e: e.matmul(ps[bg_][:, 0:n], wb[s][:, kc, j * 128:(j + 1) * 128], un[:, kc, c0:c0 + n],
                                                    start=(kc == 0), stop=(kc == NCH - 1)), reads=["wb%d" % s, "un"], writes=["ps%d" % bg_])
                        for kc in range(NCH):
                            P.pe(lambda e: e.matmul(ps[bu_][:, 0:n], wb[s][:, kc, 256 + j * 128:256 + (j + 1) * 128], un[:, kc, c0:c0 + n],
                                                    start=(kc == 0), stop=(kc == NCH - 1)), reads=["wb%d" % s, "un"], writes=["ps%d" % bu_])
                        P.act(lambda e: e.activation(gsb[:, 0:n], ps[bg_][:, 0:n], AF.Silu), reads=["ps%d" % bg_], writes=["gsb"])
                        P.dve(lambda e: e.tensor_tensor(aT[:, pr * 2 + j, c0:c0 + n], ps[bu_][:, 0:n], gsb[:, 0:n], ALU.mult),
                              reads=["ps%d" % bu_, "gsb"], writes=["aT"])
            for nt in range(4):
                banks = [nb() for _ in range(NOB)]
                for kq in range(NFC // KQ):
                    s = wload([(w_d_v[:, kq * KQ:(kq + 1) * KQ, nt * 512:(nt + 1) * 512], 0, KQ, 512)], "wd%d_%d" % (nt, kq))
                    for ob in range(NOB):
                        b = banks[ob]
                        for k in range(KQ):
                            P.pe(lambda e: e.matmul(ps[b][:, :], aT[:, kq * KQ + k, ob * 128:(ob + 1) * 128], wb[s][:, k, :],
                                                    start=(kq == 0 and k == 0), stop=(kq == NFC // KQ - 1 and k == KQ - 1)),
                                 reads=["wb%d" % s, "aT"], writes=["ps%d" % b])
                for ob in range(NOB):
                    b = banks[ob]
                    os_ = oslot[0] % 4
                    oslot[0] += 1
                    P.dve(lambda e: e.tensor_scalar(ot[os_][:], ps[b][:, :], ws_t[:, ob:ob + 1], None, ALU.mult),
                          reads=["ps%d" % b, "ws"], writes=["ot%d" % os_])
                    P.dma("sp", lambda e: e.dma_start(out=y[r0 + ob * 128:r0 + (ob + 1) * 128, nt * 512:(nt + 1) * 512], in_=ot[os_][:]),
                          "o_y%d" % os_, reads=["ot%d" % os_], final=True)
        P.emit()
    return nc


def build_E(T):
    nc = bass.Bass("TRN2", target_bir_lowering=False)
    dt = lambda n, s, k="ExternalInput", d=F32: nc.dram_tensor(n, s, d, kind=k).ap()
    h2 = dt("h2", [T, D])
    yp = dt("yp", [T, 2, D])
    rows = dt("rows", [2, D])
    out = dt("out", [T, D], "ExternalOutput")
    with contextlib.ExitStack() as st:
        sb = lambda n, s, d=F32: st.enter_context(nc.sbuf_tensor(n, s, d))
        hb = [sb("hb%d" % i, [128, D]) for i in range(2)]
        yb = [sb("yb%d" % i, [128, 2, D]) for i in range(2)]
        junk = sb("junk", [128, D])
        rows_t = sb("rows_t", [128, 2, D])
        ss = sb("ss", [128, 2, 4])
        P = Prog(nc)
        for i in range(2):
            P.dma("sp", lambda e: e.dma_start(out=rows_t[:, i, :], in_=rows[i:i + 1, :].partition_broadcast(128)), "c_r%d" % i, writes=["rows"])
        for ob in range(T // 128):
            s = ob % 2
            H, Y, S_ = hb[s], yb[s], ss[:, s, :]
            P.dma("sp", lambda e: e.dma_start(out=H[:], in_=h2[ob * 128:(ob + 1) * 128, :]), "c_h%d" % s, writes=["h%d" % s])
            P.dma("act", lambda e: e.dma_start(out=Y[:], in_=yp[ob * 128:(ob + 1) * 128, :, :]), "c_y%d" % s, writes=["y%d" % s])
            P.dve(lambda e: e.tensor_tensor(Y[:, 0, :], Y[:, 0, :], Y[:, 1, :], ALU.add), reads=["y%d" % s], writes=["y%d" % s])
            P.dve(lambda e: e.tensor_tensor(Y[:, 0, :], Y[:, 0, :], rows_t[:, 0, :], ALU.mult), reads=["y%d" % s, "rows"], writes=["y%d" % s])
            P.dve(lambda e: e.tensor_tensor(H[:], H[:], Y[:, 0, :], ALU.add), reads=["y%d" % s, "h%d" % s], writes=["h%d" % s])
            P.dve(lambda e: e.memset(S_[:, 0:1], 0.0), writes=["ss%d" % s])
            P.act(lambda e: e.activation(junk[:], H[:], AF.Square, accum_out=S_[:, 0:1]), reads=["h%d" % s, "ss%d" % s], writes=["junk", "ss%d" % s])
            P.act(lambda e: e.activation(S_[:, 1:2], S_[:, 0:1], AF.Sqrt, bias=EPS, scale=1.0 / D), reads=["ss%d" % s], writes=["ss%d" % s])
            P.dve(lambda e: e.reciprocal(S_[:, 2:3], S_[:, 1:2]), reads=["ss%d" % s], writes=["ss%d" % s])
            P.dve(lambda e: e.scalar_tensor_tensor(H[:], H[:], S_[:, 2:3], rows_t[:, 1, :], ALU.mult, ALU.mult),
                  reads=["h%d" % s, "ss%d" % s, "rows"], writes=["h%d" % s])
            P.dma("sp", lambda e: e.dma_start(out=out[ob * 128:(ob + 1) * 128, :], in_=H[:]), "o_%d" % s, reads=["h%d" % s], final=True)
        P.emit()
    return nc

import numpy as np

GRID_W = 64


def fm(v):
    return np.ascontiguousarray(np.asarray(v, np.float32).reshape(16, 128).T)


def rope_tables(pos):
    pos = np.asarray(pos, np.int64)
    row = (pos // GRID_W).astype(np.float32)
    col = (pos % GRID_W).astype(np.float32)
    inv = (10000.0 ** (-np.arange(32, dtype=np.float32) / 32)).astype(np.float32)
    ar = row[:, None] * inv
    ac = col[:, None] * inv
    ang = np.concatenate([ar, ar, ac, ac], axis=-1)
    sgn = np.ones(128, np.float32)
    sgn[0:32] = -1
    sgn[64:96] = -1
    return (np.ascontiguousarray(np.cos(ang).T.astype(np.float32)),
            np.ascontiguousarray((np.sin(ang) * sgn).T.astype(np.float32)))


def consts_A():
    cst = np.zeros((128, 3, 128), np.float32)
    cst[:, 0, :] = np.eye(128, dtype=np.float32)
    perm = np.arange(128)
    perm[0:32] += 32
    perm[32:64] -= 32
    perm[64:96] += 32
    perm[96:128] -= 32
    for m in range(128):
        cst[perm[m], 1, m] = 1.0
    j = np.arange(128)[:, None]
    i = np.arange(128)[None, :]
    bm = np.zeros((128, 3, 128), np.float32)
    for wi in range(3):
        jj = wi * 128 + j
        valid = (jj >= i) & (jj <= i + 256)
        bm[:, wi, :] = np.where(valid, 1.0, 0.0)
    return cst, bm


def prep_A(x_b, ctx_b, mod0, core_in_seq, n_cores_seq, n_tiles, p):
    T = 512 * n_tiles
    S = x_b.shape[0]
    lo = core_in_seq * T - 128
    xe = np.zeros((T + 256, 2048), np.float32)
    a, b = max(lo, 0), min(lo + T + 256, S)
    xe[a - lo:b - lo] = x_b[a:b]
    pos = np.arange(lo, lo + T + 256)
    cosT, sinT = rope_tables(np.clip(pos, 0, S - 1))
    vl = 1.0 if core_in_seq > 0 else 0.0
    vr = 1.0 if core_in_seq < n_cores_seq - 1 else 0.0
    hval = np.zeros((128, 4), np.float32)
    hval[:, 0] = vl
    hval[:, 1] = vr
    hval[:, 2] = 0.0 if vl else -1e30
    hval[:, 3] = 0.0 if vr else -1e30
    modv = np.stack([fm(mod0["sh1_l"]), fm(mod0["sc1_l"]), fm(mod0["sh1_c"]), fm(mod0["sc1_c"]),
                     fm(mod0["sh2_l"]), fm(mod0["sc2_l"]), fm(mod0["sh2_c"]), fm(mod0["sc2_c"])], axis=-1)
    grow = np.stack([mod0["g1_l"], mod0["g2_l"], mod0["g1_c"], mod0["g2_c"]]).astype(np.float32)
    ng = np.stack([fm(p["norm1_g"]), fm(p["norm2_g"])], axis=-1)
    cw = np.zeros((128, 8, 4), np.float32)
    for k in range(3):
        cw[:, :, k] = p["conv_w"][k].reshape(8, 128).T
    cw[:, :, 3] = p["conv_b"].reshape(8, 128).T
    cst, bm = consts_A()
    return {"xe": xe, "ctx": np.ascontiguousarray(ctx_b, dtype=np.float32), "modv": np.ascontiguousarray(modv),
            "grow": np.ascontiguousarray(grow), "ng": np.ascontiguousarray(ng),
            "w_in": p["w_in"], "w_out": p["w_out"], "w_g": p["ffn_w_gate"], "w_u": p["ffn_w_up"], "w_d": p["ffn_w_down"],
            "convw": cw, "sinks": p["sinks"].reshape(1, 8).astype(np.float32), "cosT": cosT, "sinT": sinT,
            "hval": hval, "cst": cst, "bmask": bm}

import numpy as np

def fm2(a):
    return np.ascontiguousarray(np.stack([fm(r) for r in a], axis=-1))

def prep_BC_common(h1_own, halo3, vl, vr, mod1, p):
    hval = np.zeros((128, 2), np.float32); hval[:, 0] = vl; hval[:, 1] = vr
    cw = np.concatenate([p["conv_w"], p["conv_b"][None]], axis=0)
    return {"h1": np.ascontiguousarray(h1_own, dtype=np.float32), "halo": np.ascontiguousarray(halo3, dtype=np.float32),
            "modv": fm2([mod1["sh1_l"], mod1["sc1_l"], mod1["sh1_c"], mod1["sc1_c"], mod1["sh2_l"], mod1["sc2_l"]]),
            "ng1": fm2([p["norm1_g"], p["norm2_g"]]), "w_in": p["w_in"], "convw": fm2(cw),
            "ga_w": p["gate_a_w"], "gx_w": p["gate_x_w"],
            "gab": fm2([p["gate_a_b"][0], p["gate_a_b"][1], p["gate_x_b"][0], p["gate_x_b"][1]]),
            "lam": fm2([p["lambda"][0], p["lambda"][1]]), "hval": hval, "ident": np.eye(128, dtype=np.float32)}

def prep_C_extra(chain14, mod1, p):
    rw = np.ascontiguousarray(p["router_w"].reshape(16, 128, 8).transpose(1, 0, 2)).astype(np.float32)
    return {"chain": np.ascontiguousarray(chain14, dtype=np.float32), "w_out": p["w_out"],
            "rows": mod1["g1_l"].reshape(1, 2048).astype(np.float32), "rw": rw,
            "rb": p["router_b"].reshape(1, 8).astype(np.float32)}


N_CORES = 8
T_CORE = 2048
SEQ = 8192


def _run(nc, in_maps):
    res = run_bass_kernel_spmd(nc, in_maps, core_ids=list(range(len(in_maps))))
    return res.results


def _unfm(t):
    return np.ascontiguousarray(t.transpose(1, 0)).reshape(-1)


def kernel(**inp):
    f32 = lambda a: np.ascontiguousarray(np.asarray(a), dtype=np.float32)
    x, c, ctx, c_ctx = f32(inp["x"]), f32(inp["c"]), f32(inp["ctx"]), f32(inp["c_ctx"])
    p0 = {k[3:]: f32(v) for k, v in inp.items() if k.startswith("l0_")}
    p1 = {k[3:]: f32(v) for k, v in inp.items() if k.startswith("l1_")}
    fng = f32(inp["final_norm_g"])

    cvs = [c[0], c[1], c_ctx]
    cv = np.ascontiguousarray(np.stack([fm(v) for v in cvs], axis=-1))
    maps = []
    for j in range(N_CORES):
        sl = slice(j * 1536, (j + 1) * 1536)
        wm = np.ascontiguousarray(np.stack([p0["w_mod"][:, sl], p1["w_mod"][:, sl]]))
        bm = np.ascontiguousarray(np.stack([p0["b_mod"][sl].reshape(12, 128).T, p1["b_mod"][sl].reshape(12, 128).T], axis=1))
        maps.append({"wm": wm, "bm": bm, "cv": cv})
    rM = _run(build_M(), maps)
    mod = np.zeros((2, 3, 12288), np.float32)
    for j in range(N_CORES):
        mo = rM[j]["mout"]
        for l in range(2):
            for v in range(3):
                mod[l, v, j * 1536:(j + 1) * 1536] = mo[:, l, :, v].T.reshape(-1)
    names = ["sh1", "sc1", "g1", "sh2", "sc2", "g2"]

    def modd(l, b):
        d = {}
        for i, n in enumerate(names):
            d[n + "_l"] = mod[l, b, i * 2048:(i + 1) * 2048]
            d[n + "_c"] = mod[l, 2, i * 2048:(i + 1) * 2048]
        return d

    maps = [prep_A(x[cid // 4], ctx[cid // 4], modd(0, cid // 4), cid % 4, 4, 4, p0) for cid in range(N_CORES)]
    rA = _run(build_A(4), maps)
    h1 = [rA[cid]["h1"] for cid in range(N_CORES)]
    hc1 = [rA[(cid // 4) * 4]["hc1"] for cid in range(N_CORES)]
    del maps

    commons = []
    for cid in range(N_CORES):
        k = cid % 4
        halo = np.zeros((3, 2048), np.float32)
        if k > 0:
            halo[0:2] = h1[cid - 1][-2:]
        if k < 3:
            halo[2] = h1[cid + 1][0]
        commons.append(prep_BC_common(h1[cid], halo, 1.0 if k > 0 else 0.0, 1.0 if k < 3 else 0.0, modd(1, cid // 4), p1))
    rB = _run(build_BC("B", T_CORE), [dict(commons[cid], hc1=hc1[cid]) for cid in range(N_CORES)])

    maps = []
    for cid in range(N_CORES):
        b, k = cid // 4, cid % 4
        chain = np.zeros((128, 16, 14), np.float32)
        chain[:, :, 0:2] = rB[cid]["cstate"]
        fwd = [None] * (3 - k) + [b * 4 + cc for cc in range(0, k)]
        bwd = [None] * k + [b * 4 + cc for cc in range(3, k, -1)]
        for j in range(3):
            for z, lst in enumerate((fwd, bwd)):
                ca = 2 + z * 6 + 2 * j
                if lst[j] is None:
                    chain[:, :, ca] = 1.0
                else:
                    st_ = rB[lst[j]]["stats"]
                    chain[:, :, ca] = st_[:, :, 2 * z]
                    chain[:, :, ca + 1] = st_[:, :, 2 * z + 1]
        maps.append(dict(commons[cid], **prep_C_extra(chain, modd(1, b), p1)))
    rC = _run(build_BC("C", T_CORE), maps)
    del maps, commons
    h2 = [rC[cid]["h2"] for cid in range(N_CORES)]
    uT_all = np.concatenate([rC[cid]["uT"] for cid in range(N_CORES)], axis=1)
    wts_all = np.concatenate([rC[cid]["wts"] for cid in range(N_CORES)], axis=0)
    del rC

    idx = [np.nonzero(wts_all[:, e] > 0)[0] for e in range(8)]
    R = max(512, int(-(-max(len(i) for i in idx) // 256) * 256))
    maps = []
    for e in range(8):
        n = len(idx[e])
        us = np.zeros((2048, R), np.float32)
        us[:, :n] = uT_all[:, idx[e]]
        w = np.zeros((R,), np.float32)
        w[:n] = wts_all[idx[e], e]
        maps.append({"uT": us, "wsel": np.ascontiguousarray(w.reshape(-1, 128).T),
                     "w_g": p1["moe_w_gate"][e], "w_u": p1["moe_w_up"][e], "w_d": p1["moe_w_down"][e]})
    rD = _run(build_D(R), maps)
    del maps, uT_all
    n_tok = wts_all.shape[0]
    yp = np.zeros((n_tok, 2, 2048), np.float32)
    slot = np.zeros((n_tok,), np.int64)
    for e in range(8):
        n = len(idx[e])
        ok = slot[idx[e]] < 2
        ii = idx[e][ok]
        yp[ii, slot[ii]] = rD[e]["y"][:n][ok]
        slot[ii] += 1
    del rD

    maps = []
    for cid in range(N_CORES):
        b = cid // 4
        rows = np.ascontiguousarray(np.stack([mod[1, b, 5 * 2048:6 * 2048], fng]))
        maps.append({"h2": h2[cid], "yp": np.ascontiguousarray(yp[cid * T_CORE:(cid + 1) * T_CORE]), "rows": rows})
    rE = _run(build_E(T_CORE), maps)
    out = np.concatenate([rE[cid]["out"] for cid in range(N_CORES)], axis=0).reshape(2, SEQ, 2048)
    return np.ascontiguousarray(out, dtype=np.float32)
```

```python
import contextlib
import numpy as np
import concourse.bass as bass
import concourse.mybir as mybir
from concourse.bass_utils import run_bass_kernel_spmd

F32 = mybir.dt.float32
BF16 = mybir.dt.bfloat16
I32 = mybir.dt.int32
AF = mybir.ActivationFunctionType
ALU = mybir.AluOpType
AX = mybir.AxisListType

SAME_ENGINE_SYNC = True
COMPUTE = ("pe", "act", "dve", "pool")


class Op:
    __slots__ = ("eng", "fn", "deps", "dma", "chan", "sig", "val", "idx")

    def __init__(self, eng, fn, dma, chan):
        self.eng = eng
        self.fn = fn
        self.dma = dma
        self.chan = chan
        self.deps = []
        self.sig = False
        self.val = 0
        self.idx = 0


class _Rec:
    def __init__(self):
        self.call = None

    def __getattr__(self, name):
        def f(*a, **k):
            self.call = (name, a, k)
            return self
        return f


class Prog:
    def __init__(self, nc, same_engine_sync=SAME_ENGINE_SYNC):
        self.nc = nc
        self.ops = []
        self.last_w = {}
        self.readers = {}
        self.same = same_engine_sync
        self.final_chans = set()

    def op(self, eng, fn, reads=(), writes=(), dma=False, chan=None, final=False):
        rec = _Rec()
        fn(rec)
        o = Op(eng, rec.call, dma, chan)
        o.idx = len(self.ops)
        deps = set()
        for r in reads:
            w = self.last_w.get(r)
            if w is not None:
                deps.add(w)
        for wk in writes:
            w = self.last_w.get(wk)
            if w is not None:
                deps.add(w)
            for rd in self.readers.get(wk, ()):
                deps.add(rd)
        deps.discard(o.idx)
        o.deps = sorted(deps)
        for r in reads:
            self.readers.setdefault(r, []).append(o.idx)
        for wk in writes:
            self.last_w[wk] = o.idx
            self.readers[wk] = []
        self.ops.append(o)
        if final:
            assert dma
            self.final_chans.add(chan)
        return o

    def pe(self, fn, reads=(), writes=()):
        return self.op("pe", fn, reads, writes)

    def act(self, fn, reads=(), writes=()):
        return self.op("act", fn, reads, writes)

    def dve(self, fn, reads=(), writes=()):
        return self.op("dve", fn, reads, writes)

    def pool(self, fn, reads=(), writes=()):
        return self.op("pool", fn, reads, writes)

    def dma(self, q, fn, chan, reads=(), writes=(), final=False):
        return self.op(q, fn, reads, writes, dma=True, chan=chan, final=final)

    def emit(self):
        nc = self.nc
        ops = self.ops
        for o in ops:
            for d in o.deps:
                a = ops[d]
                if a.dma:
                    continue
                if a.eng == o.eng and not o.dma:
                    if a.eng == "pe" or not self.same:
                        continue
                a.sig = True
        cnt = {e: 0 for e in COMPUTE + ("sp",)}
        chan_cnt = {}
        for o in ops:
            if o.dma:
                chan_cnt[o.chan] = chan_cnt.get(o.chan, 0) + 16
                o.val = chan_cnt[o.chan]
            elif o.sig:
                cnt[o.eng] += 1
                o.val = cnt[o.eng]
        chans = sorted(chan_cnt)
        engs = ("pe", "act", "dve", "pool", "sp")
        with contextlib.ExitStack() as st:
            sems = {}
            for e in COMPUTE:
                sems[e] = st.enter_context(nc.semaphore("s_" + e))
            for c in chans:
                sems["c:" + c] = st.enter_context(nc.semaphore("c_" + c))
            block = st.enter_context(nc.Block())
            handles = {"pe": nc.tensor, "act": nc.scalar, "dve": nc.vector,
                       "pool": nc.gpsimd, "sp": nc.sync}
            final = [(c, chan_cnt[c]) for c in sorted(self.final_chans)]

            def make(ename):
                def body(eng):
                    waited = {}
                    for o in ops:
                        if o.eng != ename:
                            continue
                        for d in o.deps:
                            a = ops[d]
                            if a.dma:
                                key, v = "c:" + a.chan, a.val
                            else:
                                if a.eng == ename and not o.dma:
                                    if ename == "pe" or not self.same:
                                        continue
                                key, v = a.eng, a.val
                            if waited.get(key, 0) >= v:
                                continue
                            waited[key] = v
                            eng.wait_ge(sems[key], v)
                        nm, a, k = o.fn
                        ins = getattr(eng, nm)(*a, **k)
                        if o.dma:
                            ins.then_inc(sems["c:" + o.chan], 16)
                        elif o.sig:
                            ins.then_inc(sems[o.eng], 1)
                    if ename == "sp":
                        for c, v in final:
                            eng.wait_ge(sems["c:" + c], v)
                return body

            used = set(o.eng for o in ops) | {"sp"}
            for e in engs:
                if e in used:
                    getattr(block, {"pe": "tensor", "act": "scalar", "dve": "vector",
                                    "pool": "gpsimd", "sp": "sync"}[e])(make(e))


D = 2048
NCH = 16
DFF = 5632
NFC = 44
EPS = 1e-6
ATT_SCALE = 128 ** -0.5


def build_A(n_tiles):
    T_OWN = 512 * n_tiles
    T_EXT = T_OWN + 256
    nc = bass.Bass("TRN2", target_bir_lowering=False)
    dt = lambda n, s, k="ExternalInput", d=F32: nc.dram_tensor(n, s, d, kind=k).ap()
    xe = dt("xe", [T_EXT, D])
    ctx = dt("ctx", [256, D])
    modv = dt("modv", [128, NCH, 8])
    grow = dt("grow", [4, D])
    ng = dt("ng", [128, NCH, 2])
    w_in = dt("w_in", [D, 4608])
    w_out = dt("w_out", [D, D])
    w_g = dt("w_g", [D, DFF])
    w_u = dt("w_u", [D, DFF])
    w_d = dt("w_d", [DFF, D])
    convw = dt("convw", [128, 8, 4])
    sinks = dt("sinks", [1, 8])
    cosT = dt("cosT", [128, T_EXT])
    sinT = dt("sinT", [128, T_EXT])
    hval = dt("hval", [128, 4])
    cst = dt("cst", [128, 3, 128])
    bmask = dt("bmask", [128, 3, 128])
    h1 = dt("h1", [T_OWN, D], "ExternalOutput")
    hc1 = dt("hc1", [256, D], "ExternalOutput")

    w_in_v = w_in.rearrange("(kc p) n -> p kc n", p=128)
    w_out_v = w_out.rearrange("(kc p) n -> p kc n", p=128)
    w_g_v = w_g.rearrange("(kc p) n -> p kc n", p=128)
    w_u_v = w_u.rearrange("(kc p) n -> p kc n", p=128)
    w_d_v = w_d.rearrange("(fc p) n -> p fc n", p=128)

    with contextlib.ExitStack() as st:
        sb = lambda n, s, d=F32: st.enter_context(nc.sbuf_tensor(n, s, d))
        hbuf = sb("hbuf", [128, 4, D])
        stg = sb("stg", [128, 1, D])
        xhat2 = [sb("xhat%d" % i, [128, D]) for i in range(2)]
        nslot = [0]
        bufA = sb("bufA", [128, NCH * 768], BF16)
        bufB = sb("bufB", [128, NCH, 512], BF16)
        qT = sb("qT", [128, 4, 8, 128], BF16)
        kT = sb("kT", [128, 2, 768], BF16)
        Vt = sb("Vt", [128, 6, 256], BF16)
        kTc = sb("kTc", [128, 2, 256], BF16)
        Vc = sb("Vc", [128, 2, 256], BF16)
        cos_t = sb("cos_t", [128, 768])
        sin_t = sb("sin_t", [128, 768])
        pT2 = [sb("pT%d" % i, [128, 5, 512], BF16) for i in range(2)]
        den_single = sb("den0", [128, 512])
        den2 = [den_single, den_single]
        m01 = sb("m01", [128, 2, 512], BF16)
        ones_row = sb("ones_row", [1, 128])
        esrow = sb("esrow", [1, 2, 512])
        pslot = [0]
        qsb = sb("qsb", [128, 512])
        t1 = sb("t1", [128, 512])
        t2 = sb("t2", [128, 512])
        xin_sb = sb("xin_sb", [128, 514])
        p_sb = sb("p_sb", [128, 514])
        acc = sb("acc", [128, 512])
        gsb = sb("gsb", [128, 512])
        wb = [sb("wb%d" % i, [128, NCH, 512], BF16) for i in range(3)]
        modv_t = sb("modv_t", [128, NCH, 8])
        ng_t = sb("ng_t", [128, NCH, 2])
        gm = sb("gm", [128, NCH, 4])
        grow_s = sb("grow_s", [128, 2, 512])
        convw_t = sb("convw_t", [128, 8, 4])
        esink = sb("esink", [128, 8])
        hval_t = sb("hval_t", [128, 4])
        cst_t = sb("cst_t", [128, 3, 128])
        ones_bf = sb("ones_bf", [128, 128], BF16)
        bmask_t = sb("bmask_t", [128, 3, 128])
        ss2 = [sb("ss%d" % i, [128, 8]) for i in range(2)]
        halo2 = sb("halo2", [128, NCH, 2], BF16)
        ps = [st.enter_context(nc.psum_tensor("ps%d" % i, [128, 512], F32)) for i in range(8)]

        P = Prog(nc)
        bank_ctr = [0]

        def nb():
            b = bank_ctr[0] % 8
            bank_ctr[0] += 1
            return b

        ld = lambda dst, src, key, q="sp": P.dma(q, lambda e: e.dma_start(out=dst, in_=src), "c_" + key, writes=[key])
        ld(modv_t[:], modv, "modv")
        ld(ng_t[:], ng, "ng")
        ld(convw_t[:], convw, "convw")
        ld(hval_t[:], hval, "hval")
        ld(cst_t[:], cst, "cst")
        ld(bmask_t[:], bmask, "bmask")
        ld(esink[:], sinks.partition_broadcast(128), "esink")
        P.act(lambda e: e.activation(esink[:], esink[:], AF.Exp), reads=["esink"], writes=["esink"])
        P.dve(lambda e: e.memset(ones_bf[:], 1.0), writes=["ones"])
        P.dve(lambda e: e.memset(ones_row[:], 1.0), writes=["ones"])
        for mi, wi in enumerate((0, 2)):
            for hh in range(4):
                P.dve(lambda e: e.tensor_copy(m01[:, mi, hh * 128:(hh + 1) * 128], bmask_t[:, wi, :]), reads=["bmask"], writes=["m01"])
        for h8 in range(8):
            P.dve(lambda e: e.tensor_scalar(esrow[0:1, h8 // 4, (h8 % 4) * 128:(h8 % 4 + 1) * 128], ones_row[0:1, :],
                                            esink[0:1, h8:h8 + 1], None, ALU.mult), reads=["ones", "esink"], writes=["esrow"])
        for j, (gi, sci) in enumerate([(0, 1), (0, 3), (1, 5), (1, 7)]):
            P.dve(lambda e, j=j, gi=gi, sci=sci: e.scalar_tensor_tensor(
                gm[:, :, j], modv_t[:, :, sci], 1.0, ng_t[:, :, gi], ALU.add, ALU.mult),
                reads=["modv", "ng"], writes=["gm"])
        ident = cst_t[:, 0, :]
        rotp = cst_t[:, 1, :]

        wslot = [0]

        wcache = {}
        hwq = [0]

        def wload(srcs, gid):
            s = wslot[0] % 3
            wslot[0] += 1
            kcn = srcs[0][2]
            ncols = max(c0 + n for _, c0, _, n in srcs)
            if gid not in wcache:
                for src, c0, kcn_, n in srcs:
                    P.dma("pool", lambda e: e.dma_start(out=wb[s][:, 0:kcn_, c0:c0 + n], in_=src), "w%d" % s, writes=["wb%d" % s])
                scr = nc.dram_tensor("scr_" + gid, [128, NCH, 512], BF16, kind="Internal").ap()
                wcache[gid] = scr
                P.dma("sp", lambda e: e.dma_start(out=scr[:, 0:kcn, 0:ncols], in_=wb[s][:, 0:kcn, 0:ncols]), "wst%d" % s,
                      reads=["wb%d" % s], writes=["scr_" + gid])
            else:
                scr = wcache[gid]
                q = "sp" if hwq[0] % 2 == 0 else "pool"
                hwq[0] += 1
                P.dma(q, lambda e: e.dma_start(out=wb[s][:, 0:kcn, 0:ncols], in_=scr[:, 0:kcn, 0:ncols]), "w%d%s" % (s, q),
                      reads=["scr_" + gid], writes=["wb%d" % s])
            return s

        gslot = [0]

        def gload(gi, nt):
            sl = gslot[0] % 2
            gslot[0] += 1
            P.dma("sp", lambda e: e.dma_start(out=grow_s[:, sl, :], in_=grow[gi:gi + 1, nt * 512:(nt + 1) * 512].partition_broadcast(128)),
                  "c_grow%d" % sl, writes=["grow%d" % sl])
            return sl

        def norm_block(src_ap, key_src, dstT, col0, gcol, shcol, key_dst):
            ns = nslot[0] % 2
            nslot[0] += 1
            xhat, ss, kx, ks = xhat2[ns], ss2[ns], "xhat%d" % ns, "ss%d" % ns
            P.dve(lambda e: e.memset(ss[:, 0:1], 0.0), writes=[ks])
            P.act(lambda e: e.activation(xhat[:], src_ap, AF.Square, accum_out=ss[:, 0:1]),
                  reads=[key_src, ks], writes=[kx, ks])
            P.act(lambda e: e.activation(ss[:, 1:2], ss[:, 0:1], AF.Sqrt, bias=EPS, scale=1.0 / D),
                  reads=[ks], writes=[ks])
            P.dve(lambda e: e.reciprocal(ss[:, 2:3], ss[:, 1:2]), reads=[ks], writes=[ks])
            P.dve(lambda e: e.tensor_scalar(xhat[:], src_ap, ss[:, 2:3], None, ALU.mult),
                  reads=[key_src, ks, kx], writes=[kx])
            for q4 in range(4):
                b = nb()
                for j in range(4):
                    c = q4 * 4 + j
                    P.pe(lambda e: e.transpose(ps[b][:, j * 128:(j + 1) * 128], xhat[:, c * 128:(c + 1) * 128], ident),
                         reads=[kx, "cst"], writes=["ps%d" % b])
                for j in range(4):
                    c = q4 * 4 + j
                    P.act(lambda e: e.activation(
                        dstT(c, col0), ps[b][:, j * 128:(j + 1) * 128], AF.Identity,
                        bias=modv_t[:, c, shcol:shcol + 1], scale=gm[:, c, gcol:gcol + 1]),
                        reads=["ps%d" % b, "gm", "modv"], writes=[key_dst])

        xnT = lambda c, col0, w=128: bufA[:, c * 768 + col0: c * 768 + col0 + w]
        unT = lambda c, col0, w=128: bufB[:, c, col0:col0 + w]

        def segment(kind, ti, kv_only=False):
            lat = kind == "lat"
            nown = 4 if lat else 2
            next_ = 6 if lat else 2
            own0 = 128 if lat else 0
            NO = nown * 128
            NE = next_ * 128
            src = xe if lat else ctx
            row0 = ti * 512 if lat else 0
            g1i, g2i = (0, 1) if lat else (2, 3)
            gc1, sh1, gc2, sh2 = (0, 0, 2, 4) if lat else (1, 2, 3, 6)
            dst = h1 if lat else hc1
            tag = "%s%d" % (kind, ti)
            if lat:
                P.dma("sp", lambda e: e.dma_start(out=cos_t[:], in_=cosT[:, row0:row0 + 768]), "c_cos", writes=["cos"])
                P.dma("sp", lambda e: e.dma_start(out=sin_t[:], in_=sinT[:, row0:row0 + 768]), "c_sin", writes=["sin"])
            for eb in range(next_):
                if lat and eb in (0, 5):
                    ap = stg[:, 0, :]
                    key = "stg0"
                else:
                    ob = eb - 1 if lat else eb
                    ap = hbuf[:, ob, :]
                    key = "hb%d" % ob
                P.dma("sp", lambda e, ap=ap, eb=eb: e.dma_start(out=ap, in_=src[row0 + eb * 128: row0 + (eb + 1) * 128, :]),
                      "c_" + key, writes=[key])
                norm_block(ap, key, xnT, eb * 128, gc1, sh1, "xnT")
            if lat:
                P.dve(lambda e: e.tensor_copy(halo2[:, :, 0:1], bufA[:].rearrange("p (c t) -> p c t", t=768)[:, :, 127:128]),
                      reads=["xnT"], writes=["halo2"])
                P.dve(lambda e: e.tensor_copy(halo2[:, :, 1:2], bufA[:].rearrange("p (c t) -> p c t", t=768)[:, :, 640:641]),
                      reads=["xnT"], writes=["halo2"])

            def proj(s, wc0, ncols, col0, n, b):
                for kc in range(NCH):
                    P.pe(lambda e, kc=kc: e.matmul(ps[b][:, 0:n], wb[s][:, kc, wc0:wc0 + 128], xnT(kc, col0, n),
                                                   start=(kc == 0), stop=(kc == NCH - 1)),
                         reads=["wb%d" % s, "xnT"], writes=["ps%d" % b])

            def rope(b, n, col0, dst, view):
                P.act(lambda e: e.copy(qsb[:, 0:n], ps[b][:, 0:n]), reads=["ps%d" % b], writes=["qsb"])
                b2 = nb()
                P.pe(lambda e: e.matmul(ps[b2][:, 0:n], rotp, qsb[:, 0:n], start=True, stop=True),
                     reads=["qsb", "cst"], writes=["ps%d" % b2])
                P.dve(lambda e: e.tensor_tensor(t1[:, 0:n], qsb[:, 0:n], cos_t[:, col0:col0 + n], ALU.mult),
                      reads=["qsb", "cos"], writes=["t1"])
                P.dve(lambda e: e.tensor_tensor(t2[:, 0:n], ps[b2][:, 0:n], sin_t[:, col0:col0 + n], ALU.mult),
                      reads=["ps%d" % b2, "sin"], writes=["t2"])
                P.dve(lambda e: e.tensor_tensor(dst, view(t1[:, 0:n]), view(t2[:, 0:n]), ALU.add),
                      reads=["t1", "t2"], writes=["qk"])

            for g2 in range(0 if kv_only else 2):
                s = wload([(w_in_v[:, :, g2 * 512:(g2 + 1) * 512], 0, NCH, 512)], "q%d" % g2)
                for j in range(4):
                    hd = g2 * 4 + j
                    b = nb()
                    proj(s, j * 128, 128, own0, NO, b)

                    v3 = lambda a: a.rearrange("p (a b) -> p a b", b=128)
                    if lat:
                        rope(b, NO, own0, qT[:, 0:nown, hd, :], v3)
                    else:
                        P.act(lambda e, b=b, hd=hd: e.copy(qT[:, 0:nown, hd, :], v3(ps[b][:, 0:NO])),
                              reads=["ps%d" % b], writes=["qk"])
            s = wload([(w_in_v[:, :, 1024:1536], 0, NCH, 512)], "kv")
            kdst = kT if lat else kTc
            for g in range(2):
                for (c0, n) in ([(0, 512), (512, 256)] if lat else [(0, 256)]):
                    b = nb()
                    proj(s, g * 128, 128, c0, n, b)

                    if lat:
                        rope(b, n, c0, kdst[:, g, c0:c0 + n], lambda a: a)
                    else:
                        P.act(lambda e, b=b, g=g, c0=c0, n=n: e.copy(kdst[:, g, c0:c0 + n], ps[b][:, 0:n]),
                              reads=["ps%d" % b], writes=["qk"])
            vdst = Vt if lat else Vc
            for eb in range(next_):
                b = nb()
                for kc in range(NCH):
                    P.pe(lambda e, kc=kc, eb=eb, b=b: e.matmul(ps[b][:, 0:256], xnT(kc, eb * 128), wb[s][:, kc, 256:512],
                                                              start=(kc == 0), stop=(kc == NCH - 1)),
                         reads=["wb%d" % s, "xnT"], writes=["ps%d" % b])
                P.act(lambda e, eb=eb, b=b: e.copy(vdst[:, eb, :], ps[b][:, 0:256]), reads=["ps%d" % b], writes=["qk"])
            if kv_only:
                return
            kv_blocks = (lambda qb: [("c", 0), ("c", 1), ("w", qb), ("w", qb + 1), ("w", qb + 2)]) if lat else \
                        (lambda qb: [("c", 0), ("c", 1)])
            for qb in range(nown):
                for g in range(2):
                    blks = kv_blocks(qb)
                    pp = pslot[0] % 2
                    pslot[0] += 1
                    pTt = pT2[pp]
                    for j, (kind_b, eb) in enumerate(blks):
                        b = nb()
                        ksrc = (kTc if (kind_b == "c") else kT)
                        P.pe(lambda e: e.matmul(ps[b][:, :], ksrc[:, g, eb * 128:(eb + 1) * 128],
                                                qT[:, qb, 4 * g:4 * g + 4, :].rearrange("p a b -> p (a b)"), start=True, stop=True),
                             reads=["qk"], writes=["ps%d" % b])
                        pk = "pT%d_%d" % (pp, j)
                        if kind_b == "w" and ((eb == 0 and ti == 0) or (eb == 5 and ti == n_tiles - 1)):
                            hb = hval_t[:, 2:3] if eb == 0 else hval_t[:, 3:4]
                            P.act(lambda e: e.activation(pTt[:, j, :], ps[b][:, :], AF.Exp, bias=hb, scale=ATT_SCALE),
                                  reads=["ps%d" % b, "hval"], writes=[pk])
                        else:
                            P.act(lambda e: e.activation(pTt[:, j, :], ps[b][:, :], AF.Exp, scale=ATT_SCALE),
                                  reads=["ps%d" % b], writes=[pk])
                        if kind_b == "w" and eb - qb != 1:
                            mi = 0 if eb - qb == 0 else 1
                            P.dve(lambda e: e.tensor_tensor(pTt[:, j, :], pTt[:, j, :], m01[:, mi, :], ALU.mult),
                                  reads=[pk, "m01"], writes=[pk])
                    bd = nb()
                    bo = nb()
                    nblk = len(blks)
                    for j, (kind_b, eb) in enumerate(blks):
                        P.pe(lambda e: e.matmul(ps[bd][:, :], ones_bf[:], pTt[:, j, :], start=(j == 0), stop=False),
                             reads=["ones", "pT%d_%d" % (pp, j)], writes=["ps%d" % bd])
                    P.pe(lambda e: e.matmul(ps[bd][:, :], ones_row[0:1, :], esrow[0:1, g, :], start=False, stop=True),
                         reads=["ones", "esrow"], writes=["ps%d" % bd])
                    for j, (kind_b, eb) in enumerate(blks):
                        vsrc = Vc if kind_b == "c" else Vt
                        P.pe(lambda e: e.matmul(ps[bo][:, :], vsrc[:, eb, g * 128:(g + 1) * 128], pTt[:, j, :],
                                                start=(j == 0), stop=(j == nblk - 1)),
                             reads=["qk", "pT%d_%d" % (pp, j)], writes=["ps%d" % bo])
                    P.dve(lambda e: e.reciprocal(den2[pp][:], ps[bd][:, :]), reads=["ps%d" % bd], writes=["den0"])
                    P.dve(lambda e: e.tensor_tensor(
                        bufB[:, 4 * g:4 * g + 4, qb * 128:(qb + 1) * 128],
                        ps[bo][:, :].rearrange("p (a b) -> p a b", b=128),
                        den2[pp][:].rearrange("p (a b) -> p a b", b=128), ALU.mult),
                        reads=["ps%d" % bo, "den0"], writes=["mixT"])
            for c in range(8):
                s = wload([(w_in_v[:, :, 1536 + c * 128:1536 + (c + 1) * 128], 0, NCH, 128),
                           (w_in_v[:, :, 2560 + c * 128:2560 + (c + 1) * 128], 128, NCH, 128),
                           (w_in_v[:, :, 3584 + c * 128:3584 + (c + 1) * 128], 256, NCH, 128)], "cv%d" % c)
                bx, bb, bc = nb(), nb(), nb()
                proj(s, 0, 128, own0, NO, bx)
                proj(s, 128, 128, own0, NO, bb)
                proj(s, 256, 128, own0, NO, bc)
                P.act(lambda e, bx=bx: e.copy(xin_sb[:, 1:1 + NO], ps[bx][:, 0:NO]), reads=["ps%d" % bx], writes=["xin_sb"])
                P.dve(lambda e, bc=bc: e.tensor_tensor(p_sb[:, 1:1 + NO], ps[bc][:, 0:NO], xin_sb[:, 1:1 + NO], ALU.mult),
                      reads=["ps%d" % bc, "xin_sb"], writes=["p_sb"])
                if lat:
                    bh = nb()
                    for kc in range(NCH):
                        P.pe(lambda e, kc=kc, bh=bh, s=s: e.matmul(ps[bh][:, 0:2], wb[s][:, kc, 0:128], halo2[:, kc, :],
                                                                 start=(kc == 0), stop=(kc == NCH - 1)),
                             reads=["wb%d" % s, "halo2"], writes=["ps%d" % bh])
                    for kc in range(NCH):
                        P.pe(lambda e, kc=kc, bh=bh, s=s: e.matmul(ps[bh][:, 2:4], wb[s][:, kc, 256:384], halo2[:, kc, :],
                                                                 start=(kc == 0), stop=(kc == NCH - 1)),
                             reads=["wb%d" % s, "halo2"], writes=["ps%d" % bh])
                    P.act(lambda e, bh=bh: e.copy(t2[:, 0:2], ps[bh][:, 0:2]), reads=["ps%d" % bh], writes=["t2"])
                    P.dve(lambda e, bh=bh: e.tensor_tensor(t1[:, 0:2], ps[bh][:, 2:4], t2[:, 0:2], ALU.mult),
                          reads=["ps%d" % bh, "t2"], writes=["t1"])
                    if ti == 0:
                        P.dve(lambda e: e.tensor_tensor(p_sb[:, 0:1], t1[:, 0:1], hval_t[:, 0:1], ALU.mult),
                              reads=["t1", "hval"], writes=["p_sb"])
                    else:
                        P.dve(lambda e: e.tensor_copy(p_sb[:, 0:1], t1[:, 0:1]), reads=["t1"], writes=["p_sb"])
                    if ti == n_tiles - 1:
                        P.dve(lambda e: e.tensor_tensor(p_sb[:, 513:514], t1[:, 1:2], hval_t[:, 1:2], ALU.mult),
                              reads=["t1", "hval"], writes=["p_sb"])
                    else:
                        P.dve(lambda e: e.tensor_copy(p_sb[:, 513:514], t1[:, 1:2]), reads=["t1"], writes=["p_sb"])
                else:
                    P.dve(lambda e: e.memset(p_sb[:, 0:1], 0.0), writes=["p_sb"])
                    P.dve(lambda e: e.memset(p_sb[:, 1 + NO:2 + NO], 0.0), writes=["p_sb"])
                P.dve(lambda e, c=c: e.tensor_scalar(acc[:, 0:NO], p_sb[:, 0:NO], convw_t[:, c, 0:1], convw_t[:, c, 3:4],
                                                    ALU.mult, ALU.add), reads=["p_sb", "convw"], writes=["acc"])
                P.dve(lambda e, c=c: e.scalar_tensor_tensor(acc[:, 0:NO], p_sb[:, 1:1 + NO], convw_t[:, c, 1:2], acc[:, 0:NO],
                                                           ALU.mult, ALU.add), reads=["p_sb", "convw", "acc"], writes=["acc"])
                P.dve(lambda e, c=c: e.scalar_tensor_tensor(acc[:, 0:NO], p_sb[:, 2:2 + NO], convw_t[:, c, 2:3], acc[:, 0:NO],
                                                           ALU.mult, ALU.add), reads=["p_sb", "convw", "acc"], writes=["acc"])
                P.dve(lambda e, c=c, bb=bb: e.tensor_tensor(bufB[:, 8 + c, 0:NO], ps[bb][:, 0:NO], acc[:, 0:NO], ALU.mult),
                      reads=["ps%d" % bb, "acc"], writes=["mixT"])
            for nt in range(4):
                s = wload([(w_out_v[:, :, nt * 512:(nt + 1) * 512], 0, NCH, 512)], "wo%d" % nt)
                gs = gload(g1i, nt)
                for ob in range(nown):
                    b = nb()
                    for kc in range(NCH):
                        P.pe(lambda e, kc=kc, ob=ob, b=b, s=s: e.matmul(ps[b][:, :], bufB[:, kc, ob * 128:(ob + 1) * 128],
                                                                        wb[s][:, kc, :], start=(kc == 0), stop=(kc == NCH - 1)),
                             reads=["wb%d" % s, "mixT"], writes=["ps%d" % b])
                    P.dve(lambda e, b=b, gs=gs: e.tensor_tensor(gsb[:], ps[b][:, :], grow_s[:, gs, :], ALU.mult),
                          reads=["ps%d" % b, "grow%d" % gs], writes=["gsb"])
                    P.dve(lambda e, ob=ob, nt=nt: e.tensor_tensor(hbuf[:, ob, nt * 512:(nt + 1) * 512],
                                                                  hbuf[:, ob, nt * 512:(nt + 1) * 512], gsb[:], ALU.add),
                          reads=["gsb", "hb%d" % ob], writes=["hb%d" % ob])
            for ob in range(nown):
                norm_block(hbuf[:, ob, :], "hb%d" % ob, unT, ob * 128, gc2, sh2, "mixT")
            aT = lambda fc, col0, w: bufA[:, fc * 512 + col0: fc * 512 + col0 + w]
            for half in range(2):
                for pr in range(11):
                    f0 = (half * 22 + pr * 2) * 128
                    s = wload([(w_g_v[:, :, f0:f0 + 256], 0, NCH, 256), (w_u_v[:, :, f0:f0 + 256], 256, NCH, 256)], "gu%d_%d" % (half, pr))
                    for j in range(2):
                        bg_, bu_ = nb(), nb()
                        for kc in range(NCH):
                            P.pe(lambda e, kc=kc, j=j, s=s, b=bg_: e.matmul(ps[b][:, 0:NO], wb[s][:, kc, j * 128:(j + 1) * 128],
                                                                           bufB[:, kc, 0:NO], start=(kc == 0), stop=(kc == NCH - 1)),
                                 reads=["wb%d" % s, "mixT"], writes=["ps%d" % bg_])
                        for kc in range(NCH):
                            P.pe(lambda e, kc=kc, j=j, s=s, b=bu_: e.matmul(ps[b][:, 0:NO], wb[s][:, kc, 256 + j * 128:256 + (j + 1) * 128],
                                                                           bufB[:, kc, 0:NO], start=(kc == 0), stop=(kc == NCH - 1)),
                                 reads=["wb%d" % s, "mixT"], writes=["ps%d" % bu_])
                        P.act(lambda e, b=bg_: e.activation(gsb[:, 0:NO], ps[b][:, 0:NO], AF.Silu), reads=["ps%d" % bg_], writes=["gsb"])
                        fc = pr * 2 + j
                        P.dve(lambda e, b=bu_, fc=fc: e.tensor_tensor(aT(fc, 0, NO), ps[b][:, 0:NO], gsb[:, 0:NO], ALU.mult),
                              reads=["ps%d" % bu_, "gsb", "xnT"], writes=["xnT"])
                for nt in range(4):
                    banks = [nb() for _ in range(nown)]
                    gs = gload(g2i, nt)
                    for kh in range(2):
                        fc0 = half * 22 + kh * 11
                        s = wload([(w_d_v[:, fc0:fc0 + 11, nt * 512:(nt + 1) * 512], 0, 11, 512)], "wd%d_%d_%d" % (half, nt, kh))
                        for ob in range(nown):
                            b = banks[ob]
                            for k in range(11):
                                P.pe(lambda e, k=k, kh=kh, ob=ob, b=b, s=s: e.matmul(
                                    ps[b][:, :], aT(kh * 11 + k, ob * 128, 128), wb[s][:, k, :],
                                    start=(kh == 0 and k == 0), stop=(kh == 1 and k == 10)),
                                    reads=["wb%d" % s, "xnT"], writes=["ps%d" % b])
                    for ob in range(nown):
                        b = banks[ob]
                        P.dve(lambda e, b=b, gs=gs: e.tensor_tensor(gsb[:], ps[b][:, :], grow_s[:, gs, :], ALU.mult),
                              reads=["ps%d" % b, "grow%d" % gs], writes=["gsb"])
                        P.dve(lambda e, ob=ob, nt=nt: e.tensor_tensor(hbuf[:, ob, nt * 512:(nt + 1) * 512],
                                                                      hbuf[:, ob, nt * 512:(nt + 1) * 512], gsb[:], ALU.add),
                              reads=["gsb", "hb%d" % ob], writes=["hb%d" % ob])
            orow0 = ti * 512 if lat else 0
            for ob in range(nown):
                P.dma("sp", lambda e, ob=ob: e.dma_start(out=dst[orow0 + ob * 128: orow0 + (ob + 1) * 128, :], in_=hbuf[:, ob, :]),
                      "o_%s%d" % (kind, ob), reads=["hb%d" % ob], final=True)

        segment("ctx", 0, kv_only=True)
        for ti in range(n_tiles):
            segment("lat", ti)
        segment("ctx", 0)
        P.emit()
    return nc


D = 2048
NCH = 16
EPS = 1e-6


def build_BC(mode, T):
    nc = bass.Bass("TRN2", target_bir_lowering=False)
    dt = lambda n, s, k="ExternalInput", d=F32: nc.dram_tensor(n, s, d, kind=k).ap()
    NB = T // 128
    h1 = dt("h1", [T, D])
    halo = dt("halo", [3, D])
    modv = dt("modv", [128, NCH, 6])
    ng1 = dt("ng1", [128, NCH, 2])
    w_in = dt("w_in", [D, 2 * D])
    convw = dt("convw", [128, NCH, 5])
    ga_w = dt("ga_w", [2, 16, 128, 128])
    gx_w = dt("gx_w", [2, 16, 128, 128])
    gab = dt("gab", [128, NCH, 4])
    lam = dt("lam", [128, NCH, 2])
    hval = dt("hval", [128, 2])
    ident_d = dt("ident", [128, 128])
    if mode == "B":
        hc1 = dt("hc1", [256, D])
        stats = dt("stats", [128, NCH, 4], "ExternalOutput")
        cstate = dt("cstate", [128, NCH, 2], "ExternalOutput")
    else:
        chain = dt("chain", [128, NCH, 14])
        w_out = dt("w_out", [D, D])
        rows = dt("rows", [1, D])
        rw = dt("rw", [128, NCH, 8])
        rb = dt("rb", [1, 8])
        h2 = dt("h2", [T, D], "ExternalOutput")
        u_o = dt("uT", [D, T], "ExternalOutput")
        wts = dt("wts", [T, 8], "ExternalOutput")
        yT_d = nc.dram_tensor("yT_d", [NCH, 128, T], BF16, kind="Internal").ap()
        w_out_v = w_out.rearrange("(kc p) n -> p kc n", p=128)
    w_in_v = w_in.rearrange("(kc p) n -> p kc n", p=128)
    TE = T + 3
    tiles = [(c0, min(512, T - c0)) for c0 in range(0, T, 512)]

    with contextlib.ExitStack() as st:
        sb = lambda n, s, d=F32: st.enter_context(nc.sbuf_tensor(n, s, d))
        xnT = sb("xnT", [128, NCH, TE], BF16)
        blk = sb("blk", [128, D])
        xhat = sb("xhat", [128, D])
        xr2 = [sb("xr%d" % i, [128, max(TE, 2048)]) for i in range(2)]
        xc2 = [sb("xc%d" % i, [128, T]) for i in range(2)]
        xcb2 = [sb("xcb%d" % i, [128, T], BF16) for i in range(2)]
        xr = xr2[0]
        av = [sb("a%d" % z, [128, T]) for z in range(2)]
        bv = [sb("b%d" % z, [128, T]) for z in range(2)]
        r_t = sb("r_t", [128, 512])
        wb = [sb("wb%d" % i, [128, NCH, 256], BF16) for i in range(2)]
        gw = [sb("gw%d" % i, [128, 4, 128], BF16) for i in range(2)]
        modv_t = sb("modv_t", [128, NCH, 6])
        ng_t = sb("ng_t", [128, NCH, 2])
        gm = sb("gm", [128, NCH, 3])
        convw_t = sb("convw_t", [128, NCH, 5])
        gab_t = sb("gab_t", [128, NCH, 4])
        lam_t = sb("lam_t", [128, NCH, 2])
        negc = sb("negc", [128, NCH, 2])
        neg2c = sb("neg2c", [128, NCH, 2])
        hval_t = sb("hval_t", [128, 2])
        ident = sb("ident_t", [128, 128])
        ss = sb("ss", [128, 8])
        racc = sb("racc", [128, 2, 8])
        if mode == "B":
            stats_t = sb("stats_t", [128, NCH, 4])
            cst_t = sb("cst_t", [128, NCH, 2])
        else:
            gel = sb("gel", [128, T])
            ybf = sb("ybf", [128, T], BF16)
            chain_t = sb("chain_t", [128, NCH, 14])
            Hin = sb("Hin", [128, NCH, 2])
            rows_t = sb("rows_t", [128, D])
            rw_t = sb("rw_t", [128, NCH, 8])
            rb_t = sb("rb_t", [128, 8])
            lg = sb("lg", [128, 8, 8])
        ps = [st.enter_context(nc.psum_tensor("ps%d" % i, [128, 512], F32)) for i in range(8)]

        P = Prog(nc)
        bank_ctr = [0]

        def nb():
            b = bank_ctr[0] % 8
            bank_ctr[0] += 1
            return b

        ld = lambda dst, src, key, q="sp": P.dma(q, lambda e: e.dma_start(out=dst, in_=src), "c_" + key, writes=[key])
        ld(modv_t[:], modv, "modv")
        ld(ng_t[:], ng1, "ng")
        ld(convw_t[:], convw, "convw")
        ld(gab_t[:], gab, "gab")
        ld(lam_t[:], lam, "lam")
        ld(hval_t[:], hval, "hval")
        ld(ident[:], ident_d, "ident")
        P.act(lambda e: e.activation(negc[:], lam_t[:], AF.Exp, scale=-1.0), reads=["lam"], writes=["negc"])
        P.act(lambda e: e.activation(negc[:], negc[:], AF.Ln, bias=1.0), reads=["negc"], writes=["negc"])
        P.dve(lambda e: e.tensor_scalar(neg2c[:], negc[:], -16.0, None, ALU.mult), reads=["negc"], writes=["neg2c"])
        P.dve(lambda e: e.tensor_scalar(negc[:], negc[:], -8.0, None, ALU.mult), reads=["negc", "neg2c"], writes=["negc"])
        for j, (sci, gi) in enumerate([(1, 0), (3, 0), (5, 1)]):
            P.dve(lambda e, j=j, sci=sci: e.scalar_tensor_tensor(gm[:, :, j], modv_t[:, :, sci], 1.0, ng_t[:, :, gi], ALU.add, ALU.mult),
                  reads=["modv", "ng"], writes=["gm"])
        if mode == "C":
            ld(chain_t[:], chain, "chain")
            ld(rw_t[:], rw, "rw")
            ld(rb_t[:], rb.partition_broadcast(128), "rb")
            ld(rows_t[:], rows.partition_broadcast(128), "rows3")
            P.dve(lambda e: e.tensor_copy(Hin[:], chain_t[:, :, 0:2]), reads=["chain"], writes=["Hin"])
            for z in range(2):
                for j in range(3):
                    ca = 2 + z * 6 + 2 * j
                    P.dve(lambda e, z=z, ca=ca: e.tensor_tensor(Hin[:, :, z], Hin[:, :, z], chain_t[:, :, ca], ALU.mult),
                          reads=["Hin", "chain"], writes=["Hin"])
                    P.dve(lambda e, z=z, ca=ca: e.tensor_tensor(Hin[:, :, z], Hin[:, :, z], chain_t[:, :, ca + 1], ALU.add),
                          reads=["Hin", "chain"], writes=["Hin"])

        def norm_block(src_ap, key_src, ncols, col0, gcol, shcol):
            P.dve(lambda e: e.memset(ss[:, 0:1], 0.0), writes=["ss"])
            P.act(lambda e: e.activation(xhat[:], src_ap, AF.Square, accum_out=ss[:, 0:1]),
                  reads=[key_src, "ss"], writes=["xhat", "ss"])
            P.act(lambda e: e.activation(ss[:, 1:2], ss[:, 0:1], AF.Sqrt, bias=EPS, scale=1.0 / D), reads=["ss"], writes=["ss"])
            P.dve(lambda e: e.reciprocal(ss[:, 2:3], ss[:, 1:2]), reads=["ss"], writes=["ss"])
            P.dve(lambda e: e.tensor_scalar(xhat[:], src_ap, ss[:, 2:3], None, ALU.mult),
                  reads=[key_src, "ss", "xhat"], writes=["xhat"])
            for q4 in range(4):
                b = nb()
                for j in range(4):
                    c = q4 * 4 + j
                    P.pe(lambda e: e.transpose(ps[b][:, j * 128:(j + 1) * 128], xhat[:, c * 128:(c + 1) * 128], ident[:]),
                         reads=["xhat", "ident"], writes=["ps%d" % b])
                for j in range(4):
                    c = q4 * 4 + j
                    P.act(lambda e: e.activation(xnT[:, c, col0:col0 + ncols], ps[b][:, j * 128:j * 128 + ncols], AF.Identity,
                                                 bias=modv_t[:, c, shcol:shcol + 1], scale=gm[:, c, gcol:gcol + 1]),
                          reads=["ps%d" % b, "gm", "modv"], writes=["xnT"])

        wslot = [0]

        def mixer(src, Ts, has_halo, gcol, shcol, out_stats):
            nblk = Ts // 128
            P.dve(lambda e: e.memset(blk[:], 0.0), writes=["blk"])
            if has_halo:
                P.dma("sp", lambda e: e.dma_start(out=blk[0:3, :], in_=halo), "c_blk", writes=["blk"])
            norm_block(blk[:], "blk", 3, 0, gcol, shcol)
            P.dve(lambda e: e.tensor_copy(xnT[:, :, Ts + 2:Ts + 3], xnT[:, :, 2:3]), reads=["xnT"], writes=["xnT"])
            for ob in range(nblk):
                P.dma("sp", lambda e: e.dma_start(out=blk[:], in_=src[ob * 128:(ob + 1) * 128, :]), "c_blk", writes=["blk"])
                norm_block(blk[:], "blk", 128, 2 + ob * 128, gcol, shcol)
            tl = [(c0, min(512, Ts - c0)) for c0 in range(0, Ts, 512)]
            wbase = wslot[0]
            wslot[0] += NCH

            def front(h):
                s = (wbase + h) % 2
                xr, xc, xcb = xr2[s], xc2[s], xcb2[s]
                kxr, kxc, kxcb = "xr%d" % s, "xc%d" % s, "xcb%d" % s
                if s == 0:
                    kxr = "xr"
                r_all, i_all = blk, xhat
                P.dma("pool", lambda e: e.dma_start(out=wb[s][:, :, 0:128], in_=w_in_v[:, :, h * 128:(h + 1) * 128]),
                      "w%d" % s, writes=["wb%d" % s])
                P.dma("pool", lambda e: e.dma_start(out=wb[s][:, :, 128:256], in_=w_in_v[:, :, D + h * 128:D + (h + 1) * 128]),
                      "w%d" % s, writes=["wb%d" % s])
                P.dma("pool", lambda e: e.dma_start(out=gw[s][:, 0:2, :], in_=ga_w[:, h, :, :].rearrange("z i j -> i z j")),
                      "gw%d" % s, writes=["gw%d" % s])
                P.dma("pool", lambda e: e.dma_start(out=gw[s][:, 2:4, :], in_=gx_w[:, h, :, :].rearrange("z i j -> i z j")),
                      "gw%d" % s, writes=["gw%d" % s])
                for (c0, n) in [(0, min(512, Ts + 3))] + [(c, min(512, Ts + 3 - c)) for c in range(512, Ts + 3, 512)]:
                    b = nb()
                    for kc in range(NCH):
                        P.pe(lambda e: e.matmul(ps[b][:, 0:n], wb[s][:, kc, 128:256], xnT[:, kc, c0:c0 + n],
                                                start=(kc == 0), stop=(kc == NCH - 1)),
                             reads=["wb%d" % s, "xnT"], writes=["ps%d" % b])
                    P.act(lambda e: e.copy(xr[:, c0:c0 + n], ps[b][:, 0:n]), reads=["ps%d" % b], writes=[kxr])
                if has_halo:
                    P.dve(lambda e: e.tensor_scalar(xr[:, 0:2], xr[:, 0:2], hval_t[:, 0:1], None, ALU.mult),
                          reads=[kxr, "hval"], writes=[kxr])
                    P.dve(lambda e: e.tensor_scalar(xr[:, Ts + 2:Ts + 3], xr[:, Ts + 2:Ts + 3], hval_t[:, 1:2], None, ALU.mult),
                          reads=[kxr, "hval"], writes=[kxr])
                else:
                    P.dve(lambda e: e.memset(xr[:, 0:2], 0.0), reads=[kxr], writes=[kxr])
                    P.dve(lambda e: e.memset(xr[:, Ts + 2:Ts + 3], 0.0), reads=[kxr], writes=[kxr])
                P.dve(lambda e: e.tensor_scalar(xc[:, 0:Ts], xr[:, 0:Ts], convw_t[:, h, 0:1], convw_t[:, h, 4:5], ALU.mult, ALU.add),
                      reads=[kxr, "convw"], writes=[kxc])
                for k in range(1, 4):
                    P.dve(lambda e: e.scalar_tensor_tensor(xc[:, 0:Ts], xr[:, k:k + Ts], convw_t[:, h, k:k + 1], xc[:, 0:Ts],
                                                           ALU.mult, ALU.add), reads=[kxr, "convw", kxc], writes=[kxc])
                P.dve(lambda e: e.tensor_copy(xcb[:, 0:Ts], xc[:, 0:Ts]), reads=[kxc], writes=[kxcb])

            def back(h):
                s = (wbase + h) % 2
                xr, xc, xcb = xr2[s], xc2[s], xcb2[s]
                kxr, kxc, kxcb = "xr%d" % s, "xc%d" % s, "xcb%d" % s
                if s == 0:
                    kxr = "xr"
                r_all, i_all = blk, xhat
                if mode == "C":
                    for (c0, n) in tl:
                        b = nb()
                        for kc in range(NCH):
                            P.pe(lambda e: e.matmul(ps[b][:, 0:n], wb[s][:, kc, 0:128], xnT[:, kc, 2 + c0:2 + c0 + n],
                                                    start=(kc == 0), stop=(kc == NCH - 1)),
                                 reads=["wb%d" % s, "xnT"], writes=["ps%d" % b])
                        P.act(lambda e: e.activation(gel[:, c0:c0 + n], ps[b][:, 0:n], AF.Gelu), reads=["ps%d" % b], writes=["gel"])
                if out_stats is not None:
                    P.dve(lambda e: e.memset(racc[:], 0.0), writes=["racc"])
                for z in range(2):
                    for ti, (c0, n) in enumerate(tl):
                        br = nb()
                        P.pe(lambda e: e.matmul(ps[br][:, 0:n], gw[s][:, z, :], xcb[:, c0:c0 + n], start=True, stop=True),
                             reads=["gw%d" % s, kxcb], writes=["ps%d" % br])
                        if out_stats is not None:
                            P.act(lambda e: e.activation(r_all[:, c0:c0 + n], ps[br][:, 0:n], AF.Sigmoid, bias=gab_t[:, h, z:z + 1],
                                                         accum_out=racc[:, z, ti:ti + 1]),
                                  reads=["ps%d" % br, "gab", "racc"], writes=["blk", "racc"])
                        else:
                            P.act(lambda e: e.activation(r_all[:, c0:c0 + n], ps[br][:, 0:n], AF.Sigmoid, bias=gab_t[:, h, z:z + 1]),
                                  reads=["ps%d" % br, "gab"], writes=["blk"])
                    for ti, (c0, n) in enumerate(tl):
                        bi = nb()
                        P.pe(lambda e: e.matmul(ps[bi][:, 0:n], gw[s][:, 2 + z, :], xcb[:, c0:c0 + n], start=True, stop=True),
                             reads=["gw%d" % s, kxcb], writes=["ps%d" % bi])
                        P.act(lambda e: e.activation(i_all[:, c0:c0 + n], ps[bi][:, 0:n], AF.Sigmoid, bias=gab_t[:, h, 2 + z:3 + z]),
                              reads=["ps%d" % bi, "gab"], writes=["xhat"])
                    P.act(lambda e: e.activation(av[z][:, 0:Ts], r_all[:, 0:Ts], AF.Exp, scale=negc[:, h, z:z + 1]),
                          reads=["blk", "negc"], writes=["a%d" % z])
                    P.act(lambda e: e.activation(r_all[:, 0:Ts], r_all[:, 0:Ts], AF.Exp, scale=neg2c[:, h, z:z + 1]),
                          reads=["blk", "neg2c"], writes=["blk"])
                    P.act(lambda e: e.activation(r_all[:, 0:Ts], r_all[:, 0:Ts], AF.Sqrt, bias=1.0, scale=-1.0),
                          reads=["blk"], writes=["blk"])
                    P.dve(lambda e: e.tensor_tensor(i_all[:, 0:Ts], i_all[:, 0:Ts], r_all[:, 0:Ts], ALU.mult),
                          reads=["xhat", "blk"], writes=["xhat"])
                    P.dve(lambda e: e.tensor_tensor(bv[z][:, 0:Ts], i_all[:, 0:Ts], xc[:, 0:Ts], ALU.mult),
                          reads=["xhat", kxc], writes=["b%d" % z])
                if out_stats is None:
                    P.dve(lambda e: e.tensor_tensor_scan(bv[0][:, 0:Ts], av[0][:, 0:Ts], bv[0][:, 0:Ts], Hin[:, h, 0:1], ALU.mult, ALU.add),
                          reads=["a0", "b0", "Hin"], writes=["b0"])
                    P.dve(lambda e: e.tensor_tensor_scan(bv[1][:, Ts - 1::-1], av[1][:, Ts - 1::-1], bv[1][:, Ts - 1::-1],
                                                         Hin[:, h, 1:2], ALU.mult, ALU.add),
                          reads=["a1", "b1", "Hin"], writes=["b1"])
                    P.dve(lambda e: e.tensor_tensor(bv[0][:, 0:Ts], bv[0][:, 0:Ts], bv[1][:, 0:Ts], ALU.add),
                          reads=["b0", "b1"], writes=["b0"])
                    P.dve(lambda e: e.tensor_tensor(ybf[:, 0:Ts], bv[0][:, 0:Ts], gel[:, 0:Ts], ALU.mult),
                          reads=["b0", "gel"], writes=["ybf"])
                    P.dma("sp", lambda e: e.dma_start(out=yT_d[h], in_=ybf[:, 0:Ts]), "c_y", reads=["ybf"], writes=["yT_d"])
                else:
                    tile_, kind = out_stats
                    P.dve(lambda e: e.tensor_tensor_scan(bv[0][:, 0:Ts], av[0][:, 0:Ts], bv[0][:, 0:Ts], 0.0, ALU.mult, ALU.add),
                          reads=["a0", "b0"], writes=["b0"])
                    P.dve(lambda e: e.tensor_tensor_scan(bv[1][:, Ts - 1::-1], av[1][:, Ts - 1::-1], bv[1][:, Ts - 1::-1],
                                                         0.0, ALU.mult, ALU.add),
                          reads=["a1", "b1"], writes=["b1"])
                    if kind == "lat":
                        P.dve(lambda e: e.tensor_copy(tile_[:, h, 1:2], bv[0][:, Ts - 1:Ts]), reads=["b0"], writes=["stt"])
                        P.dve(lambda e: e.tensor_copy(tile_[:, h, 3:4], bv[1][:, 0:1]), reads=["b1"], writes=["stt"])
                        for z in range(2):
                            P.dve(lambda e: e.reduce_sum(ss[:, 4 + z:5 + z], racc[:, z, :], axis=AX.X), reads=["racc"], writes=["ss"])
                            P.act(lambda e: e.activation(tile_[:, h, 2 * z:2 * z + 1], ss[:, 4 + z:5 + z], AF.Exp, scale=negc[:, h, z:z + 1]),
                                  reads=["ss", "negc"], writes=["stt"])
                    else:
                        P.dve(lambda e: e.tensor_copy(tile_[:, h, 0:1], bv[0][:, Ts - 1:Ts]), reads=["b0"], writes=["stt"])
                        P.dve(lambda e: e.tensor_copy(tile_[:, h, 1:2], bv[1][:, 0:1]), reads=["b1"], writes=["stt"])


            front(0)
            for h in range(NCH):
                if h + 1 < NCH:
                    front(h + 1)
                back(h)

        if mode == "B":
            mixer(hc1, 256, False, 1, 2, (cst_t, "ctx"))
            mixer(h1, T, True, 0, 0, (stats_t, "lat"))
            P.dma("sp", lambda e: e.dma_start(out=stats, in_=stats_t[:]), "o_st", reads=["stt"], final=True)
            P.dma("sp", lambda e: e.dma_start(out=cstate, in_=cst_t[:]), "o_cs", reads=["stt"], final=True)
        else:
            mixer(h1, T, True, 0, 0, None)
            yT_v = yT_d.rearrange("c p t -> p c t")
            if T >= 2048:
                hosts = [(xc2[0], "xc0"), (xc2[1], "xc1"), (av[0], "a0"), (av[1], "a1"), (bv[0], "b0"), (bv[1], "b1"),
                         (gel, "gel"), (xr2[1], "xr1")]
                wres = [(t_[:, 0:2048].bitcast(BF16).rearrange("p (kc n) -> p kc n", n=256), k_) for t_, k_ in hosts]
            else:
                wres_t = [st.enter_context(nc.sbuf_tensor("wres%d" % i, [128, NCH, 256], BF16)) for i in range(8)]
                wres = [(wres_t[i][:], "wres%d" % i) for i in range(8)]
            for nt in range(8):
                P.dma("pool", lambda e: e.dma_start(out=wres[nt][0], in_=w_out_v[:, :, nt * 256:(nt + 1) * 256]),
                      "wres%d" % nt, writes=[wres[nt][1]])
            for ob in range(NB):
                P.dma("sp", lambda e: e.dma_start(out=xnT[:, :, 0:128], in_=yT_v[:, :, ob * 128:(ob + 1) * 128]), "c_yl",
                      reads=["yT_d"], writes=["xnT"])
                P.dma("sp", lambda e: e.dma_start(out=blk[:], in_=h1[ob * 128:(ob + 1) * 128, :]), "c_blk", writes=["blk"])
                for nt in range(8):
                    b = nb()
                    for kc in range(NCH):
                        P.pe(lambda e: e.matmul(ps[b][:, 0:256], xnT[:, kc, 0:128], wres[nt][0][:, kc, :], start=(kc == 0), stop=(kc == NCH - 1)),
                             reads=[wres[nt][1], "xnT"], writes=["ps%d" % b])
                    P.dve(lambda e: e.tensor_tensor(r_t[:, 0:256], ps[b][:, 0:256], rows_t[:, nt * 256:(nt + 1) * 256], ALU.mult),
                          reads=["ps%d" % b, "rows3"], writes=["r_t"])
                    P.dve(lambda e: e.tensor_tensor(blk[:, nt * 256:(nt + 1) * 256], blk[:, nt * 256:(nt + 1) * 256], r_t[:, 0:256], ALU.add),
                          reads=["r_t", "blk"], writes=["blk"])
                P.dma("sp", lambda e: e.dma_start(out=h2[ob * 128:(ob + 1) * 128, :], in_=blk[:]), "o_h2", reads=["blk"], final=True)
                P.dve(lambda e: e.memset(ss[:, 0:1], 0.0), writes=["ss"])
                P.act(lambda e: e.activation(xhat[:], blk[:], AF.Square, accum_out=ss[:, 0:1]), reads=["blk", "ss"], writes=["xhat", "ss"])
                P.act(lambda e: e.activation(ss[:, 1:2], ss[:, 0:1], AF.Sqrt, bias=EPS, scale=1.0 / D), reads=["ss"], writes=["ss"])
                P.dve(lambda e: e.reciprocal(ss[:, 2:3], ss[:, 1:2]), reads=["ss"], writes=["ss"])
                P.dve(lambda e: e.tensor_scalar(xhat[:], blk[:], ss[:, 2:3], None, ALU.mult),
                      reads=["blk", "ss", "xhat"], writes=["xhat"])
                uT = xr[:, 0:2048].rearrange("p (c t) -> p c t", t=128)
                for q4 in range(4):
                    b = nb()
                    for j in range(4):
                        c = q4 * 4 + j
                        P.pe(lambda e: e.transpose(ps[b][:, j * 128:(j + 1) * 128], xhat[:, c * 128:(c + 1) * 128], ident[:]),
                             reads=["xhat", "ident"], writes=["ps%d" % b])
                    for j in range(4):
                        c = q4 * 4 + j
                        P.act(lambda e: e.activation(uT[:, c, :], ps[b][:, j * 128:(j + 1) * 128], AF.Identity,
                                                     bias=modv_t[:, c, 4:5], scale=gm[:, c, 2:3]),
                              reads=["ps%d" % b, "gm", "modv"], writes=["xr"])
                P.dma("sp", lambda e: e.dma_start(out=u_o.rearrange("(c p) t -> p c t", p=128)[:, :, ob * 128:(ob + 1) * 128], in_=uT),
                      "o_u", reads=["xr"], final=True)
                if True:
                    pass
                b = nb()
                for kc in range(NCH):
                    P.pe(lambda e: e.matmul(ps[b][:, 0:8], uT[:, kc, :], rw_t[:, kc, :], start=(kc == 0), stop=(kc == NCH - 1)),
                         reads=["xr", "rw"], writes=["ps%d" % b])
                L, m1, k1, L2, m2, k2, e2, w1 = [lg[:, i, :] for i in range(8)]
                P.dve(lambda e: e.tensor_tensor(L, ps[b][:, 0:8], rb_t[:], ALU.add), reads=["ps%d" % b, "rb"], writes=["lg"])
                P.dve(lambda e: e.reduce_max(m1[:, 0:1], L, axis=AX.X), reads=["lg"], writes=["lg"])
                P.dve(lambda e: e.tensor_scalar(k1, L, m1[:, 0:1], None, ALU.is_equal), reads=["lg"], writes=["lg"])
                P.dve(lambda e: e.scalar_tensor_tensor(L2, k1, -1e30, L, ALU.mult, ALU.add), reads=["lg"], writes=["lg"])
                P.dve(lambda e: e.reduce_max(m2[:, 0:1], L2, axis=AX.X), reads=["lg"], writes=["lg"])
                P.dve(lambda e: e.tensor_scalar(k2, L2, m2[:, 0:1], None, ALU.is_equal), reads=["lg"], writes=["lg"])
                P.dve(lambda e: e.tensor_tensor(e2[:, 0:1], m2[:, 0:1], m1[:, 0:1], ALU.subtract), reads=["lg"], writes=["lg"])
                P.act(lambda e: e.activation(e2[:, 0:1], e2[:, 0:1], AF.Exp), reads=["lg"], writes=["lg"])
                P.dve(lambda e: e.tensor_scalar(w1[:, 0:1], e2[:, 0:1], 1.0, None, ALU.add), reads=["lg"], writes=["lg"])
                P.dve(lambda e: e.reciprocal(w1[:, 0:1], w1[:, 0:1]), reads=["lg"], writes=["lg"])
                P.dve(lambda e: e.tensor_tensor(w1[:, 1:2], e2[:, 0:1], w1[:, 0:1], ALU.mult), reads=["lg"], writes=["lg"])
                P.dve(lambda e: e.tensor_scalar(k1, k1, w1[:, 0:1], None, ALU.mult), reads=["lg"], writes=["lg"])
                P.dve(lambda e: e.scalar_tensor_tensor(k1, k2, w1[:, 1:2], k1, ALU.mult, ALU.add), reads=["lg"], writes=["lg"])
                P.dma("sp", lambda e: e.dma_start(out=wts[ob * 128:(ob + 1) * 128, :], in_=k1), "o_w", reads=["lg"], final=True)
        P.emit()
    return nc


D = 2048
NCH = 16
EPS = 1e-6


def build_M():
    nc = bass.Bass("TRN2", target_bir_lowering=False)
    dt = lambda n, s, k="ExternalInput", d=F32: nc.dram_tensor(n, s, d, kind=k).ap()
    wm = dt("wm", [2, D, 1536])
    bm = dt("bm", [128, 2, 12])
    cv = dt("cv", [128, NCH, 3])
    out = dt("mout", [128, 2, 12, 3], "ExternalOutput")
    with contextlib.ExitStack() as st:
        sb = lambda n, s, d=F32: st.enter_context(nc.sbuf_tensor(n, s, d))
        w_t = sb("w_t", [128, NCH, 1536])
        bm_t = sb("bm_t", [128, 2, 12])
        cv_t = sb("cv_t", [128, NCH, 3])
        o_t = sb("o_t", [128, 2, 12, 3])
        ps = [st.enter_context(nc.psum_tensor("ps%d" % i, [128, 512], F32)) for i in range(2)]
        P = Prog(nc)
        P.dma("sp", lambda e: e.dma_start(out=bm_t[:], in_=bm), "c_bm", writes=["bm"])
        P.dma("sp", lambda e: e.dma_start(out=cv_t[:], in_=cv), "c_cv", writes=["cv"])
        P.act(lambda e: e.activation(cv_t[:], cv_t[:], AF.Silu), reads=["cv"], writes=["cv"])
        for l in range(2):
            for half in range(2):
                P.dma("sp" if half == 0 else "act", lambda e: e.dma_start(out=w_t[:, :, half * 768:(half + 1) * 768],
                                                  in_=wm[l].rearrange("(kc p) n -> p kc n", p=128)[:, :, half * 768:(half + 1) * 768]),
                      "c_w%d" % half, writes=["w%d" % half])
            for j in range(12):
                b = j % 2
                for kc in range(NCH):
                    P.pe(lambda e: e.matmul(ps[b][:, 0:3], w_t[:, kc, j * 128:(j + 1) * 128], cv_t[:, kc, :],
                                            start=(kc == 0), stop=(kc == NCH - 1)), reads=["w%d" % (j // 6), "cv"], writes=["ps%d" % b])
                P.dve(lambda e: e.tensor_scalar(o_t[:, l, j, :], ps[b][:, 0:3], bm_t[:, l, j:j + 1], None, ALU.add),
                      reads=["ps%d" % b, "bm"], writes=["o"])
        P.dma("sp", lambda e: e.dma_start(out=out, in_=o_t[:]), "o_o", reads=["o"], final=True)
        P.emit()
    return nc


def build_D(R, DFF=7168):
    NFC = DFF // 128
    nc = bass.Bass("TRN2", target_bir_lowering=False)
    dt = lambda n, s, k="ExternalInput", d=F32: nc.dram_tensor(n, s, d, kind=k).ap()
    uT = dt("uT", [D, R])
    wsel = dt("wsel", [128, R // 128])
    w_g = dt("w_g", [D, DFF])
    w_u = dt("w_u", [D, DFF])
    w_d = dt("w_d", [DFF, D])
    y = dt("y", [R, D], "ExternalOutput")
    uT_v = uT.rearrange("(kc p) r -> p kc r", p=128)
    w_g_v = w_g.rearrange("(kc p) n -> p kc n", p=128)
    w_u_v = w_u.rearrange("(kc p) n -> p kc n", p=128)
    w_d_v = w_d.rearrange("(fc p) n -> p fc n", p=128)
    KQ = 14
    ST = 1024
    n_super = (R + ST - 1) // ST
    with contextlib.ExitStack() as st:
        sb = lambda n, s, d=F32: st.enter_context(nc.sbuf_tensor(n, s, d))
        un = sb("un", [128, NCH, ST], BF16)
        aT = sb("aT", [128, NFC, ST], BF16)
        wb = [sb("wb%d" % i, [128, NCH, 512], BF16) for i in range(3)]
        gsb = sb("gsb", [128, 512])
        ot = [sb("ot%d" % i, [128, 512]) for i in range(4)]
        ws_t = sb("ws_t", [128, 8])
        ps = [st.enter_context(nc.psum_tensor("ps%d" % i, [128, 512], F32)) for i in range(8)]
        P = Prog(nc)
        bank_ctr = [0]

        def nb():
            b = bank_ctr[0] % 8
            bank_ctr[0] += 1
            return b
        wslot = [0]
        oslot = [0]
        wcache = {}
        hwq = [0]

        def wload(srcs, gid):
            s = wslot[0] % 3
            wslot[0] += 1
            kcn = srcs[0][2]
            ncols = max(c0 + n for _, c0, _, n in srcs)
            if gid not in wcache:
                for src, c0, kcn_, n in srcs:
                    P.dma("pool", lambda e: e.dma_start(out=wb[s][:, 0:kcn_, c0:c0 + n], in_=src), "w%d" % s, writes=["wb%d" % s])
                if R > 512:
                    scr = nc.dram_tensor("scr_" + gid, [128, NCH, 512], BF16, kind="Internal").ap()
                    wcache[gid] = scr
                    P.dma("sp", lambda e: e.dma_start(out=scr[:, 0:kcn, 0:ncols], in_=wb[s][:, 0:kcn, 0:ncols]), "wst%d" % s,
                          reads=["wb%d" % s], writes=["scr_" + gid])
            else:
                scr = wcache[gid]
                q = "sp" if hwq[0] % 2 == 0 else "pool"
                hwq[0] += 1
                P.dma(q, lambda e: e.dma_start(out=wb[s][:, 0:kcn, 0:ncols], in_=scr[:, 0:kcn, 0:ncols]), "w%d%s" % (s, q),
                      reads=["scr_" + gid], writes=["wb%d" % s])
            return s
        for rt in range(n_super):
            r0 = rt * ST
            NR = min(ST, R - r0)
            NOB = NR // 128
            halves = [(c0, min(512, NR - c0)) for c0 in range(0, NR, 512)]
            P.dma("pool", lambda e: e.dma_start(out=un[:, :, 0:NR], in_=uT_v[:, :, r0:r0 + NR]), "c_un", writes=["un"])
            P.dma("sp", lambda e: e.dma_start(out=ws_t[:, 0:NOB], in_=wsel[:, rt * 8:rt * 8 + NOB]), "c_ws", writes=["ws"])
            for pr in range(NFC // 2):
                f0 = pr * 256
                s = wload([(w_g_v[:, :, f0:f0 + 256], 0, NCH, 256), (w_u_v[:, :, f0:f0 + 256], 256, NCH, 256)], "gu%d" % pr)
                for j in range(2):
                    for (c0, n) in halves:
                        bg_, bu_ = nb(), nb()
                        for kc in range(NCH):
                            P.pe(lambda e: e.matmul(ps[bg_][:, 0:n], wb[s][:, kc, j * 128:(j + 1) * 128], un[:, kc, c0:c0 + n],
                                                    start=(kc == 0), stop=(kc == NCH - 1)), reads=["wb%d" % s, "un"], writes=["ps%d" % bg_])
                        for kc in range(NCH):
                            P.pe(lambda e: e.matmul(ps[bu_][:, 0:n], wb[s][:, kc, 256 + j * 128:256 + (j + 1) * 128], un[:, kc, c0:c0 + n],
                                                    start=(kc == 0), stop=(kc == NCH - 1)), reads=["wb%d" % s, "un"], writes=["ps%d" % bu_])
                        P.act(lambda e: e.activation(gsb[:, 0:n], ps[bg_][:, 0:n], AF.Silu), reads=["ps%d" % bg_], writes=["gsb"])
                        P.dve(lambda e: e.tensor_tensor(aT[:, pr * 2 + j, c0:c0 + n], ps[bu_][:, 0:n], gsb[:, 0:n], ALU.mult),
                              reads=["ps%d" % bu_, "gsb"], writes=["aT"])
            for hb in range((NOB + 3) // 4):
                obs = list(range(hb * 4, min(NOB, hb * 4 + 4)))
                for nt in range(4):
                    banks = {ob: nb() for ob in obs}
                    for kq in range(NFC // KQ):
                        s = wload([(w_d_v[:, kq * KQ:(kq + 1) * KQ, nt * 512:(nt + 1) * 512], 0, KQ, 512)], "wd%d_%d" % (nt, kq))
                        for ob in obs:
                            b = banks[ob]
                            for k in range(KQ):
                                P.pe(lambda e: e.matmul(ps[b][:, :], aT[:, kq * KQ + k, ob * 128:(ob + 1) * 128], wb[s][:, k, :],
                                                        start=(kq == 0 and k == 0), stop=(kq == NFC // KQ - 1 and k == KQ - 1)),
                                     reads=["wb%d" % s, "aT"], writes=["ps%d" % b])
                    for ob in obs:
                        b = banks[ob]
                        os_ = oslot[0] % 4
                        oslot[0] += 1
                        P.dve(lambda e: e.tensor_scalar(ot[os_][:], ps[b][:, :], ws_t[:, ob:ob + 1], None, ALU.mult),
                              reads=["ps%d" % b, "ws"], writes=["ot%d" % os_])
                        P.dma("act", lambda e: e.dma_start(out=y[r0 + ob * 128:r0 + (ob + 1) * 128, nt * 512:(nt + 1) * 512], in_=ot[os_][:]),
                              "o_y%d" % os_, reads=["ot%d" % os_], final=True)
        P.emit()
    return nc


def build_E(T):
    nc = bass.Bass("TRN2", target_bir_lowering=False)
    dt = lambda n, s, k="ExternalInput", d=F32: nc.dram_tensor(n, s, d, kind=k).ap()
    h2 = dt("h2", [T, D])
    yp = dt("yp", [T, 2, D])
    rows = dt("rows", [2, D])
    out = dt("out", [T, D], "ExternalOutput")
    with contextlib.ExitStack() as st:
        sb = lambda n, s, d=F32: st.enter_context(nc.sbuf_tensor(n, s, d))
        hb = [sb("hb%d" % i, [128, D]) for i in range(2)]
        yb = [sb("yb%d" % i, [128, 2, D]) for i in range(2)]
        junk = sb("junk", [128, D])
        rows_t = sb("rows_t", [128, 2, D])
        ss = sb("ss", [128, 2, 4])
        P = Prog(nc)
        for i in range(2):
            P.dma("sp", lambda e: e.dma_start(out=rows_t[:, i, :], in_=rows[i:i + 1, :].partition_broadcast(128)), "c_r%d" % i, writes=["rows"])
        for ob in range(T // 128):
            s = ob % 2
            H, Y, S_ = hb[s], yb[s], ss[:, s, :]
            P.dma("sp", lambda e: e.dma_start(out=H[:], in_=h2[ob * 128:(ob + 1) * 128, :]), "c_h%d" % s, writes=["h%d" % s])
            P.dma("act", lambda e: e.dma_start(out=Y[:], in_=yp[ob * 128:(ob + 1) * 128, :, :]), "c_y%d" % s, writes=["y%d" % s])
            P.dve(lambda e: e.tensor_tensor(Y[:, 0, :], Y[:, 0, :], Y[:, 1, :], ALU.add), reads=["y%d" % s], writes=["y%d" % s])
            P.dve(lambda e: e.tensor_tensor(Y[:, 0, :], Y[:, 0, :], rows_t[:, 0, :], ALU.mult), reads=["y%d" % s, "rows"], writes=["y%d" % s])
            P.dve(lambda e: e.tensor_tensor(H[:], H[:], Y[:, 0, :], ALU.add), reads=["y%d" % s, "h%d" % s], writes=["h%d" % s])
            P.dve(lambda e: e.memset(S_[:, 0:1], 0.0), writes=["ss%d" % s])
            P.act(lambda e: e.activation(junk[:], H[:], AF.Square, accum_out=S_[:, 0:1]), reads=["h%d" % s, "ss%d" % s], writes=["junk", "ss%d" % s])
            P.act(lambda e: e.activation(S_[:, 1:2], S_[:, 0:1], AF.Sqrt, bias=EPS, scale=1.0 / D), reads=["ss%d" % s], writes=["ss%d" % s])
            P.dve(lambda e: e.reciprocal(S_[:, 2:3], S_[:, 1:2]), reads=["ss%d" % s], writes=["ss%d" % s])
            P.dve(lambda e: e.scalar_tensor_tensor(H[:], H[:], S_[:, 2:3], rows_t[:, 1, :], ALU.mult, ALU.mult),
                  reads=["h%d" % s, "ss%d" % s, "rows"], writes=["h%d" % s])
            P.dma("sp", lambda e: e.dma_start(out=out[ob * 128:(ob + 1) * 128, :], in_=H[:]), "o_%d" % s, reads=["h%d" % s], final=True)
        P.emit()
    return nc

import numpy as np

GRID_W = 64


def fm(v):
    return np.ascontiguousarray(np.asarray(v, np.float32).reshape(16, 128).T)


def rope_tables(pos):
    pos = np.asarray(pos, np.int64)
    row = (pos // GRID_W).astype(np.float32)
    col = (pos % GRID_W).astype(np.float32)
    inv = (10000.0 ** (-np.arange(32, dtype=np.float32) / 32)).astype(np.float32)
    ar = row[:, None] * inv
    ac = col[:, None] * inv
    ang = np.concatenate([ar, ar, ac, ac], axis=-1)
    sgn = np.ones(128, np.float32)
    sgn[0:32] = -1
    sgn[64:96] = -1
    return (np.ascontiguousarray(np.cos(ang).T.astype(np.float32)),
            np.ascontiguousarray((np.sin(ang) * sgn).T.astype(np.float32)))


def consts_A():
    cst = np.zeros((128, 3, 128), np.float32)
    cst[:, 0, :] = np.eye(128, dtype=np.float32)
    perm = np.arange(128)
    perm[0:32] += 32
    perm[32:64] -= 32
    perm[64:96] += 32
    perm[96:128] -= 32
    for m in range(128):
        cst[perm[m], 1, m] = 1.0
    j = np.arange(128)[:, None]
    i = np.arange(128)[None, :]
    bm = np.zeros((128, 3, 128), np.float32)
    for wi in range(3):
        jj = wi * 128 + j
        valid = (jj >= i) & (jj <= i + 256)
        bm[:, wi, :] = np.where(valid, 1.0, 0.0)
    return cst, bm


def prep_A(x_b, ctx_b, mod0, core_in_seq, n_cores_seq, n_tiles, p):
    T = 512 * n_tiles
    S = x_b.shape[0]
    lo = core_in_seq * T - 128
    xe = np.zeros((T + 256, 2048), np.float32)
    a, b = max(lo, 0), min(lo + T + 256, S)
    xe[a - lo:b - lo] = x_b[a:b]
    pos = np.arange(lo, lo + T + 256)
    cosT, sinT = rope_tables(np.clip(pos, 0, S - 1))
    vl = 1.0 if core_in_seq > 0 else 0.0
    vr = 1.0 if core_in_seq < n_cores_seq - 1 else 0.0
    hval = np.zeros((128, 4), np.float32)
    hval[:, 0] = vl
    hval[:, 1] = vr
    hval[:, 2] = 0.0 if vl else -1e30
    hval[:, 3] = 0.0 if vr else -1e30
    modv = np.stack([fm(mod0["sh1_l"]), fm(mod0["sc1_l"]), fm(mod0["sh1_c"]), fm(mod0["sc1_c"]),
                     fm(mod0["sh2_l"]), fm(mod0["sc2_l"]), fm(mod0["sh2_c"]), fm(mod0["sc2_c"])], axis=-1)
    grow = np.stack([mod0["g1_l"], mod0["g2_l"], mod0["g1_c"], mod0["g2_c"]]).astype(np.float32)
    ng = np.stack([fm(p["norm1_g"]), fm(p["norm2_g"])], axis=-1)
    cw = np.zeros((128, 8, 4), np.float32)
    for k in range(3):
        cw[:, :, k] = p["conv_w"][k].reshape(8, 128).T
    cw[:, :, 3] = p["conv_b"].reshape(8, 128).T
    cst, bm = consts_A()
    return {"xe": xe, "ctx": np.ascontiguousarray(ctx_b, dtype=np.float32), "modv": np.ascontiguousarray(modv),
            "grow": np.ascontiguousarray(grow), "ng": np.ascontiguousarray(ng),
            "w_in": p["w_in"], "w_out": p["w_out"], "w_g": p["ffn_w_gate"], "w_u": p["ffn_w_up"], "w_d": p["ffn_w_down"],
            "convw": cw, "sinks": p["sinks"].reshape(1, 8).astype(np.float32), "cosT": cosT, "sinT": sinT,
            "hval": hval, "cst": cst, "bmask": bm}

import numpy as np

def fm2(a):
    return np.ascontiguousarray(np.stack([fm(r) for r in a], axis=-1))

def prep_BC_common(h1_own, halo3, vl, vr, mod1, p):
    hval = np.zeros((128, 2), np.float32); hval[:, 0] = vl; hval[:, 1] = vr
    cw = np.concatenate([p["conv_w"], p["conv_b"][None]], axis=0)
    return {"h1": np.ascontiguousarray(h1_own, dtype=np.float32), "halo": np.ascontiguousarray(halo3, dtype=np.float32),
            "modv": fm2([mod1["sh1_l"], mod1["sc1_l"], mod1["sh1_c"], mod1["sc1_c"], mod1["sh2_l"], mod1["sc2_l"]]),
            "ng1": fm2([p["norm1_g"], p["norm2_g"]]), "w_in": p["w_in"], "convw": fm2(cw),
            "ga_w": p["gate_a_w"], "gx_w": p["gate_x_w"],
            "gab": fm2([p["gate_a_b"][0], p["gate_a_b"][1], p["gate_x_b"][0], p["gate_x_b"][1]]),
            "lam": fm2([p["lambda"][0], p["lambda"][1]]), "hval": hval, "ident": np.eye(128, dtype=np.float32)}

def prep_C_extra(chain14, mod1, p):
    rw = np.ascontiguousarray(p["router_w"].reshape(16, 128, 8).transpose(1, 0, 2)).astype(np.float32)
    return {"chain": np.ascontiguousarray(chain14, dtype=np.float32), "w_out": p["w_out"],
            "rows": mod1["g1_l"].reshape(1, 2048).astype(np.float32), "rw": rw,
            "rb": p["router_b"].reshape(1, 8).astype(np.float32)}


N_CORES = 8
T_CORE = 2048
SEQ = 8192


def _run(nc, in_maps):
    res = run_bass_kernel_spmd(nc, in_maps, core_ids=list(range(len(in_maps))))
    return res.results


def _unfm(t):
    return np.ascontiguousarray(t.transpose(1, 0)).reshape(-1)


def kernel(**inp):
    f32 = lambda a: np.ascontiguousarray(np.asarray(a), dtype=np.float32)
    x, c, ctx, c_ctx = f32(inp["x"]), f32(inp["c"]), f32(inp["ctx"]), f32(inp["c_ctx"])
    p0 = {k[3:]: f32(v) for k, v in inp.items() if k.startswith("l0_")}
    p1 = {k[3:]: f32(v) for k, v in inp.items() if k.startswith("l1_")}
    fng = f32(inp["final_norm_g"])

    cvs = [c[0], c[1], c_ctx]
    cv = np.ascontiguousarray(np.stack([fm(v) for v in cvs], axis=-1))
    maps = []
    for j in range(N_CORES):
        sl = slice(j * 1536, (j + 1) * 1536)
        wm = np.ascontiguousarray(np.stack([p0["w_mod"][:, sl], p1["w_mod"][:, sl]]))
        bm = np.ascontiguousarray(np.stack([p0["b_mod"][sl].reshape(12, 128).T, p1["b_mod"][sl].reshape(12, 128).T], axis=1))
        maps.append({"wm": wm, "bm": bm, "cv": cv})
    rM = _run(build_M(), maps)
    mod = np.zeros((2, 3, 12288), np.float32)
    for j in range(N_CORES):
        mo = rM[j]["mout"]
        for l in range(2):
            for v in range(3):
                mod[l, v, j * 1536:(j + 1) * 1536] = mo[:, l, :, v].T.reshape(-1)
    names = ["sh1", "sc1", "g1", "sh2", "sc2", "g2"]

    def modd(l, b):
        d = {}
        for i, n in enumerate(names):
            d[n + "_l"] = mod[l, b, i * 2048:(i + 1) * 2048]
            d[n + "_c"] = mod[l, 2, i * 2048:(i + 1) * 2048]
        return d

    maps = [prep_A(x[cid // 4], ctx[cid // 4], modd(0, cid // 4), cid % 4, 4, 4, p0) for cid in range(N_CORES)]
    rA = _run(build_A(4), maps)
    h1 = [rA[cid]["h1"] for cid in range(N_CORES)]
    hc1 = [rA[(cid // 4) * 4]["hc1"] for cid in range(N_CORES)]
    del maps

    commons = []
    for cid in range(N_CORES):
        k = cid % 4
        halo = np.zeros((3, 2048), np.float32)
        if k > 0:
            halo[0:2] = h1[cid - 1][-2:]
        if k < 3:
            halo[2] = h1[cid + 1][0]
        commons.append(prep_BC_common(h1[cid], halo, 1.0 if k > 0 else 0.0, 1.0 if k < 3 else 0.0, modd(1, cid // 4), p1))
    rB = _run(build_BC("B", T_CORE), [dict(commons[cid], hc1=hc1[cid]) for cid in range(N_CORES)])

    maps = []
    for cid in range(N_CORES):
        b, k = cid // 4, cid % 4
        chain = np.zeros((128, 16, 14), np.float32)
        chain[:, :, 0:2] = rB[cid]["cstate"]
        fwd = [None] * (3 - k) + [b * 4 + cc for cc in range(0, k)]
        bwd = [None] * k + [b * 4 + cc for cc in range(3, k, -1)]
        for j in range(3):
            for z, lst in enumerate((fwd, bwd)):
                ca = 2 + z * 6 + 2 * j
                if lst[j] is None:
                    chain[:, :, ca] = 1.0
                else:
                    st_ = rB[lst[j]]["stats"]
                    chain[:, :, ca] = st_[:, :, 2 * z]
                    chain[:, :, ca + 1] = st_[:, :, 2 * z + 1]
        maps.append(dict(commons[cid], **prep_C_extra(chain, modd(1, b), p1)))
    rC = _run(build_BC("C", T_CORE), maps)
    del maps, commons
    h2 = [rC[cid]["h2"] for cid in range(N_CORES)]
    uT_all = np.concatenate([rC[cid]["uT"] for cid in range(N_CORES)], axis=1)
    wts_all = np.concatenate([rC[cid]["wts"] for cid in range(N_CORES)], axis=0)
    del rC

    idx = [np.nonzero(wts_all[:, e] > 0)[0] for e in range(8)]
    R = max(512, int(-(-max(len(i) for i in idx) // 256) * 256))
    maps = []
    for e in range(8):
        n = len(idx[e])
        us = np.zeros((2048, R), np.float32)
        us[:, :n] = uT_all[:, idx[e]]
        w = np.zeros((R,), np.float32)
        w[:n] = wts_all[idx[e], e]
        maps.append({"uT": us, "wsel": np.ascontiguousarray(w.reshape(-1, 128).T),
                     "w_g": p1["moe_w_gate"][e], "w_u": p1["moe_w_up"][e], "w_d": p1["moe_w_down"][e]})
    rD = _run(build_D(R), maps)
    del maps, uT_all
    n_tok = wts_all.shape[0]
    yp = np.zeros((n_tok, 2, 2048), np.float32)
    slot = np.zeros((n_tok,), np.int64)
    for e in range(8):
        n = len(idx[e])
        ok = slot[idx[e]] < 2
        ii = idx[e][ok]
        yp[ii, slot[ii]] = rD[e]["y"][:n][ok]
        slot[ii] += 1
    del rD

    maps = []
    for cid in range(N_CORES):
        b = cid // 4
        rows = np.ascontiguousarray(np.stack([mod[1, b, 5 * 2048:6 * 2048], fng]))
        maps.append({"h2": h2[cid], "yp": np.ascontiguousarray(yp[cid * T_CORE:(cid + 1) * T_CORE]), "rows": rows})
    rE = _run(build_E(T_CORE), maps)
    out = np.concatenate([rE[cid]["out"] for cid in range(N_CORES)], axis=0).reshape(2, SEQ, 2048)
    return np.ascontiguousarray(out, dtype=np.float32)
```
